# Optimizing a Trainium2 kernel written in Bass

```python
import math
import jax
import jax.numpy as jnp
from jax import lax
import numpy as np

D_MODEL = 1024
BATCH = 2
SEQ = 8192
DEPTH = 2

GRID_W = 64
CTX_LEN = 256
EPS = 1e-6
N_MOD = 6
F32 = jnp.float32

FNET_GROUPS = 4
FNET_GROUP_DIM = 64
FNET_DIM = FNET_GROUPS * FNET_GROUP_DIM
NA_HEADS = 4
NA_HEAD_DIM = 64
NA_DIM = NA_HEADS * NA_HEAD_DIM
NA_WIN_ROWS = 8
NA_WIN_COLS = 16
CONV_DIM = 256
CONV_WIDTH = 3
DIFF_HEADS = 4
DIFF_QK_DIM = 32
DIFF_V_DIM = 64
DIFF_QK_WIDTH = DIFF_HEADS * 2 * DIFF_QK_DIM
DIFF_V_WIDTH = DIFF_HEADS * DIFF_V_DIM
ROPE_BASE = 10000.0
Q_BLOCK = 128
N_BRANCHES = 4
BRANCH_DIM = 256

OFF_A = 0
OFF_B_Q = OFF_A + FNET_DIM
OFF_B_K = OFF_B_Q + NA_DIM
OFF_B_V = OFF_B_K + NA_DIM
OFF_C_B = OFF_B_V + NA_DIM
OFF_C_C = OFF_C_B + CONV_DIM
OFF_C_X = OFF_C_C + CONV_DIM
OFF_D_Q = OFF_C_X + CONV_DIM
OFF_D_K = OFF_D_Q + DIFF_QK_WIDTH
OFF_D_V = OFF_D_K + DIFF_QK_WIDTH
IN_DIM = OFF_D_V + DIFF_V_WIDTH

N_EXPERTS = 256
TOP_K = 8
N_GROUPS = 8
TOPK_GROUPS = 4
EXPERT_DIM = 256
SHARED_DIM = 256
ROUTED_SCALE = 2.5
MOE_BLOCK = 128

kernel_name = 'hybrid_parallel_mixer_moe_dit'


def rmsnorm(x, g):
    xf = x.astype(F32)
    y = xf * lax.rsqrt(jnp.mean(xf * xf, axis=-1, keepdims=True) + EPS)
    return (y * g.astype(F32)).astype(x.dtype)


def ada_chunks(cvec, w, b):
    m = jax.nn.silu(cvec) @ w + b
    return jnp.split(m, N_MOD, axis=-1)


def modulate(h, shift, scale):
    return h * (1.0 + scale) + shift


def fourier_mix(u):
    b, n, _ = u.shape
    ug = u.reshape(b, n, FNET_GROUPS, FNET_GROUP_DIM).astype(F32)
    f = jnp.fft.fft2(ug, axes=(1, 3), norm='ortho').real
    return f.reshape(b, n, FNET_DIM).astype(u.dtype)


def short_conv_mix(gate_b, gate_c, xs, w_conv):
    u = gate_c * xs
    y = lax.conv_general_dilated(
        u, w_conv[:, None, :].astype(u.dtype), window_strides=(1,),
        padding=[(CONV_WIDTH // 2, CONV_WIDTH // 2)],
        dimension_numbers=('NWC', 'WIO', 'NWC'), feature_group_count=CONV_DIM)
    return gate_b * y


def neighbourhood_attention(q, k, v, k_ctx, v_ctx, rel_bias, rows):
    b, s, h, d = q.shape
    wr = min(NA_WIN_ROWS, rows)
    scale = d ** -0.5
    qg = q.reshape(b, rows, GRID_W, h, d)
    kg = k.reshape(b, rows, GRID_W, h, d)
    vg = v.reshape(b, rows, GRID_W, h, d)
    r = jnp.arange(rows)
    row_idx = jnp.clip(r - wr // 2, 0, rows - wr)[:, None] + jnp.arange(wr)[None, :]
    k_band = kg[:, row_idx]
    v_band = vg[:, row_idx]
    cq = jnp.arange(GRID_W)
    col_lo = jnp.clip(cq - NA_WIN_COLS // 2, 0, GRID_W - NA_WIN_COLS)
    col_ok = (cq[None, :] >= col_lo[:, None]) & (cq[None, :] < col_lo[:, None] + NA_WIN_COLS)
    dr_idx = row_idx - r[:, None] + NA_WIN_ROWS - 1
    dc_idx = jnp.clip(cq[None, :] - cq[:, None] + NA_WIN_COLS - 1, 0, 2 * NA_WIN_COLS - 2)
    bias = rel_bias[:, dr_idx[:, None, :, None], dc_idx[None, :, None, :]]
    s_win = jnp.einsum('brqhd,brwkhd->bhrqwk', qg, k_band).astype(F32) * scale + bias.astype(F32)
    s_win = jnp.where(col_ok[:, None, :], s_win, -jnp.inf)
    s_ctx = jnp.einsum('brqhd,blhd->bhrql', qg, k_ctx).astype(F32) * scale
    n_win = wr * GRID_W
    scores = jnp.concatenate([s_win.reshape(b, h, rows, GRID_W, n_win), s_ctx], axis=-1)
    p = jax.nn.softmax(scores, axis=-1).astype(v.dtype)
    p_win = p[..., :n_win].reshape(b, h, rows, GRID_W, wr, GRID_W)
    p_ctx = p[..., n_win:]
    o = (jnp.einsum('bhrqwk,brwkhd->brqhd', p_win, v_band)
         + jnp.einsum('bhrql,blhd->brqhd', p_ctx, v_ctx))
    return o.reshape(b, s, h * d)


def dense_attention(q, k, v):
    b, n, h, d = q.shape
    s = jnp.einsum('bqhd,bkhd->bhqk', q, k).astype(F32) * d ** -0.5
    p = jax.nn.softmax(s, axis=-1).astype(v.dtype)
    return jnp.einsum('bhqk,bkhd->bqhd', p, v).reshape(b, n, h * d)


def axial_rope_tables(row, col):
    half = DIFF_QK_DIM // 2
    inv = 1.0 / (ROPE_BASE ** (jnp.arange(0, half, 2, dtype=F32) / half))
    ang_r = row.astype(F32)[:, None] * inv
    ang_c = col.astype(F32)[:, None] * inv
    return (jnp.cos(ang_r), jnp.sin(ang_r), jnp.cos(ang_c), jnp.sin(ang_c))


def rotate_section(x, cos, sin):
    x1, x2 = jnp.split(x, 2, axis=-1)
    cos = cos[:, None, None, :].astype(x.dtype)
    sin = sin[:, None, None, :].astype(x.dtype)
    return jnp.concatenate([x1 * cos - x2 * sin, x1 * sin + x2 * cos], axis=-1)


def apply_axial_rope(x, tables):
    cos_r, sin_r, cos_c, sin_c = tables
    half = x.shape[-1] // 2
    return jnp.concatenate([rotate_section(x[..., :half], cos_r, sin_r),
                            rotate_section(x[..., half:], cos_c, sin_c)], axis=-1)


def diff_lambda_value(lp, lam_init):
    lp = lp.astype(F32)
    return jnp.exp(jnp.sum(lp[0] * lp[1])) - jnp.exp(jnp.sum(lp[2] * lp[3])) + lam_init


def diff_attention_dense(q, k, v, lam):
    s = jnp.einsum('bqhmd,bkhmd->bhmqk', q, k).astype(F32) * DIFF_QK_DIM ** -0.5
    p = jax.nn.softmax(s, axis=-1)
    a = (p[:, :, 0] - lam * p[:, :, 1]).astype(v.dtype)
    return jnp.einsum('bhqk,bkhd->bqhd', a, v)


def diff_attention_blocked(q, k, v, lam):
    b, s = q.shape[:2]
    nb = s // Q_BLOCK
    qb = jnp.moveaxis(q.reshape(b, nb, Q_BLOCK, DIFF_HEADS, 2, DIFF_QK_DIM), 1, 0)
    o = lax.map(lambda qi: diff_attention_dense(qi, k, v, lam), qb)
    return jnp.moveaxis(o, 0, 1).reshape(b, s, DIFF_HEADS, DIFF_V_DIM)


def diff_out(o, sub_g, lam_init):
    b, n = o.shape[:2]
    return (rmsnorm(o, sub_g) * (1.0 - lam_init)).reshape(b, n, DIFF_V_WIDTH)


def latent_branches(p, kv_b_ctx, kv_d_ctx, rows, rope, conv_w, rel_bias, lam, sub_g, lam_init):
    b, s, _ = p.shape
    l = kv_b_ctx.shape[1]
    y_a = fourier_mix(p[..., OFF_A:OFF_B_Q])
    q_b = p[..., OFF_B_Q:OFF_B_K].reshape(b, s, NA_HEADS, NA_HEAD_DIM)
    k_b = p[..., OFF_B_K:OFF_B_V].reshape(b, s, NA_HEADS, NA_HEAD_DIM)
    v_b = p[..., OFF_B_V:OFF_C_B].reshape(b, s, NA_HEADS, NA_HEAD_DIM)
    k_bc = kv_b_ctx[..., :NA_DIM].reshape(b, l, NA_HEADS, NA_HEAD_DIM)
    v_bc = kv_b_ctx[..., NA_DIM:].reshape(b, l, NA_HEADS, NA_HEAD_DIM)
    y_b = neighbourhood_attention(q_b, k_b, v_b, k_bc, v_bc, rel_bias, rows)
    y_c = short_conv_mix(p[..., OFF_C_B:OFF_C_C], p[..., OFF_C_C:OFF_C_X], p[..., OFF_C_X:OFF_D_Q], conv_w)
    q_d = apply_axial_rope(p[..., OFF_D_Q:OFF_D_K].reshape(b, s, DIFF_HEADS, 2, DIFF_QK_DIM), rope)
    k_d = apply_axial_rope(p[..., OFF_D_K:OFF_D_V].reshape(b, s, DIFF_HEADS, 2, DIFF_QK_DIM), rope)
    v_d = p[..., OFF_D_V:].reshape(b, s, DIFF_HEADS, DIFF_V_DIM)
    k_dc = kv_d_ctx[..., :DIFF_QK_WIDTH].reshape(b, l, DIFF_HEADS, 2, DIFF_QK_DIM)
    v_dc = kv_d_ctx[..., DIFF_QK_WIDTH:].reshape(b, l, DIFF_HEADS, DIFF_V_DIM)
    o_d = diff_attention_blocked(q_d, jnp.concatenate([k_d, k_dc], axis=1),
                                 jnp.concatenate([v_d, v_dc], axis=1), lam)
    y_d = diff_out(o_d, sub_g, lam_init)
    return [y_a, y_b, y_c, y_d]


def context_branches(pc, conv_w, lam, sub_g, lam_init):
    b, l, _ = pc.shape
    y_a = fourier_mix(pc[..., OFF_A:OFF_B_Q])
    q_b = pc[..., OFF_B_Q:OFF_B_K].reshape(b, l, NA_HEADS, NA_HEAD_DIM)
    k_b = pc[..., OFF_B_K:OFF_B_V].reshape(b, l, NA_HEADS, NA_HEAD_DIM)
    v_b = pc[..., OFF_B_V:OFF_C_B].reshape(b, l, NA_HEADS, NA_HEAD_DIM)
    y_b = dense_attention(q_b, k_b, v_b)
    y_c = short_conv_mix(pc[..., OFF_C_B:OFF_C_C], pc[..., OFF_C_C:OFF_C_X], pc[..., OFF_C_X:OFF_D_Q], conv_w)
    q_d = pc[..., OFF_D_Q:OFF_D_K].reshape(b, l, DIFF_HEADS, 2, DIFF_QK_DIM)
    k_d = pc[..., OFF_D_K:OFF_D_V].reshape(b, l, DIFF_HEADS, 2, DIFF_QK_DIM)
    v_d = pc[..., OFF_D_V:].reshape(b, l, DIFF_HEADS, DIFF_V_DIM)
    y_d = diff_out(diff_attention_dense(q_d, k_d, v_d, lam), sub_g, lam_init)
    return [y_a, y_b, y_c, y_d]


def merge_branches(h, branches, w_gate, w_branch, w_o):
    merged = jax.nn.sigmoid(h @ w_gate[0]) * (branches[0] @ w_branch[0])
    for i in range(1, N_BRANCHES):
        merged = merged + jax.nn.sigmoid(h @ w_gate[i]) * (branches[i] @ w_branch[i])
    return merged @ w_o


def route(h, w_router, bias):
    n = h.shape[0]
    scores = jax.nn.sigmoid((h @ w_router).astype(F32))
    grp = (scores + bias.astype(F32)).reshape(n, N_GROUPS, N_EXPERTS // N_GROUPS)
    grp_score = jnp.sum(lax.top_k(grp, 2)[0], axis=-1)
    _, grp_idx = lax.top_k(grp_score, TOPK_GROUPS)
    grp_mask = jnp.any(grp_idx[..., None] == jnp.arange(N_GROUPS), axis=-2)
    choice = jnp.where(grp_mask[..., None], grp, -jnp.inf).reshape(n, N_EXPERTS)
    _, idx = lax.top_k(choice, TOP_K)
    w = jnp.take_along_axis(scores, idx, axis=-1)
    w = w / jnp.sum(w, axis=-1, keepdims=True) * ROUTED_SCALE
    return idx, w


def routed_experts(h, idx, wts, w_g, w_u, w_d):
    n, d = h.shape
    a = n * TOP_K
    flat_e = idx.reshape(a)
    order = jnp.argsort(flat_e)
    sorted_e = flat_e[order]
    counts = jnp.bincount(flat_e, length=N_EXPERTS)
    padded = (counts + MOE_BLOCK - 1) // MOE_BLOCK * MOE_BLOCK
    pad_end = jnp.cumsum(padded)
    pad_start = pad_end - padded
    grp_start = jnp.cumsum(counts) - counts
    dest = pad_start[sorted_e] + jnp.arange(a) - grp_start[sorted_e]
    n_blocks = (a + N_EXPERTS * (MOE_BLOCK - 1)) // MOE_BLOCK
    n_rows = n_blocks * MOE_BLOCK
    row_tok = jnp.full((n_rows,), n, jnp.int32).at[dest].set((order // TOP_K).astype(jnp.int32))
    row_w = jnp.zeros((n_rows,), F32).at[dest].set(wts.reshape(a)[order])
    block_e = jnp.minimum(jnp.searchsorted(pad_end, jnp.arange(n_blocks) * MOE_BLOCK, side='right'),
                          N_EXPERTS - 1)
    h_pad = jnp.concatenate([h, jnp.zeros((1, d), h.dtype)], axis=0)

    def block(acc, args):
        toks, e, w = args
        xb = h_pad[toks]
        yb = (jax.nn.silu(xb @ w_g[e]) * (xb @ w_u[e])) @ w_d[e]
        return acc.at[toks].add(yb * w[:, None].astype(yb.dtype)), None

    acc, _ = lax.scan(block, jnp.zeros((n + 1, d), h.dtype),
                      (row_tok.reshape(n_blocks, MOE_BLOCK), block_e, row_w.reshape(n_blocks, MOE_BLOCK)))
    return acc[:n]


def moe_ffn(h, w_router, router_bias, w_g, w_u, w_d, sw_g, sw_u, sw_d):
    idx, wts = route(h, w_router, router_bias)
    shared = (jax.nn.silu(h @ sw_g) * (h @ sw_u)) @ sw_d
    return shared + routed_experts(h, idx, wts, w_g, w_u, w_d)


def setup_inputs(seed: int = 0) -> dict:
    key = jax.random.key(seed)
    ks = iter(jax.random.split(key, 32))
    D = D_MODEL

    def nrm(shape, scale):
        return jax.random.normal(next(ks), shape, F32) * scale

    return {
        'x': nrm((BATCH, SEQ, D), 1.0),
        'c': nrm((BATCH, D), 1.0),
        'ctx': nrm((BATCH, CTX_LEN, D), 1.0),
        'c_ctx': nrm((D,), 1.0),
        'ada_w': nrm((DEPTH, D, N_MOD * D), 0.3 * D ** -0.5),
        'ada_b': nrm((DEPTH, N_MOD * D), 0.02),
        'norm1_g': 1.0 + nrm((DEPTH, D), 0.02),
        'w_in': nrm((DEPTH, D, IN_DIM), D ** -0.5),
        'conv_w': nrm((DEPTH, CONV_WIDTH, CONV_DIM), CONV_WIDTH ** -0.5),
        'na_rel_bias': nrm((DEPTH, NA_HEADS, 2 * NA_WIN_ROWS - 1, 2 * NA_WIN_COLS - 1), 0.1),
        'diff_lambda': nrm((DEPTH, 4, DIFF_QK_DIM), 0.1),
        'diff_subln_g': 1.0 + nrm((DEPTH, DIFF_V_DIM), 0.02),
        'w_branch_gate': nrm((DEPTH, N_BRANCHES, D, D), D ** -0.5),
        'w_branch': nrm((DEPTH, N_BRANCHES, BRANCH_DIM, D), BRANCH_DIM ** -0.5),
        'w_out': nrm((DEPTH, D, D), D ** -0.5),
        'norm2_g': 1.0 + nrm((DEPTH, D), 0.02),
        'router_w': nrm((DEPTH, D, N_EXPERTS), D ** -0.5),
        'router_bias': nrm((DEPTH, N_EXPERTS), 0.01),
        'expert_w_gate': nrm((DEPTH, N_EXPERTS, D, EXPERT_DIM), D ** -0.5),
        'expert_w_up': nrm((DEPTH, N_EXPERTS, D, EXPERT_DIM), D ** -0.5),
        'expert_w_down': nrm((DEPTH, N_EXPERTS, EXPERT_DIM, D), EXPERT_DIM ** -0.5),
        'shared_w_gate': nrm((DEPTH, D, SHARED_DIM), D ** -0.5),
        'shared_w_up': nrm((DEPTH, D, SHARED_DIM), D ** -0.5),
        'shared_w_down': nrm((DEPTH, SHARED_DIM, D), SHARED_DIM ** -0.5),
        'final_norm_g': 1.0 + nrm((D,), 0.02),
    }


def reference(x, c, ctx, c_ctx, ada_w, ada_b, norm1_g, w_in, conv_w, na_rel_bias, diff_lambda,
              diff_subln_g, w_branch_gate, w_branch, w_out, norm2_g, router_w, router_bias,
              expert_w_gate, expert_w_up, expert_w_down, shared_w_gate, shared_w_up, shared_w_down,
              final_norm_g):
    b, s, d = x.shape
    l_ctx = ctx.shape[1]
    rows = s // GRID_W
    t = jnp.arange(s)
    rope = axial_rope_tables(t // GRID_W, t % GRID_W)
    xc = ctx
    for layer in range(DEPTH):
        last = layer == DEPTH - 1
        lam_init = 0.8 - 0.6 * math.exp(-0.3 * layer)
        lam = diff_lambda_value(diff_lambda[layer], lam_init)
        m_lat = [u[:, None, :] for u in ada_chunks(c, ada_w[layer], ada_b[layer])]
        m_ctx = ada_chunks(c_ctx, ada_w[layer], ada_b[layer])
        w_in_l = w_in[layer]

        h = modulate(rmsnorm(x, norm1_g[layer]), m_lat[0], m_lat[1])
        hc = modulate(rmsnorm(xc, norm1_g[layer]), m_ctx[0], m_ctx[1])
        p = h @ w_in_l
        if last:
            kv_b_ctx = hc @ w_in_l[:, OFF_B_K:OFF_C_B]
            kv_d_ctx = hc @ w_in_l[:, OFF_D_K:]
        else:
            pc = hc @ w_in_l
            kv_b_ctx = pc[..., OFF_B_K:OFF_C_B]
            kv_d_ctx = pc[..., OFF_D_K:]
        br = latent_branches(p, kv_b_ctx, kv_d_ctx, rows, rope, conv_w[layer], na_rel_bias[layer],
                             lam, diff_subln_g[layer], lam_init)
        x_new = x + m_lat[2] * merge_branches(h, br, w_branch_gate[layer], w_branch[layer], w_out[layer])
        if not last:
            brc = context_branches(pc, conv_w[layer], lam, diff_subln_g[layer], lam_init)
            xc = xc + m_ctx[2] * merge_branches(hc, brc, w_branch_gate[layer], w_branch[layer], w_out[layer])
        x = x_new

        h2 = modulate(rmsnorm(x, norm2_g[layer]), m_lat[3], m_lat[4]).reshape(b * s, d)
        if last:
            tokens = h2
        else:
            hc2 = modulate(rmsnorm(xc, norm2_g[layer]), m_ctx[3], m_ctx[4]).reshape(b * l_ctx, d)
            tokens = jnp.concatenate([h2, hc2], axis=0)
        y = moe_ffn(tokens, router_w[layer], router_bias[layer], expert_w_gate[layer], expert_w_up[layer],
                    expert_w_down[layer], shared_w_gate[layer], shared_w_up[layer], shared_w_down[layer])
        x = x + m_lat[5] * y[:b * s].reshape(b, s, d)
        if not last:
            xc = xc + m_ctx[5] * y[b * s:].reshape(b, l_ctx, d)
    return rmsnorm(x, final_norm_g)
```

```python
import os
import numpy as np
from contextlib import ExitStack
import concourse.bass as bass
import concourse.mybir as mybir
from concourse.bass_utils import run_bass_kernel_spmd

F32 = mybir.dt.float32
BF16 = mybir.dt.bfloat16
AF = mybir.ActivationFunctionType
ALU = mybir.AluOpType
AX = mybir.AxisListType

ENGS = ["pe", "act", "dve", "pool", "sp"]
NDMA = 40


class Prog:
    def __init__(self, nc):
        self.nc = nc
        self.es = ExitStack()
        self.streams = {e: [] for e in ENGS}
        self.cnt = {e: 0 for e in ENGS}
        self.sem = {e: self.es.enter_context(nc.semaphore(f"sem_{e}")) for e in ENGS}
        self.dsem = [self.es.enter_context(nc.semaphore(f"dsem{i}")) for i in range(NDMA)]
        self.dcnt = [0] * NDMA
        self.dnext = 0
        self.waited = {e: {} for e in ENGS}
        self.state = {}
        self.nwaits = 0

    def sb(self, name, shape, dt):
        return self.es.enter_context(self.nc.sbuf_tensor(name, list(shape), dt))

    def ps(self, name, shape, dt=F32):
        return self.es.enter_context(self.nc.psum_tensor(name, list(shape), dt))

    def _st(self, k):
        s = self.state.get(k)
        if s is None:
            s = {"w": None, "r": {}}
            self.state[k] = s
        return s

    def _need(self, eng, tok, waits):
        if tok is None:
            return
        kind, a, v = tok
        if kind == "eng":
            if a == "pe" and eng == "pe":
                return
            key = ("e", a)
        else:
            key = ("d", a)
        if self.waited[eng].get(key, 0) >= v:
            return
        self.waited[eng][key] = v
        waits.append((self.sem[a] if kind == "eng" else self.dsem[a], v))

    def _deps(self, eng, reads, writes):
        waits = []
        for k in reads:
            self._need(eng, self._st(k)["w"], waits)
        for k in writes:
            s = self._st(k)
            self._need(eng, s["w"], waits)
            for t in s["r"].values():
                self._need(eng, t, waits)
        self.nwaits += len(waits)
        return waits

    def _commit(self, tok, reads, writes):
        for k in reads:
            s = self._st(k)
            rk = (tok[0], tok[1])
            s["r"][rk] = tok
        for k in writes:
            s = self._st(k)
            s["w"] = tok
            s["r"] = {}

    def op(self, eng, fn, reads=(), writes=()):
        pr = [k for k in reads if isinstance(k, str) and k.startswith("ps")]
        if pr:
            reads = [k for k in reads if k not in pr]
            writes = list(writes) + pr
        waits = self._deps(eng, reads, writes)
        self.cnt[eng] += 1
        tok = ("eng", eng, self.cnt[eng])
        self._commit(tok, reads, writes)
        self.streams[eng].append((waits, fn, True))

    def dma(self, eng, out, in_, reads=(), writes=(), **kw):
        s = self.dnext
        self.dnext = (self.dnext + 1) % NDMA
        waits = self._deps(eng, reads, writes)
        if self.dcnt[s] > 0:
            self._need(eng, ("dma", s, 16 * self.dcnt[s]), waits)
        self.dcnt[s] += 1
        tok = ("dma", s, 16 * self.dcnt[s])
        self._commit(tok, reads, writes)
        sem = self.dsem[s]

        def fn(e, out=out, in_=in_, kw=kw, sem=sem):
            e.dma_start(out=out, in_=in_, **kw).then_inc(sem, 16)
            return None

        self.streams[eng].append((waits, fn, False))

    def finish(self):
        waits = []
        for s in range(NDMA):
            if self.dcnt[s] > 0:
                self._need("sp", ("dma", s, 16 * self.dcnt[s]), waits)
        for e in ENGS:
            if e != "sp" and self.cnt[e] > 0:
                self._need("sp", ("eng", e, self.cnt[e]), waits)
        self.streams["sp"].append((waits, None, False))

    def emit(self):
        self.finish()
        nc = self.nc
        with nc.Block() as block:
            def mk(name):
                def body(e):
                    sem = self.sem[name]
                    for waits, fn, track in self.streams[name]:
                        for (s, v) in waits:
                            e.wait_ge(s, v)
                        if fn is None:
                            continue
                        ins = fn(e)
                        if track:
                            ins.then_inc(sem, 1)
                return body
            block.tensor(mk("pe"))
            block.scalar(mk("act"))
            block.vector(mk("dve"))
            block.gpsimd(mk("pool"))
            block.sync(mk("sp"))
        self.es.close()


def _mm(P, out, lhsT, rhs, start=True, stop=True, reads=(), writes=()):
    P.op("pe", lambda e: e.matmul(out, lhsT=lhsT, rhs=rhs, start=start, stop=stop), reads, writes)

def _tr(P, out, in_, ident, reads=(), writes=()):
    P.op("pe", lambda e: e.transpose(out, in_, ident), reads, writes)

def _act(P, out, in_, func, reads=(), writes=(), **kw):
    P.op("act", lambda e: e.activation(out=out, in_=in_, func=func, **kw), reads, writes)

def _tt(P, eng, out, in0, in1, op, reads=(), writes=()):
    P.op(eng, lambda e: e.tensor_tensor(out=out, in0=in0, in1=in1, op=op), reads, writes)

def _ts(P, eng, out, in0, s1, s2, op0, op1=None, reads=(), writes=()):
    if op1 is None:
        P.op(eng, lambda e: e.tensor_scalar(out=out, in0=in0, scalar1=s1, scalar2=None, op0=op0), reads, writes)
    else:
        P.op(eng, lambda e: e.tensor_scalar(out=out, in0=in0, scalar1=s1, scalar2=s2, op0=op0, op1=op1), reads, writes)

def _stt(P, eng, out, in0, scalar, in1, op0, op1, reads=(), writes=()):
    P.op(eng, lambda e: e.scalar_tensor_tensor(out=out, in0=in0, scalar=scalar, in1=in1, op0=op0, op1=op1), reads, writes)

def _cp(P, eng, out, in_, reads=(), writes=()):
    if eng == "act":
        P.op("act", lambda e: e.copy(out=out, in_=in_), reads, writes)
    else:
        P.op(eng, lambda e: e.tensor_copy(out=out, in_=in_), reads, writes)

def _barrier(P):
    toks = [("eng", e, P.cnt[e]) for e in ENGS if P.cnt[e] > 0]
    toks += [("dma", s, 16 * P.dcnt[s]) for s in range(NDMA) if P.dcnt[s] > 0]
    for e in ENGS:
        waits = []
        for t in toks:
            P._need(e, t, waits)
        if waits:
            P.streams[e].append((waits, None, False))
    P.state = {}


import math
import numpy as np
import ml_dtypes

BFNP = ml_dtypes.bfloat16
S = 8192
L = 256
EPS = 1e-6
NCH = 16
W_G1, W_G2, W_G3, W_G4, W_CB, W_V, W_GC = 0, 128, 256, 384, 512, 576, 704
WCOLS = 832


def consts_A():
    c = {}
    c["ident"] = np.eye(128, dtype=np.float32).astype(BFNP)
    c["ones"] = np.ones((128, 128), np.float32).astype(BFNP)
    e = np.zeros((65, 64), np.float32); e[64, :] = 1.0
    c["e64"] = e
    c["ones64"] = np.ones((64, 64), np.float32)
    n = np.arange(128)
    a = 2 * np.pi * np.outer(n, n) / 128.0
    c["dft128"] = np.stack([np.cos(a), -np.sin(a), -np.cos(a)], 1).astype(BFNP)
    k1 = np.arange(128)[:, None]; n2 = np.arange(64)[None, :]
    t = 2 * np.pi * (k1 * n2) / 8192.0
    c["tw"] = np.stack([np.cos(t), np.sin(t)], 1).astype(np.float32)
    m = np.arange(64)
    a64 = 2 * np.pi * np.outer(m, m) / 64.0
    c["dft64"] = np.stack([np.cos(a64), np.sin(a64)], 1).astype(BFNP)
    c["cs64"] = np.concatenate([np.cos(a64), np.sin(a64)], 1).astype(np.float32)
    q = np.arange(256)
    a256 = 2 * np.pi * np.outer(q, q) / 256.0
    d = np.stack([np.cos(a256), -np.sin(a256)], 1)
    c["dft256"] = d.reshape(2, 128, 2, 256).transpose(1, 0, 2, 3).astype(BFNP)
    tt_ = np.arange(S)
    half = 16
    inv = 1.0 / (10000.0 ** (np.arange(0, half, 2, dtype=np.float32) / half))
    ang_r = (tt_ // 64).astype(np.float32)[:, None] * inv
    ang_c = (tt_ % 64).astype(np.float32)[:, None] * inv
    cos32 = np.concatenate([np.cos(ang_r), np.cos(ang_r), np.cos(ang_c), np.cos(ang_c)], 1)
    sin32 = np.concatenate([-np.sin(ang_r), np.sin(ang_r), -np.sin(ang_c), np.sin(ang_c)], 1)
    c["ropec"] = np.ascontiguousarray(np.concatenate([cos32, cos32], 1).T.astype(np.float32))
    c["ropes"] = np.ascontiguousarray(np.concatenate([sin32, sin32], 1).T.astype(np.float32))
    return c


ROPE_PERM = np.concatenate([np.arange(8, 16), np.arange(0, 8), np.arange(24, 32), np.arange(16, 24)])


def prep_A(inp, layer, x_cur, xc_cur, cst):
    maps = []
    w_in = inp["w_in"][layer]
    rb = inp["na_rel_bias"][layer]
    qc = np.arange(64)[:, None]; kc = np.arange(64)[None, :]
    col_lo = np.clip(qc - 8, 0, 48)
    ok = (kc >= col_lo) & (kc < col_lo + 16)
    dc = np.clip(kc - qc + 15, 0, 30)
    for core in range(8):
        b, g = core // 4, core % 4
        m = {}
        m["xT"] = np.ascontiguousarray(x_cur[b].T)
        m["cT"] = np.ascontiguousarray(xc_cur[b].T)
        cv = np.stack([inp["c"][b], inp["c_ctx"]], 0)
        m["cvec"] = np.ascontiguousarray(cv.reshape(2, 8, 128).transpose(2, 0, 1).reshape(128, 16))
        m["adaw"] = np.ascontiguousarray(inp["ada_w"][layer][:, 0:2048])
        m["adab"] = np.ascontiguousarray(inp["ada_b"][layer][0:2048].reshape(16, 128).T)
        m["g1"] = np.ascontiguousarray(inp["norm1_g"][layer].reshape(8, 128).T)
        def cols(off, perm=None):
            w = w_in[:, off + 64 * g: off + 64 * g + 64]
            if perm is not None:
                w = w[:, np.concatenate([perm, 32 + perm])]
            return w
        OFF = dict(A=0, BQ=256, BK=512, BV=768, CB=1024, CC=1280, CX=1536, DQ=1792, DK=2048, DV=2304)
        wh = np.concatenate([cols(OFF["DQ"]), cols(OFF["BQ"]), cols(OFF["DK"]), cols(OFF["BK"]),
                             cols(OFF["DQ"], ROPE_PERM), cols(OFF["CC"]), cols(OFF["DK"], ROPE_PERM), cols(OFF["CX"]),
                             cols(OFF["CB"]), cols(OFF["DV"]), cols(OFF["BV"])], 1)
        m["wh"] = np.ascontiguousarray(wh)
        m["waT"] = np.ascontiguousarray(cols(OFF["A"]).T)
        cw = np.zeros((128, 3), np.float32)
        cw[64:128, :] = inp["conv_w"][layer][:, 64 * g: 64 * g + 64].T
        m["cw"] = cw
        tb = rb[g][:, dc]
        tb = np.where(ok[None], tb, np.float32(-30000.0)).astype(np.float32)
        tb = np.ascontiguousarray(tb.transpose(2, 0, 1))
        tpad = np.concatenate([tb, np.zeros((64, 1, 64), np.float32)], 1)
        nab = np.zeros((2, 128, 16, 64), np.float32)
        nab[0, 0:64] = tpad; nab[0, 64:128, 0:15] = tpad[:, 1:16]
        nab[1, 64:128] = tpad; nab[1, 0:64, 0:15] = tpad[:, 1:16]
        m["nab"] = np.ascontiguousarray(nab.transpose(1, 0, 2, 3).reshape(128, 2 * 16 * 64))
        m["dl"] = np.ascontiguousarray(np.broadcast_to(inp["diff_lambda"][layer].reshape(1, 128), (128, 128)))
        m["subg"] = np.ascontiguousarray(inp["diff_subln_g"][layer].reshape(64, 1))
        for k in ("ident", "ones", "e64", "ones64", "dft128", "tw", "dft64", "cs64", "dft256", "ropec", "ropes"):
            m[k] = cst[k]
        maps.append(m)
    return maps


def build_A(layer, ctx_br, stop=99):
    lam_init = 0.8 - 0.6 * math.exp(-0.3 * layer)
    nc = bass.Bass("TRN2", target_bir_lowering=False)
    P = Prog(nc)

    def din(name, shape, dt=F32):
        return nc.dram_tensor(name, list(shape), dt, kind="ExternalInput").ap()

    def dout(name, shape, dt=BF16):
        return nc.dram_tensor(name, list(shape), dt, kind="ExternalOutput").ap()

    xT = din("xT", [1024, S]); cT = din("cT", [1024, L])
    cvec_d = din("cvec", [128, 16]); adaw_d = din("adaw", [1024, 2048]); adab_d = din("adab", [128, 16])
    g1_d = din("g1", [128, 8]); wh_d = din("wh", [1024, 704]); waT_d = din("waT", [64, 1024])
    cw_d = din("cw", [128, 3]); nab_d = din("nab", [128, 2048]); dl_d = din("dl", [128, 128]); subg_d = din("subg", [64, 1])
    ident_d = din("ident", [128, 128], BF16); ones_d = din("ones", [128, 128], BF16)
    e64_d = din("e64", [65, 64]); ones64_d = din("ones64", [64, 64])
    dft128_d = din("dft128", [128, 3, 128], BF16); tw_d = din("tw", [128, 2, 64])
    dft64_d = din("dft64", [64, 2, 64], BF16); cs64_d = din("cs64", [64, 128])
    dft256_d = din("dft256", [128, 2, 2, 256], BF16)
    ropec_d = din("ropec", [64, S]); ropes_d = din("ropes", [64, S])
    ya_d = dout("ya", [64, 8192]); yb_d = dout("ybT", [64, S]); yc_d = dout("ycT", [64, S]); yd_d = dout("ydT", [64, S])
    yctx_d = dout("yctx", [4, 64, L]) if ctx_br else None

    TQ = P.sb("TQ", [128, S], BF16)
    TK = P.sb("TK", [128, S + L], BF16)
    VD = P.sb("VD", [128, 66, 65], BF16)
    VB = P.sb("VB", [128, 66, 65], BF16)
    S1 = P.sb("S1", [128, S], BF16)
    S2 = P.sb("S2", [128, S + 2], BF16)
    S3 = P.sb("S3", [128, S], BF16)
    S4 = P.sb("S4", [128, 8, 512], F32)
    S5 = P.sb("S5", [128, 2, 8, 512], BF16)
    wbf = P.sb("wbf", [128, 8, WCOLS], BF16)
    tmpf = [P.sb(f"tmpf{i}", [128, 512], F32) for i in range(6)]
    tmpb = [P.sb(f"tmpb{i}", [128, 512], BF16) for i in range(4)]
    ident = P.sb("ident_s", [128, 128], BF16); ones = P.sb("ones_s", [128, 128], BF16)
    e64 = P.sb("e64_s", [65, 64], F32); ones64 = P.sb("ones64_s", [64, 64], F32)
    dft128 = P.sb("dft128_s", [128, 3, 128], BF16); tw = P.sb("tw_s", [128, 2, 64], F32)
    dft64 = P.sb("dft64_s", [64, 2, 64], BF16); cs64 = P.sb("cs64_s", [64, 128], F32)
    dft256 = P.sb("dft256_s", [128, 2, 2, 256], BF16)
    cvec = P.sb("cvec_s", [128, 16], F32); scv = P.sb("scv", [128, 16], F32)
    adab = P.sb("adab_s", [128, 16], F32); g1 = P.sb("g1_s", [128, 8], F32)
    modv = P.sb("modv", [128, 2, 16], F32)
    Amod = P.sb("Amod", [128, 2, 8], F32)
    cw = P.sb("cw_s", [128, 3], F32); nab = P.sb("nab_s", [128, 2, 16, 64], F32)
    dl = P.sb("dl_s", [128, 128], F32); subg = P.sb("subg_s", [64, 1], F32)
    lamt = P.sb("lamt", [128, 8], F32)
    epsb = P.sb("epsb", [128, 1], F32)
    ropec = P.sb("ropec_s", [64, 512], F32); ropes = P.sb("ropes_s", [64, 512], F32)
    Osb = [P.sb(f"Osb{i}", [65, 512], F32) for i in range(2)]
    obuf = [P.sb(f"obuf{i}", [64, 512], BF16) for i in range(2)]
    VBo = P.sb("VBo", [64, 66, 65], BF16)
    TQc = P.sb("TQc", [128, L], BF16)
    Uc = P.sb("Uc", [128, L + 2], BF16); CBc = P.sb("CBc", [128, L], BF16)
    Gtok = P.sb("Gtok", [128, 2, 128], BF16)
    ps = [P.ps(f"ps{i}", [128, 512], F32) for i in range(8)]

    P.op("pool", lambda e: e.memset(epsb[:], EPS), writes=["epsb"])
    waT = S3[:].bitcast(F32)[0:64, 0:1024]
    for (dst, src, key) in [(ident, ident_d, "ident"), (ones, ones_d, "ones"), (e64, e64_d, "e64"), (ones64, ones64_d, "ones64"),
                            (dft128, dft128_d, "dft128"), (tw, tw_d, "tw"), (dft64, dft64_d, "dft64"), (cs64, cs64_d, "cs64"),
                            (dft256, dft256_d, "dft256"), (cvec, cvec_d, "cvec"), (adab, adab_d, "adab"), (g1, g1_d, "g1"),
                            (cw, cw_d, "cw"), (dl, dl_d, "dl"), (subg, subg_d, "subg")]:
        P.dma("sp", dst[:], src, writes=[key])
    P.dma("sp", waT, waT_d, writes=["waT"])
    P.dma("sp", nab[:], nab_d.rearrange("p (v d q) -> p v d q", v=2, d=16), writes=["nab"])

    whv = wh_d.rearrange("(k p) c -> p k c", p=128)
    P.dma("sp", S4[:, :, 0:512], whv[:, :, 0:512], writes=["S4"])
    _cp(P, "dve", wbf[:, :, 0:512], S4[:, :, 0:512], reads=["S4"], writes=["wbf"])
    P.dma("sp", S4[:, :, 0:192], whv[:, :, 512:704], writes=["S4"])
    _cp(P, "dve", wbf[:, :, 512:704], S4[:, :, 0:192], reads=["S4"], writes=["wbf"])
    for k in range(8):
        _mm(P, ps[k % 2][:, 0:128], lhsT=waT[:, k * 128:(k + 1) * 128], rhs=cs64[:], reads=["waT", "cs64"], writes=[f"ps{k % 2}"])
        _cp(P, "act", wbf[:, k, W_GC:W_GC + 128], ps[k % 2][:, 0:128], reads=[f"ps{k % 2}"], writes=["wbf"])

    _act(P, scv[:], cvec[:], AF.Silu, reads=["cvec"], writes=["scv"])
    adv = adaw_d.rearrange("(k p) c -> p k c", p=128)
    for q4 in range(4):
        P.dma("sp", S4[:], adv[:, :, q4 * 512:(q4 + 1) * 512], writes=["S4"])
        for jj in range(4):
            j = q4 * 4 + jj
            for k in range(8):
                _mm(P, ps[2][:, 2 * j:2 * j + 2], lhsT=S4[:, k, jj * 128:(jj + 1) * 128], rhs=scv[:, k::8],
                    start=(k == 0), stop=(k == 7), reads=["S4", "scv"], writes=["ps2"])
    for v in range(2):
        _tt(P, "dve", modv[:, v, :], ps[2][:, v:32:2], adab[:], ALU.add, reads=["ps2", "adab"], writes=["modv"])
        _stt(P, "dve", Amod[:, v, :], modv[:, v, 8:16], 1.0, g1[:], ALU.add, ALU.mult, reads=["modv", "g1"], writes=["Amod"])

    _tt(P, "dve", tmpf[0][:, 0:32], dl[:, 0:32], dl[:, 32:64], ALU.mult, reads=["dl"], writes=["tmpf0"])
    _tt(P, "dve", tmpf[0][:, 32:64], dl[:, 64:96], dl[:, 96:128], ALU.mult, reads=["dl"], writes=["tmpf0"])
    P.op("dve", lambda e: e.reduce_sum(out=lamt[:, 0:1], in_=tmpf[0][:, 0:32], axis=AX.X), reads=["tmpf0"], writes=["lamt"])
    P.op("dve", lambda e: e.reduce_sum(out=lamt[:, 1:2], in_=tmpf[0][:, 32:64], axis=AX.X), reads=["tmpf0"], writes=["lamt"])
    _act(P, lamt[:, 2:4], lamt[:, 0:2], AF.Exp, reads=["lamt"], writes=["lamt"])
    _stt(P, "dve", lamt[:, 4:5], lamt[:, 3:4], -lam_init, lamt[:, 2:3], ALU.add, ALU.subtract, reads=["lamt"], writes=["lamt"])
    _ts(P, "dve", lamt[0:64, 5:6], subg[:], 1.0 - lam_init, None, ALU.mult, reads=["subg", "lamt"], writes=["lamt"])

    _barrier(P)

    if stop == 0:
        P.emit(); return nc
    GT = S1; U = S2; CB = S3
    sq = S5[:, 0]; hh = S5[:, 1]
    P.op("pool", lambda e: e.memset(U[:, 0:1], 0.0), writes=[("U", -1)])
    P.op("pool", lambda e: e.memset(U[:, S + 1:S + 2], 0.0), writes=[("U", 99)])
    P.op("pool", lambda e: e.memset(Uc[:], 0.0), writes=["Uc"])
    P.op("pool", lambda e: e.memset(VD[:, :, 64:65], 1.0), writes=["VDones"])
    P.op("pool", lambda e: e.memset(VB[:, :, 64:65], 1.0), writes=["VBones"])

    def chunk(src_ap, n, vec, j):
        lat = vec == 0
        P.dma("sp", S4[:, :, 0:n], src_ap, writes=["x"])
        if lat:
            P.dma("sp", ropec[:], ropec_d[:, j * 512:(j + 1) * 512], writes=["ropec"])
            P.dma("sp", ropes[:], ropes_d[:, j * 512:(j + 1) * 512], writes=["ropes"])
        _act(P, sq[:, :, 0:n], S4[:, :, 0:n], AF.Square, reads=["x"], writes=["sq"])
        for k in range(8):
            _mm(P, ps[0][:, 0:n], lhsT=ones[:], rhs=sq[:, k, 0:n], start=(k == 0), stop=(k == 7), reads=["sq", "ones"], writes=["ps0"])
        _act(P, tmpf[0][:, 0:n], ps[0][:, 0:n], AF.Sqrt, scale=1.0 / 1024.0, bias=epsb[:, 0:1], reads=["ps0"], writes=["tmpf0"])
        P.op("dve", lambda e: e.reciprocal(out=tmpf[1][:, 0:n], in_=tmpf[0][:, 0:n]), reads=["tmpf0"], writes=["rstd"])
        for k in range(8):
            t = tmpf[2 + (k % 2)]
            _stt(P, "dve", t[:, 0:n], S4[:, k, 0:n], Amod[:, vec, k:k + 1], tmpf[1][:, 0:n], ALU.mult, ALU.mult,
                 reads=["x", "rstd", "Amod"], writes=[f"tmpf{2 + k % 2}"])
            _act(P, hh[:, k, 0:n], t[:, 0:n], AF.Identity, bias=modv[:, vec, k:k + 1], reads=[f"tmpf{2 + k % 2}", "modv"], writes=[("h", k)])
        hkeys = [("h", k) for k in range(8)]

        def proj(pst, pkey, c0, m, p0=0):
            for k in range(8):
                _mm(P, pst[p0:p0 + m, 0:n], lhsT=wbf[:, k, c0:c0 + m], rhs=hh[:, k, 0:n], start=(k == 0), stop=(k == 7),
                    reads=hkeys + ["wbf"], writes=[pkey])

        tq_cols = slice(j * 512, (j + 1) * 512) if lat else None
        tk_cols = slice(j * 512, (j + 1) * 512) if lat else slice(S, S + L)
        proj(ps[1], "ps1", W_G1, 128)
        proj(ps[2], "ps2", W_G2, 128)
        proj(ps[3], "ps3", W_G3, 128)
        proj(ps[4], "ps4", W_G4, 128)
        if lat:
            for (pa, pak, pb_, pbk, dst, dk_) in [(ps[1], "ps1", ps[3], "ps3", TQ, ("TQ", j, 0)), (ps[2], "ps2", ps[4], "ps4", TK, ("TK", j, 0))]:
                _tt(P, "dve", tmpf[4][0:64, :], pa[0:64, :], ropec[:], ALU.mult, reads=[pak, "ropec"], writes=["tmpf4"])
                _tt(P, "dve", tmpf[5][0:64, :], pb_[0:64, :], ropes[:], ALU.mult, reads=[pbk, "ropes"], writes=["tmpf5"])
                _tt(P, "pool", dst[0:64, tq_cols], tmpf[4][0:64, :], tmpf[5][0:64, :], ALU.add, reads=["tmpf4", "tmpf5"], writes=[dk_])
            _cp(P, "act", TQ[64:128, tq_cols], ps[1][64:128, :], reads=["ps1"], writes=[("TQ", j, 1)])
            _cp(P, "act", TK[64:128, tk_cols], ps[2][64:128, :], reads=["ps2"], writes=[("TK", j, 1)])
        else:
            _cp(P, "act", TQc[:, :], ps[1][:, 0:n], reads=["ps1"], writes=["TQc"])
            _cp(P, "act", TK[:, tk_cols], ps[2][:, 0:n], reads=["ps2"], writes=[("TK", "c")])
        _cp(P, "act", tmpf[4][64:128, 0:n], ps[3][64:128, 0:n], reads=["ps3"], writes=["tmpf4"])
        if lat:
            _tt(P, "dve", U[64:128, 1 + j * 512:1 + (j + 1) * 512], tmpf[4][64:128, :], ps[4][64:128, :], ALU.mult,
                reads=["tmpf4", "ps4"], writes=[("U", j)])
        else:
            _tt(P, "dve", Uc[64:128, 1:1 + n], tmpf[4][64:128, 0:n], ps[4][64:128, 0:n], ALU.mult, reads=["tmpf4", "ps4", "Uc"], writes=["Uc"])
        proj(ps[5], "ps5", W_CB, 64, p0=64)
        if lat:
            _cp(P, "act", CB[64:128, j * 512:(j + 1) * 512], ps[5][64:128, :], reads=["ps5"], writes=[("CB", j)])
        else:
            _cp(P, "act", CBc[64:128, 0:n], ps[5][64:128, 0:n], reads=["ps5"], writes=["CBc"])
        if lat:
            proj(ps[6], "ps6", W_GC, 128)
            _cp(P, "act", GT[:, j * 512:(j + 1) * 512], ps[6][:, :], reads=["ps6"], writes=[("GT", j)])
        ntile = n // 128
        ncol = 128 if lat else 256
        for t in range(ntile):
            for k in range(8):
                _mm(P, ps[7][:, t * 128:(t + 1) * 128] if lat else ps[7][:, t * 256:(t + 1) * 256],
                    lhsT=hh[:, k, t * 128:(t + 1) * 128], rhs=wbf[:, k, W_V:W_V + ncol],
                    start=(k == 0), stop=(k == 7), reads=hkeys + ["wbf"], writes=["ps7"])
        t0 = j * 4 if lat else 64
        if lat:
            pv = ps[7][:, :].rearrange("p (t c) -> p t c", c=128)
            _cp(P, "dve", VD[:, t0:t0 + ntile, 0:64], pv[:, :, 0:64], reads=["ps7"], writes=[("VD", j)])
            _cp(P, "act", VB[:, t0:t0 + ntile, 0:64], pv[:, :, 64:128], reads=["ps7"], writes=[("VB", j)])
        else:
            pv = ps[7][:, :].rearrange("p (t c) -> p t c", c=256)
            _cp(P, "dve", VD[:, t0:t0 + ntile, 0:64], pv[:, :, 0:64], reads=["ps7"], writes=[("VD", "c")])
            _cp(P, "act", VB[:, t0:t0 + ntile, 0:64], pv[:, :, 64:128], reads=["ps7"], writes=[("VB", "c")])
            _cp(P, "dve", Gtok[:, :, :], pv[:, :, 128:256], reads=["ps7"], writes=["Gtok"])

    xv = xT.rearrange("(k p) t -> p k t", p=128)
    cv_ = cT.rearrange("(k p) t -> p k t", p=128)
    chunk(cv_, L, 1, None)
    import os
    for j in range(int(os.environ.get('CHUNKS', NCH))):
        chunk(xv[:, :, j * 512:(j + 1) * 512], 512, 0, j)

    if stop == 1:
        P.emit(); return nc
    P.dma("sp", VBo[:, :, :], VB[64:128, :, :], reads=[("VB", j) for j in range(NCH)] + [("VB", "c"), "VBones"], writes=["VBo"])
    def conv(Ut, CBt, n_tot, out_ap_fn, step):
        for c0 in range(0, n_tot, step):
            n = min(step, n_tot - c0)
            t = tmpf[0]; o = tmpb[0]
            _ts(P, "dve", t[64:128, 0:n], Ut[64:128, c0:c0 + n], cw[64:128, 0:1], None, ALU.mult, reads=["Uall", "cw"], writes=["tmpf0"])
            _stt(P, "dve", t[64:128, 0:n], Ut[64:128, c0 + 1:c0 + 1 + n], cw[64:128, 1:2], t[64:128, 0:n], ALU.mult, ALU.add,
                 reads=["Uall", "tmpf0"], writes=["tmpf0"])
            _stt(P, "dve", t[64:128, 0:n], Ut[64:128, c0 + 2:c0 + 2 + n], cw[64:128, 2:3], t[64:128, 0:n], ALU.mult, ALU.add,
                 reads=["Uall", "tmpf0"], writes=["tmpf0"])
            _tt(P, "dve", o[64:128, 0:n], t[64:128, 0:n], CBt[64:128, c0:c0 + n], ALU.mult, reads=["tmpf0", "CBall"], writes=["tmpb0"])
            P.dma("sp", out_ap_fn(c0, n), o[64:128, 0:n], reads=["tmpb0"])

    _barrier(P)
    conv(U, CB, S, lambda c0, n: yc_d[:, c0:c0 + n], 512)
    if ctx_br:
        conv(Uc, CBc, L, lambda c0, n: yctx_d[2, :, c0:c0 + n], 256)
    _barrier(P)

    if stop == 2:
        P.emit(); return nc
    Gsb = S4[:].rearrange("p a b -> p (a b)").bitcast(BF16).rearrange("p (n c) -> p n c", c=128)
    Apr = S3[:, 0:4096].rearrange("p (n c) -> p n c", c=64)
    Api = S3[:, 4096:8192].rearrange("p (n c) -> p n c", c=64)
    Atr = S2[0:64, 0:8192].rearrange("p (c k) -> p c k", k=128)
    Ati = S5[:].rearrange("p a b c -> p (a b c)")[0:64, 0:8192].rearrange("p (c k) -> p c k", k=128)
    psb = [p_[:].bitcast(BF16) for p_ in ps]
    for i in range(8):
        pb = psb[i % 2]
        for q in range(8):
            n2 = i * 8 + q
            _tr(P, pb[:, q * 128:(q + 1) * 128], GT[:, n2::64], ident[:], reads=["ident"], writes=[f"ps{i % 2}"])
        _cp(P, "act" if i % 2 else "dve", Gsb[:, i * 8:(i + 1) * 8, :], pb[:, :].rearrange("p (n c) -> p n c", c=128),
            reads=[f"ps{i % 2}"], writes=[("G", i)])
    for gI in range(8):
        gc = Gsb[:, gI * 8:(gI + 1) * 8, 0:64]; gs = Gsb[:, gI * 8:(gI + 1) * 8, 64:128]
        par, pai = ps[2 + (gI % 2) * 2], ps[3 + (gI % 2) * 2]
        kr, ki = f"ps{2 + (gI % 2) * 2}", f"ps{3 + (gI % 2) * 2}"
        _mm(P, par[:, :], lhsT=dft128[:, 0, :], rhs=gc, start=True, stop=False, reads=[("G", gI)], writes=[kr])
        _mm(P, par[:, :], lhsT=dft128[:, 1, :], rhs=gs, start=False, stop=True, reads=[("G", gI)], writes=[kr])
        _mm(P, pai[:, :], lhsT=dft128[:, 2, :], rhs=gs, start=True, stop=False, reads=[("G", gI)], writes=[ki])
        _mm(P, pai[:, :], lhsT=dft128[:, 1, :], rhs=gc, start=False, stop=True, reads=[("G", gI)], writes=[ki])
        tcb = tw[:, 0, gI * 8:(gI + 1) * 8].unsqueeze(2).broadcast_to([128, 8, 64])
        tsb = tw[:, 1, gI * 8:(gI + 1) * 8].unsqueeze(2).broadcast_to([128, 8, 64])
        v3 = lambda a: a[:, :].rearrange("p (n c) -> p n c", c=64)
        _tt(P, "dve", v3(tmpf[0]), v3(par), tcb, ALU.mult, reads=[kr], writes=["tmpf0"])
        _tt(P, "dve", v3(tmpf[1]), v3(pai), tsb, ALU.mult, reads=[ki], writes=["tmpf1"])
        _tt(P, "pool", Apr[:, gI * 8:(gI + 1) * 8, :], v3(tmpf[0]), v3(tmpf[1]), ALU.add, reads=["tmpf0", "tmpf1"], writes=[("Apr", gI)])
        _tt(P, "dve", v3(tmpf[2]), v3(pai), tcb, ALU.mult, reads=[ki], writes=["tmpf2"])
        _tt(P, "dve", v3(tmpf[3]), v3(par), tsb, ALU.mult, reads=[kr], writes=["tmpf3"])
        _tt(P, "pool", Api[:, gI * 8:(gI + 1) * 8, :], v3(tmpf[2]), v3(tmpf[3]), ALU.subtract, reads=["tmpf2", "tmpf3"], writes=[("Api", gI)])
    apr_all = [("Apr", i) for i in range(8)]; api_all = [("Api", i) for i in range(8)]
    for (src, dst, rk, nm) in [(Apr, Atr, apr_all, "Atr"), (Api, Ati, api_all, "Ati")]:
        for i in range(8):
            pb = psb[6 + (i % 2)]
            for q in range(8):
                c = i * 8 + q
                _tr(P, pb[0:64, q * 128:(q + 1) * 128], src[:, :, c], ident[:], reads=rk + ["ident"], writes=[f"ps{6 + i % 2}"])
            _cp(P, "act" if i % 2 else "dve", dst[:, i * 8:(i + 1) * 8, :], pb[0:64, :].rearrange("p (c k) -> p c k", k=128),
                reads=[f"ps{6 + i % 2}"], writes=[(nm, i)])
    nrm = 1.0 / math.sqrt(8192.0 * 64.0)
    for i in range(16):
        pp = ps[i % 2]; pk = f"ps{i % 2}"
        _mm(P, pp[0:64, :], lhsT=dft64[:, 0, :], rhs=Atr[:, i * 4:(i + 1) * 4, :], start=True, stop=False, reads=[("Atr", i // 2)], writes=[pk])
        _mm(P, pp[0:64, :], lhsT=dft64[:, 1, :], rhs=Ati[:, i * 4:(i + 1) * 4, :], start=False, stop=True, reads=[("Ati", i // 2)], writes=[pk])
        ob = tmpb[i % 2]
        _act(P, ob[0:64, :], pp[0:64, :], AF.Copy, scale=nrm, reads=[pk], writes=[f"tmpb{i % 2}"])
        P.dma("sp", ya_d[:, i * 512:(i + 1) * 512], ob[0:64, :], reads=[f"tmpb{i % 2}"])
    if ctx_br:
        for t in range(2):
            _mm(P, ps[2][0:64, 0:256], lhsT=Gtok[:, t, 0:64], rhs=dft256[:, t, 0, :], start=(t == 0), stop=False, reads=["Gtok", "dft256"], writes=["ps2"])
            _mm(P, ps[2][0:64, 0:256], lhsT=Gtok[:, t, 64:128], rhs=dft256[:, t, 1, :], start=False, stop=(t == 1), reads=["Gtok", "dft256"], writes=["ps2"])
        _act(P, tmpb[2][0:64, 0:256], ps[2][0:64, 0:256], AF.Copy, scale=1.0 / math.sqrt(256.0 * 64.0), reads=["ps2"], writes=["tmpb2"])
        P.dma("sp", yctx_d[0, :, :], tmpb[2][0:64, 0:256], reads=["tmpb2"])
    _barrier(P)

    if stop == 3:
        P.emit(); return nc
    def lbcast_recip(Ot, okey, n, dst, dkey, pp, pk):
        _mm(P, pp[0:64, 0:n], lhsT=e64[:], rhs=Ot[:, 0:n], reads=[okey, "e64"], writes=[pk])
        P.op("dve", lambda e: e.reciprocal(out=dst[0:64, 0:n], in_=pp[0:64, 0:n]), reads=[pk], writes=[dkey])

    def dense_attn(q_ap, k_tile_fn, v_tile_fn, nkt, n, scale, Ot, okey, pO, pOk, ps_s):
        for kt in range(nkt):
            pp, pk = ps_s[kt % len(ps_s)]
            pt = tmpb[kt % 4]; ptk = f"tmpb{kt % 4}"
            kap, kk = k_tile_fn(kt)
            _mm(P, pp[:, 0:n], lhsT=kap, rhs=q_ap[0], reads=list(kk) + list(q_ap[1]), writes=[pk])
            _act(P, pt[:, 0:n], pp[:, 0:n], AF.Exp, scale=scale, reads=[pk], writes=[ptk])
            vap, vk = v_tile_fn(kt)
            _mm(P, pO[0:65, 0:n], lhsT=vap, rhs=pt[:, 0:n], start=(kt == 0), stop=(kt == nkt - 1), reads=list(vk) + [ptk], writes=[pOk])
        _cp(P, "dve", Ot[:, 0:n], pO[0:65, 0:n], reads=[pOk], writes=[okey])

    def diff_combine(n, out_dram):
        lbcast_recip(Osb[0], "Osb0", n, tmpf[0], "tmpf0", ps[6], "ps6")
        lbcast_recip(Osb[1], "Osb1", n, tmpf[1], "tmpf1", ps[7], "ps7")
        _tt(P, "dve", tmpf[2][0:64, 0:n], Osb[0][0:64, 0:n], tmpf[0][0:64, 0:n], ALU.mult, reads=["Osb0", "tmpf0"], writes=["tmpf2"])
        _tt(P, "pool", tmpf[3][0:64, 0:n], Osb[1][0:64, 0:n], tmpf[1][0:64, 0:n], ALU.mult, reads=["Osb1", "tmpf1"], writes=["tmpf3"])
        _stt(P, "dve", tmpf[2][0:64, 0:n], tmpf[3][0:64, 0:n], lamt[0:64, 4:5], tmpf[2][0:64, 0:n], ALU.mult, ALU.add,
             reads=["tmpf3", "tmpf2", "lamt"], writes=["tmpf2"])
        _act(P, tmpf[4][0:64, 0:n], tmpf[2][0:64, 0:n], AF.Square, reads=["tmpf2"], writes=["tmpf4"])
        _mm(P, ps[6][0:64, 0:n], lhsT=ones64[:], rhs=tmpf[4][0:64, 0:n], reads=["tmpf4", "ones64"], writes=["ps6"])
        _act(P, tmpf[4][0:64, 0:n], ps[6][0:64, 0:n], AF.Sqrt, scale=1.0 / 64.0, bias=epsb[0:64, 0:1], reads=["ps6"], writes=["tmpf4"])
        P.op("dve", lambda e: e.reciprocal(out=tmpf[5][0:64, 0:n], in_=tmpf[4][0:64, 0:n]), reads=["tmpf4"], writes=["tmpf5"])
        _stt(P, "dve", tmpb[0][0:64, 0:n], tmpf[2][0:64, 0:n], lamt[0:64, 5:6], tmpf[5][0:64, 0:n], ALU.mult, ALU.mult,
             reads=["tmpf2", "tmpf5", "lamt"], writes=["tmpb0"])
        P.dma("sp", out_dram, tmpb[0][0:64, 0:n], reads=["tmpb0"])

    tq_all = [("TQ", j, h) for j in range(NCH) for h in range(2)]
    tk_all = [("TK", j, h) for j in range(NCH) for h in range(2)] + [("TK", "c")]
    v_all = [("VD", j) for j in range(NCH)] + [("VB", j) for j in range(NCH)] + [("VD", "c"), ("VB", "c"), "VDones", "VBones"]

    sc_b = 64 ** -0.5
    for r8 in range(int(os.environ.get('NA_R8', 16))):
        pO = ps[4 + (r8 % 2)]; pOk = f"ps{4 + r8 % 2}"
        for rr in range(8):
            r = r8 * 8 + rr
            w0 = min(max(r - 4, 0), 120)
            dr0 = w0 - r + 7
            pw = ps[rr % 2]; pwk = f"ps{rr % 2}"
            pc = ps[2 + (rr % 2)]; pck = f"ps{2 + rr % 2}"
            qap = TQ[64:128, r * 64:(r + 1) * 64]
            for jw in range(8):
                w = w0 + jw
                _mm(P, pw[0:64, jw * 64:(jw + 1) * 64], lhsT=TK[64:128, w * 64:(w + 1) * 64], rhs=qap, writes=[pwk])
            for t in range(2):
                _mm(P, pc[:, t * 64:(t + 1) * 64], lhsT=TK[64:128, S + t * 128:S + (t + 1) * 128], rhs=qap, writes=[pck])
            bt = nab[0:64, 0, dr0:dr0 + 8, :]
            tf = tmpf[rr % 2]; tfk = f"tmpf{rr % 2}"
            _stt(P, "dve", tf[0:64, :].rearrange("p (a b) -> p a b", b=64), pw[0:64, :].rearrange("p (a b) -> p a b", b=64),
                 sc_b, bt, ALU.mult, ALU.add, reads=[pwk], writes=[tfk])
            pt = tmpb[rr % 2]; ptk = f"tmpb{rr % 2}"
            ptc = tmpb[2 + rr % 2]; ptck = f"tmpb{2 + rr % 2}"
            _act(P, pt[0:64, :], tf[0:64, :], AF.Exp, reads=[tfk], writes=[ptk])
            _act(P, ptc[:, 0:128], pc[:, 0:128], AF.Exp, scale=sc_b, reads=[pck], writes=[ptck])
            for jw in range(8):
                w = w0 + jw
                vsrc = VB if w % 2 == 0 else VBo
                _mm(P, pO[0:65, rr * 64:(rr + 1) * 64], lhsT=vsrc[0:64, w // 2, :],
                    rhs=pt[0:64, jw * 64:(jw + 1) * 64], start=(jw == 0), stop=False, reads=[ptk], writes=[pOk])
            for t in range(2):
                _mm(P, pO[0:65, rr * 64:(rr + 1) * 64], lhsT=VB[:, 64 + t, :], rhs=ptc[:, t * 64:(t + 1) * 64],
                    start=False, stop=(t == 1), reads=[ptck], writes=[pOk])
        Ot = Osb[r8 % 2]; okey = f"Osb{r8 % 2}"
        _cp(P, "dve", Ot[:, :], pO[0:65, :], reads=[pOk], writes=[okey])
        if os.environ.get('NA_EPI', '1') == '0':
            continue
        lbcast_recip(Ot, okey, 512, tmpf[2], "tmpf2", ps[6 + r8 % 2], f"ps{6 + r8 % 2}")
        ob = obuf[r8 % 2]; obk = f"obuf{r8 % 2}"
        _tt(P, "pool", ob[0:64, :], Ot[0:64, :], tmpf[2][0:64, :], ALU.mult, reads=[okey, "tmpf2"], writes=[obk])
        P.dma("sp", yb_d[:, r8 * 512:(r8 + 1) * 512], ob[0:64, :], reads=[obk])
    if ctx_br and os.environ.get('NA_CTX', '1') == '1':
        dense_attn((TQc[64:128, :], []), lambda kt: (TK[64:128, S + kt * 128:S + (kt + 1) * 128], []),
                   lambda kt: (VB[:, 64 + kt, :], []), 2, L, sc_b, Osb[0], "Osb0", ps[4], "ps4", [(ps[0], "ps0"), (ps[1], "ps1")])
        lbcast_recip(Osb[0], "Osb0", L, tmpf[2], "tmpf2", ps[6], "ps6")
        _tt(P, "pool", tmpb[2][0:64, 0:L], Osb[0][0:64, 0:L], tmpf[2][0:64, 0:L], ALU.mult, reads=["Osb0", "tmpf2"], writes=["tmpb2"])
        P.dma("sp", yctx_d[1, :, :], tmpb[2][0:64, 0:L], reads=["tmpb2"])
    _barrier(P)

    if stop == 4:
        P.emit(); return nc
    sc_d = 32 ** -0.5
    for qg in range(16):
        for mI in range(2):
            rows = slice(mI * 32, mI * 32 + 32)
            dense_attn((TQ[rows, qg * 512:(qg + 1) * 512], []),
                       lambda kt, rows=rows: (TK[rows, kt * 128:(kt + 1) * 128], []),
                       lambda kt: (VD[:, kt, :], []), 66, 512, sc_d, Osb[mI], f"Osb{mI}", ps[4 + mI], f"ps{4 + mI}",
                       [(ps[0], "ps0"), (ps[1], "ps1"), (ps[2], "ps2"), (ps[3], "ps3")])
        diff_combine(512, yd_d[:, qg * 512:(qg + 1) * 512])
    if ctx_br:
        for mI in range(2):
            rows = slice(mI * 32, mI * 32 + 32)
            dense_attn((TQc[rows, :], []), lambda kt, rows=rows: (TK[rows, S + kt * 128:S + (kt + 1) * 128], []),
                       lambda kt: (VD[:, 64 + kt, :], []), 2, L, sc_d, Osb[mI], f"Osb{mI}", ps[4 + mI], f"ps{4 + mI}",
                       [(ps[0], "ps0"), (ps[1], "ps1")])
        diff_combine(L, yctx_d[3, :, :])
    P.emit()
    return nc


import math
import numpy as np
import ml_dtypes

BFNP = ml_dtypes.bfloat16
EPS = 1e-6
NE = 256


def prep_B(inp, layer, x_cur, xc_cur, brT, brcT, last):
    maps = []
    for core in range(8):
        b, q = core // 4, core % 4
        m = {}
        xs = x_cur[b, q * 2048:(q + 1) * 2048].T
        bs = brT[b][:, q * 2048:(q + 1) * 2048]
        if not last:
            xs = np.concatenate([xs, xc_cur[b, q * 64:(q + 1) * 64].T], 1)
            bs = np.concatenate([bs, brcT[b][:, q * 64:(q + 1) * 64]], 1)
        m["xT"] = np.ascontiguousarray(xs, dtype=np.float32)
        m["brT"] = np.ascontiguousarray(bs)
        cv = np.stack([inp["c"][b], inp["c_ctx"]], 0)
        m["cvec"] = np.ascontiguousarray(cv.reshape(2, 8, 128).transpose(2, 0, 1).reshape(128, 16))
        m["adaw"] = inp["ada_w"][layer]
        m["adab"] = np.ascontiguousarray(inp["ada_b"][layer].reshape(48, 128).T)
        m["g1"] = np.ascontiguousarray(inp["norm1_g"][layer].reshape(8, 128).T)
        m["g2"] = np.ascontiguousarray(inp["norm2_g"][layer].reshape(8, 128).T)
        m["gf"] = np.ascontiguousarray(inp["final_norm_g"].reshape(8, 128).T)
        m["wgate"] = inp["w_branch_gate"][layer]
        m["wbr"] = inp["w_branch"][layer]
        m["wout"] = inp["w_out"][layer]
        m["wrt"] = inp["router_w"][layer]
        m["rbias"] = np.ascontiguousarray(np.broadcast_to(inp["router_bias"][layer][None, :], (128, NE)))
        m["ident"] = np.eye(128, dtype=np.float32).astype(BFNP)
        m["identf"] = np.eye(128, dtype=np.float32)
        m["ones"] = np.ones((128, 128), np.float32).astype(BFNP)
        maps.append(m)
    return maps


def build_B1(layer, last):
    import os
    T = 2048 if last else 2112
    NT = T // 128 if last else 17
    chunks = [(i * 512, 512, 0) for i in range(4)] + ([] if last else [(2048, 64, 1)])
    nc = bass.Bass("TRN2", target_bir_lowering=False)
    P = Prog(nc)

    def din(name, shape, dt=F32):
        return nc.dram_tensor(name, list(shape), dt, kind="ExternalInput").ap()

    xT = din("xT", [1024, T]); brT = din("brT", [1024, T], BF16)
    cvec_d = din("cvec", [128, 16]); adaw_d = din("adaw", [1024, 6144]); adab_d = din("adab", [128, 48])
    g1_d = din("g1", [128, 8]); g2_d = din("g2", [128, 8]); gf_d = din("gf", [128, 8])
    wgate_d = din("wgate", [4, 1024, 1024]); wbr_d = din("wbr", [4, 256, 1024]); wout_d = din("wout", [1024, 1024])
    wrt_d = din("wrt", [1024, NE]); rbias_d = din("rbias", [128, NE])
    ident_d = din("ident", [128, 128], BF16); identf_d = din("identf", [128, 128]); ones_d = din("ones", [128, 128], BF16)
    xmid_d = nc.dram_tensor("xmidT", [1024, T], F32, kind="ExternalOutput").ap()
    h2o_d = nc.dram_tensor("h2T", [1024, T], BF16, kind="ExternalOutput").ap()
    wro_d = nc.dram_tensor("wr", [NT * 128, NE], F32, kind="ExternalOutput").ap()
    modo_d = nc.dram_tensor("modvo", [128, 96], F32, kind="ExternalOutput").ap()

    H2 = P.sb("H2", [128, 8, T], BF16)
    Wr = P.sb("Wr", [128, NT, 260], F32)
    ident = P.sb("ident_s", [128, 128], BF16); identf = P.sb("identf_s", [128, 128], F32); ones = P.sb("ones_s", [128, 128], BF16)
    cvec = P.sb("cvec_s", [128, 16], F32); scv = P.sb("scv", [128, 16], F32)
    adab = P.sb("adab_s", [128, 48], F32)
    g1 = P.sb("g1_s", [128, 8], F32); g2 = P.sb("g2_s", [128, 8], F32); gf = P.sb("gf_s", [128, 8], F32)
    modv = P.sb("modv", [128, 2, 48], F32)
    Am1 = P.sb("Am1", [128, 2, 8], F32); Am2 = P.sb("Am2", [128, 2, 8], F32)
    epsb = P.sb("epsb", [128, 1], F32); zerob = P.sb("zerob", [128, 1], F32)
    rbias = P.sb("rbias_s", [128, NE], F32)
    wrt = P.sb("wrt_s", [128, 8, NE], F32)
    tmpf = [P.sb(f"tmpf{i}", [128, 512], F32) for i in range(5)]
    tmpb = [P.sb(f"tmpb{i}", [128, 512], BF16) for i in range(4)]
    rt = P.sb("rt", [128, 8, 8], F32); rs = P.sb("rs", [128, 64], F32)
    AR = P.sb("AR", [128, 56 * 1024], BF16)
    ps = [P.ps(f"ps{i}", [128, 512], F32) for i in range(8)]

    def carve(off_kb, nbytes, dt):
        a = AR[:, off_kb * 512: off_kb * 512 + nbytes // 2]
        return a if dt == BF16 else a.bitcast(F32)

    xc_ = carve(0, 16384, F32).rearrange("p (k t) -> p k t", k=8)
    sq = carve(16, 8192, BF16).rearrange("p (k t) -> p k t", k=8)
    hh = carve(24, 8192, BF16).rearrange("p (k t) -> p k t", k=8)
    brc = carve(32, 8192, BF16).rearrange("p (k t) -> p k t", k=8)
    macc = carve(40, 16384, F32).rearrange("p (k t) -> p k t", k=8)
    mrg = carve(56, 8192, BF16).rearrange("p (k t) -> p k t", k=8)
    wg = carve(64, 16384, BF16).rearrange("p (k c) -> p k c", k=8)
    wb = carve(80, 16384, BF16).rearrange("p (k c) -> p k c", k=8)
    stg = carve(96, 16384, F32).rearrange("p (k c) -> p k c", k=8)
    yacc = carve(0, NT * 4096, F32).rearrange("p (t f) -> p t f", t=NT)
    wG = carve(72, 8192, BF16).rearrange("p (k c) -> p k c", k=8)
    wU = carve(80, 8192, BF16).rearrange("p (k c) -> p k c", k=8)
    wD = carve(88, 8192, BF16).rearrange("p (c f) -> p c f", c=4)
    sgb = carve(96, 1024, BF16); hmb = carve(97, 1024, BF16)
    hmT = [carve(98 + i, 1024, BF16) for i in range(2)]
    xo = carve(72, 16384, F32).rearrange("p (k t) -> p k t", k=8)
    fo = carve(88, 16384, F32).rearrange("p (k t) -> p k t", k=8)
    sq2 = carve(104, 8192, BF16).rearrange("p (k t) -> p k t", k=8)

    for (dst, src, key) in [(ident, ident_d, "ident"), (identf, identf_d, "identf"), (ones, ones_d, "ones"), (cvec, cvec_d, "cvec"),
                            (adab, adab_d, "adab"), (g1, g1_d, "g1"), (g2, g2_d, "g2"), (gf, gf_d, "gf"), (rbias, rbias_d, "rbias")]:
        P.dma("sp", dst[:], src, writes=[key])
    P.dma("sp", wrt[:], wrt_d.rearrange("(k p) e -> p k e", p=128), writes=["wrt"])
    P.op("pool", lambda e: e.memset(epsb[:], EPS), writes=["epsb"])
    P.op("pool", lambda e: e.memset(zerob[:], 0.0), writes=["zerob"])
    P.op("pool", lambda e: e.memset(Wr[:], 0.0), writes=[("Wr", t_) for t_ in range(NT)])
    _act(P, scv[:], cvec[:], AF.Silu, reads=["cvec"], writes=["scv"])
    adv = adaw_d.rearrange("(k p) c -> p k c", p=128)
    for q12 in range(12):
        P.dma("sp", stg[:], adv[:, :, q12 * 512:(q12 + 1) * 512], writes=["stg"])
        for jj in range(4):
            j = q12 * 4 + jj
            for k in range(8):
                _mm(P, ps[2][:, 2 * j:2 * j + 2], lhsT=stg[:, k, jj * 128:(jj + 1) * 128], rhs=scv[:, k::8],
                    start=(k == 0), stop=(k == 7), reads=["stg", "scv"], writes=["ps2"])
    for v in range(2):
        _tt(P, "dve", modv[:, v, :], ps[2][:, v:96:2], adab[:], ALU.add, reads=["ps2", "adab"], writes=["modv"])
        _stt(P, "dve", Am1[:, v, :], modv[:, v, 8:16], 1.0, g1[:], ALU.add, ALU.mult, reads=["modv", "g1"], writes=["Am1"])
        _stt(P, "dve", Am2[:, v, :], modv[:, v, 32:40], 1.0, g2[:], ALU.add, ALU.mult, reads=["modv", "g2"], writes=["Am2"])
    for i in range(4):
        P.dma("pool", wb[:, 2 * i:2 * i + 2, :], wbr_d[i].rearrange("(h p) f -> p h f", p=128), writes=["wb"])
    _barrier(P)

    def norm_chunk(src, n, A_ap, B_ap, dst_bf=None, dst_f32=None, sqb=None, fkey="macc"):
        sqb = sq if sqb is None else sqb
        _act(P, sqb[:, :, 0:n], src[:, :, 0:n], AF.Square, reads=["x"], writes=["sq"])
        for k in range(8):
            _mm(P, ps[0][:, 0:n], lhsT=ones[:], rhs=sqb[:, k, 0:n], start=(k == 0), stop=(k == 7), reads=["sq", "ones"], writes=["ps0"])
        _act(P, tmpf[0][:, 0:n], ps[0][:, 0:n], AF.Sqrt, scale=1.0 / 1024.0, bias=epsb[:, 0:1], reads=["ps0"], writes=["tmpf0"])
        P.op("dve", lambda e: e.reciprocal(out=tmpf[1][:, 0:n], in_=tmpf[0][:, 0:n]), reads=["tmpf0"], writes=["rstd"])
        for k in range(8):
            t = tmpf[2 + (k % 2)]; tk = f"tmpf{2 + k % 2}"
            _stt(P, "dve", t[:, 0:n], src[:, k, 0:n], A_ap(k), tmpf[1][:, 0:n], ALU.mult, ALU.mult, reads=["x", "rstd"], writes=[tk])
            if dst_f32 is not None:
                _act(P, dst_f32[:, k, 0:n], t[:, 0:n], AF.Identity, bias=B_ap(k), reads=[tk], writes=[(fkey, k)])
                if dst_bf is not None:
                    _cp(P, "pool", dst_bf(k), dst_f32[:, k, 0:n], reads=[(fkey, k)], writes=[("h", k)])
            else:
                _act(P, dst_bf(k), t[:, 0:n], AF.Identity, bias=B_ap(k), reads=[tk], writes=[("h", k)])

    xv = xT.rearrange("(k p) t -> p k t", p=128)
    bv = brT.rearrange("(k p) t -> p k t", p=128)
    xmv = xmid_d.rearrange("(k p) t -> p k t", p=128)
    wov = wout_d.rearrange("(k p) f -> p k f", p=128)
    hkeys = [("h", k) for k in range(8)]
    for (c0, n, vec) in chunks:
        P.dma("sp", xc_[:, :, 0:n], xv[:, :, c0:c0 + n], writes=["x"])
        P.dma("sp", brc[:, :, 0:n], bv[:, :, c0:c0 + n], writes=["br"])
        norm_chunk(xc_, n, lambda k: Am1[:, vec, k:k + 1], lambda k: modv[:, vec, k:k + 1], lambda k: hh[:, k, 0:n])
        for i in range(4):
            P.dma("pool", wg[:], wgate_d[i].rearrange("(k p) f -> p k f", p=128), reads=[], writes=["wg"])
            for oc in range(8):
                pg = ps[1 + (oc % 2)]; pgk = f"ps{1 + oc % 2}"
                pp = ps[3 + (oc % 2)]; ppk = f"ps{3 + oc % 2}"
                for k in range(8):
                    _mm(P, pg[:, 0:n], lhsT=wg[:, k, oc * 128:(oc + 1) * 128], rhs=hh[:, k, 0:n], start=(k == 0), stop=(k == 7),
                        reads=hkeys + ["wg"], writes=[pgk])
                gt = tmpb[oc % 2]; gk = f"tmpb{oc % 2}"
                _act(P, gt[:, 0:n], pg[:, 0:n], AF.Sigmoid, reads=[pgk], writes=[gk])
                for h in range(2):
                    _mm(P, pp[:, 0:n], lhsT=wb[:, 2 * i + h, oc * 128:(oc + 1) * 128], rhs=brc[:, 2 * i + h, 0:n], start=(h == 0), stop=(h == 1),
                        reads=["br", "wb"], writes=[ppk])
                if i == 0:
                    _tt(P, "dve", macc[:, oc, 0:n], pp[:, 0:n], gt[:, 0:n], ALU.mult, reads=[ppk, gk], writes=[("macc", oc)])
                else:
                    t = tmpf[4]
                    _tt(P, "dve", t[:, 0:n], pp[:, 0:n], gt[:, 0:n], ALU.mult, reads=[ppk, gk], writes=["tmpf4"])
                    _tt(P, "pool", macc[:, oc, 0:n], macc[:, oc, 0:n], t[:, 0:n], ALU.add, reads=["tmpf4"], writes=[("macc", oc)])
        for oc in range(8):
            _cp(P, "act", mrg[:, oc, 0:n], macc[:, oc, 0:n], reads=[("macc", oc)], writes=[("mrg", oc)])
        P.dma("pool", wg[:], wov, writes=["wg"])
        mkeys = [("mrg", k) for k in range(8)]
        for oc in range(8):
            po = ps[5 + (oc % 2)]; pok = f"ps{5 + oc % 2}"
            for k in range(8):
                _mm(P, po[:, 0:n], lhsT=wg[:, k, oc * 128:(oc + 1) * 128], rhs=mrg[:, k, 0:n], start=(k == 0), stop=(k == 7),
                    reads=mkeys + ["wg"], writes=[pok])
            _stt(P, "dve", xc_[:, oc, 0:n], po[:, 0:n], modv[:, vec, 16 + oc:17 + oc], xc_[:, oc, 0:n], ALU.mult, ALU.add,
                 reads=[pok, "x"], writes=["x"])
        P.dma("sp", xmv[:, :, c0:c0 + n], xc_[:, :, 0:n], reads=["x"])
        norm_chunk(xc_, n, lambda k: Am2[:, vec, k:k + 1], lambda k: modv[:, vec, 24 + k:25 + k],
                   lambda k: H2[:, k, c0:c0 + n], dst_f32=macc)
        for tt_ in range((n + 127) // 128):
            tn = min(128, n - tt_ * 128)
            ti = c0 // 128 + tt_
            pr = ps[7]
            for k in range(8):
                _mm(P, pr[0:tn, 0:NE], lhsT=macc[:, k, tt_ * 128:tt_ * 128 + tn], rhs=wrt[:, k, :], start=(k == 0), stop=(k == 7),
                    reads=[("macc", kk) for kk in range(8)] + ["wrt"], writes=["ps7"])
            sc = tmpf[0][:, 0:NE]; sbv = tmpf[1][:, 0:NE]; ch = tmpf[2][:, 0:NE]
            _act(P, sc[0:tn], pr[0:tn, 0:NE], AF.Sigmoid, reads=["ps7"], writes=["tmpf0"])
            _tt(P, "dve", sbv[0:tn], sc[0:tn], rbias[0:tn, :], ALU.add, reads=["tmpf0", "rbias"], writes=["rstd"])
            for g in range(8):
                P.op("dve", lambda e, g=g, tn=tn, sbv=sbv: e.max(out=rt[0:tn, g, :], in_=sbv[0:tn, g * 32:(g + 1) * 32]), reads=["rstd"], writes=["rt"])
            _tt(P, "dve", rs[0:tn, 0:8], rt[0:tn, :, 0], rt[0:tn, :, 1], ALU.add, reads=["rt"], writes=["rs"])
            P.op("dve", lambda e, tn=tn: e.max(out=rs[0:tn, 8:16], in_=rs[0:tn, 0:8]), reads=["rs"], writes=["rs"])
            _ts(P, "dve", rs[0:tn, 16:24], rs[0:tn, 0:8], rs[0:tn, 11:12], None, ALU.is_ge, reads=["rs"], writes=["rs"])
            _ts(P, "dve", rs[0:tn, 24:32], rs[0:tn, 16:24], 1.0, 1.0e9, ALU.subtract, ALU.mult, reads=["rs"], writes=["rs"])
            gm = rs[0:tn, 16:24].unsqueeze(2).broadcast_to([tn, 8, 32])
            pen = rs[0:tn, 24:32].unsqueeze(2).broadcast_to([tn, 8, 32])
            v3 = lambda a: a.rearrange("p (g e) -> p g e", e=32)
            _tt(P, "dve", v3(ch[0:tn]), v3(sbv[0:tn]), gm, ALU.mult, reads=["rstd", "rs"], writes=["tmpf2"])
            _tt(P, "dve", v3(ch[0:tn]), v3(ch[0:tn]), pen, ALU.add, reads=["tmpf2", "rs"], writes=["tmpf2"])
            P.op("dve", lambda e, tn=tn, ch=ch: e.max(out=rs[0:tn, 32:40], in_=ch[0:tn]), reads=["tmpf2"], writes=["rs"])
            _ts(P, "dve", ch[0:tn], ch[0:tn], rs[0:tn, 39:40], None, ALU.is_ge, reads=["tmpf2", "rs"], writes=["tmpf2"])
            _tt(P, "dve", sc[0:tn], sc[0:tn], ch[0:tn], ALU.mult, reads=["tmpf0", "tmpf2"], writes=["tmpf0"])
            P.op("dve", lambda e, tn=tn, sc=sc: e.reduce_sum(out=rs[0:tn, 40:41], in_=sc[0:tn], axis=AX.X), reads=["tmpf0"], writes=["rs"])
            P.op("dve", lambda e, tn=tn: e.reciprocal(out=rs[0:tn, 41:42], in_=rs[0:tn, 40:41]), reads=["rs"], writes=["rs"])
            _ts(P, "dve", Wr[0:tn, ti, 0:NE], sc[0:tn], rs[0:tn, 41:42], 2.5, ALU.mult, ALU.mult, reads=["tmpf0", "rs"], writes=[("Wr", ti)])
    P.dma("sp", h2o_d.rearrange("(k p) t -> p k t", p=128), H2[:], reads=[("h", k) for k in range(8)])
    P.dma("sp", wro_d.rearrange("(t p) e -> p t e", p=128), Wr[:, :, 0:NE], reads=[("Wr", t_) for t_ in range(NT)])
    P.dma("sp", modo_d, modv[:].rearrange("p v j -> p (v j)"), reads=["modv"])
    P.emit()
    return nc


def build_B2(last, NG=8):
    NT = 16 if last else 17
    TG = NT * 128
    TA = NG * TG
    nc = bass.Bass("TRN2", target_bir_lowering=False)
    P = Prog(nc)

    def din(name, shape, dt=F32):
        return nc.dram_tensor(name, list(shape), dt, kind="ExternalInput").ap()

    h2_d = din("h2a", [1024, TA], BF16); wr_d = din("wra", [TA, 33])
    ewg_d = din("ewg", [33, 1024, 256]); ewu_d = din("ewu", [33, 1024, 256]); ewd_d = din("ewd", [33, 256, 1024])
    ident_d = din("ident", [128, 128], BF16)
    yp_d = nc.dram_tensor("ypart", [TA, 1024], F32, kind="ExternalOutput").ap()
    H2 = P.sb("H2", [128, 8, TG], BF16)
    Wr = P.sb("Wr", [128, NT, 33], F32)
    ident = P.sb("ident_s", [128, 128], BF16)
    yacc = P.sb("yacc", [128, NT, 1024], F32)
    wG = P.sb("wG", [128, 8, 512], BF16); wU = P.sb("wU", [128, 8, 512], BF16); wD = P.sb("wD", [128, 4, 1024], BF16)
    sgb = P.sb("sgb", [128, 512], BF16); hmb = P.sb("hmb", [128, 512], BF16)
    hmT = [P.sb(f"hmT{i}", [128, 512], BF16) for i in range(2)]
    ps = [P.ps(f"ps{i}", [128, 512], F32) for i in range(8)]
    P.dma("sp", ident[:], ident_d, writes=["ident"])
    units = [(2 * p_, 2) for p_ in range(16)] + [(32, 1)]
    ewg_v = ewg_d.rearrange("e (k p) h -> e p k h", p=128)
    ewu_v = ewu_d.rearrange("e (k p) h -> e p k h", p=128)
    ewd_v = ewd_d.rearrange("e (c p) f -> e p c f", p=128)
    h2v = h2_d.rearrange("(k p) t -> p k t", p=128)
    wrv = wr_d.rearrange("(t p) e -> p t e", p=128)
    ypv = yp_d.rearrange("(t p) f -> p t f", p=128)
    cnt = 0
    for gi in range(NG):
        P.dma("sp", H2[:], h2v[:, :, gi * TG:(gi + 1) * TG], writes=["H2"])
        P.dma("sp", Wr[:], wrv[:, gi * NT:(gi + 1) * NT, :], writes=["Wr"])
        P.op("pool", lambda e: e.memset(yacc[:], 0.0), writes=["yacc"] + [("yacc", t, h) for t in range(NT) for h in range(2)])
        for ui, (e0, ne) in enumerate(units):
            W = ne * 256
            for j in range(ne):
                P.dma("pool", wG[:, :, j * 256:(j + 1) * 256], ewg_v[e0 + j], writes=["wG"])
                P.dma("pool", wU[:, :, j * 256:(j + 1) * 256], ewu_v[e0 + j], writes=["wU"])
                P.dma("pool", wD[:, 2 * j:2 * j + 2, :], ewd_v[e0 + j], writes=["wD"])
            for t in range(NT):
                par = cnt % 2
                cnt += 1
                pG, pGk = ps[par * 2], f"ps{par * 2}"
                pU, pUk = ps[par * 2 + 1], f"ps{par * 2 + 1}"
                for k in range(8):
                    _mm(P, pG[:, 0:W], lhsT=H2[:, k, t * 128:(t + 1) * 128], rhs=wG[:, k, 0:W], start=(k == 0), stop=(k == 7), reads=["wG", "H2"], writes=[pGk])
                for k in range(8):
                    _mm(P, pU[:, 0:W], lhsT=H2[:, k, t * 128:(t + 1) * 128], rhs=wU[:, k, 0:W], start=(k == 0), stop=(k == 7), reads=["wU", "H2"], writes=[pUk])
                _act(P, sgb[:, 0:W], pG[:, 0:W], AF.Silu, reads=[pGk], writes=["sgb"])
                for j in range(ne):
                    _stt(P, "dve", hmb[:, j * 256:(j + 1) * 256], pU[:, j * 256:(j + 1) * 256], Wr[:, t, e0 + j:e0 + j + 1],
                         sgb[:, j * 256:(j + 1) * 256], ALU.mult, ALU.mult, reads=[pUk, "sgb", "Wr"], writes=["hmb"])
                pT = ps[4 + par][:].bitcast(BF16); pTk = f"ps{4 + par}"
                for c in range(2 * ne):
                    _tr(P, pT[:, c * 128:(c + 1) * 128], hmb[:, c * 128:(c + 1) * 128], ident[:], reads=["hmb", "ident"], writes=[pTk])
                hT = hmT[par]; hTk = f"hmT{par}"
                _cp(P, "act", hT[:, 0:2 * ne * 128], pT[:, 0:2 * ne * 128], reads=[pTk], writes=[hTk])
                for half in range(2):
                    py = ps[6 + half]; pyk = f"ps{6 + half}"
                    for c in range(2 * ne):
                        _mm(P, py[:, :], lhsT=hT[:, c * 128:(c + 1) * 128], rhs=wD[:, c, half * 512:(half + 1) * 512],
                            start=(c == 0), stop=(c == 2 * ne - 1), reads=[hTk, "wD"], writes=[pyk])
                    _tt(P, "dve", yacc[:, t, half * 512:(half + 1) * 512], yacc[:, t, half * 512:(half + 1) * 512], py[:, :], ALU.add,
                        reads=[pyk], writes=[("yacc", t, half)])
        P.dma("sp", ypv[:, gi * NT:(gi + 1) * NT, :], yacc[:], reads=[("yacc", t, h) for t in range(NT) for h in range(2)])
    P.emit()
    return nc


def build_B3(last):
    T = 2048 if last else 2112
    chunks = [(i * 512, 512, 0) for i in range(4)] + ([] if last else [(2048, 64, 1)])
    nc = bass.Bass("TRN2", target_bir_lowering=False)
    P = Prog(nc)

    def din(name, shape, dt=F32):
        return nc.dram_tensor(name, list(shape), dt, kind="ExternalInput").ap()

    yp_d = din("yp8", [8, T, 1024]); xm_d = din("xmi", [1024, T]); mod_d = din("modvi", [128, 96]); gf_d = din("gf", [128, 8])
    identf_d = din("identf", [128, 128]); ones_d = din("ones", [128, 128], BF16)
    out_d = nc.dram_tensor("outT", [1024, T], F32, kind="ExternalOutput").ap()
    modv = P.sb("modv", [128, 2, 48], F32); gf = P.sb("gf_s", [128, 8], F32)
    identf = P.sb("identf_s", [128, 128], F32); ones = P.sb("ones_s", [128, 128], BF16)
    epsb = P.sb("epsb", [128, 1], F32); zerob = P.sb("zerob", [128, 1], F32)
    ysum = [P.sb(f"ysum{i}", [128, 8, 1024], F32) for i in range(2)]
    xo = P.sb("xo", [128, 8, 512], F32); fo = P.sb("fo", [128, 8, 512], F32); sq = P.sb("sq", [128, 8, 512], BF16)
    ytile = P.sb("ytile", [128, 4, 1024], F32)
    tmpf = [P.sb(f"tmpf{i}", [128, 512], F32) for i in range(5)]
    ps = [P.ps(f"ps{i}", [128, 512], F32) for i in range(8)]
    P.dma("sp", modv[:].rearrange("p v j -> p (v j)"), mod_d, writes=["modv"])
    P.dma("sp", gf[:], gf_d, writes=["gf"]); P.dma("sp", identf[:], identf_d, writes=["identf"]); P.dma("sp", ones[:], ones_d, writes=["ones"])
    P.op("pool", lambda e: e.memset(epsb[:], EPS), writes=["epsb"])
    P.op("pool", lambda e: e.memset(zerob[:], 0.0), writes=["zerob"])
    ypv = yp_d.rearrange("c t f -> t c f")
    xmv = xm_d.rearrange("(k p) t -> p k t", p=128)
    outv = out_d.rearrange("(k p) t -> p k t", p=128)
    ti_g = 0
    for (c0, n, vec) in chunks:
        P.dma("sp", xo[:, :, 0:n], xmv[:, :, c0:c0 + n], writes=["x"])
        ntl = (n + 127) // 128
        for tt_ in range(ntl):
            tn = min(128, n - tt_ * 128)
            ys = ysum[ti_g % 2]; ysk = f"ysum{ti_g % 2}"
            ti_g += 1
            P.dma("sp", ys[0:tn], ypv[c0 + tt_ * 128:c0 + tt_ * 128 + tn, :, :], writes=[ysk])
            _tt(P, "dve", ys[0:tn, 0:4, :], ys[0:tn, 0:4, :], ys[0:tn, 4:8, :], ALU.add, reads=[ysk], writes=[ysk])
            _tt(P, "pool", ys[0:tn, 0:2, :], ys[0:tn, 0:2, :], ys[0:tn, 2:4, :], ALU.add, reads=[ysk], writes=[ysk])
            _tt(P, "dve", ytile[0:tn, tt_, :], ys[0:tn, 0, :], ys[0:tn, 1, :], ALU.add, reads=[ysk], writes=[("yt", tt_)])
        for oc in range(8):
            py = ps[oc % 2]; pyk = f"ps{oc % 2}"
            for tt_ in range(ntl):
                tn = min(128, n - tt_ * 128)
                _tr(P, py[:, tt_ * 128:tt_ * 128 + tn], ytile[0:tn, tt_, oc * 128:(oc + 1) * 128], identf[0:tn, 0:tn],
                    reads=[("yt", tt_), "identf"], writes=[pyk])
            _stt(P, "dve", xo[:, oc, 0:n], py[:, 0:n], modv[:, vec, 40 + oc:41 + oc], xo[:, oc, 0:n], ALU.mult, ALU.add,
                 reads=[pyk, "x", "modv"], writes=["x"])
        if last:
            _act(P, sq[:, :, 0:n], xo[:, :, 0:n], AF.Square, reads=["x"], writes=["sq"])
            for k in range(8):
                _mm(P, ps[2][:, 0:n], lhsT=ones[:], rhs=sq[:, k, 0:n], start=(k == 0), stop=(k == 7), reads=["sq", "ones"], writes=["ps2"])
            _act(P, tmpf[0][:, 0:n], ps[2][:, 0:n], AF.Sqrt, scale=1.0 / 1024.0, bias=epsb[:, 0:1], reads=["ps2", "epsb"], writes=["tmpf0"])
            P.op("dve", lambda e, n=n: e.reciprocal(out=tmpf[1][:, 0:n], in_=tmpf[0][:, 0:n]), reads=["tmpf0"], writes=["rstd"])
            for k in range(8):
                _stt(P, "dve", fo[:, k, 0:n], xo[:, k, 0:n], gf[:, k:k + 1], tmpf[1][:, 0:n], ALU.mult, ALU.mult, reads=["x", "rstd", "gf"], writes=[("fo", k)])
            P.dma("sp", outv[:, :, c0:c0 + n], fo[:, :, 0:n], reads=[("fo", k) for k in range(8)])
        else:
            P.dma("sp", outv[:, :, c0:c0 + n], xo[:, :, 0:n], reads=["x"])
    P.emit()
    return nc


def _run(nc, maps):
    res = run_bass_kernel_spmd(nc, maps, core_ids=list(range(8)))
    return res.results


def kernel(**inp):
    inp = {k: np.asarray(v) for k, v in inp.items()}
    cst = consts_A()
    x_cur = np.ascontiguousarray(inp["x"], dtype=np.float32)
    xc_cur = np.ascontiguousarray(inp["ctx"], dtype=np.float32)
    ident = np.eye(128, dtype=np.float32).astype(BFNP)
    identf = np.eye(128, dtype=np.float32)
    ones = np.ones((128, 128), np.float32).astype(BFNP)
    for layer in range(2):
        last = layer == 1
        ra = _run(build_A(layer, not last), prep_A(inp, layer, x_cur, xc_cur, cst))
        brT = np.zeros((2, 1024, S), dtype=BFNP)
        brcT = None if last else np.zeros((2, 1024, L), dtype=BFNP)
        for core in range(8):
            b, g = core // 4, core % 4
            r = ra[core]
            ya = np.asarray(r["ya"]).reshape(64, 64, 128).transpose(1, 0, 2).reshape(64, S)
            for i, arr in enumerate([ya, np.asarray(r["ybT"]), np.asarray(r["ycT"]), np.asarray(r["ydT"])]):
                brT[b, i * 256 + 64 * g:i * 256 + 64 * g + 64, :] = arr
            if not last:
                yc = np.asarray(r["yctx"])
                for i in range(4):
                    brcT[b, i * 256 + 64 * g:i * 256 + 64 * g + 64, :] = yc[i]
        del ra
        r1 = _run(build_B1(layer, last), prep_B(inp, layer, x_cur, xc_cur, brT, brcT, last))
        T = 2048 if last else 2112
        NT = 16 if last else 17
        TG = NT * 128
        h2_all = np.zeros((1024, 8 * TG), dtype=BFNP)
        wr_full = np.zeros((8 * TG, NE), dtype=np.float32)
        for tc in range(8):
            h2_all[:, tc * TG:tc * TG + T] = np.asarray(r1[tc]["h2T"])
            wr_full[tc * TG:(tc + 1) * TG] = np.asarray(r1[tc]["wr"])
        xmid = [np.asarray(r1[tc]["xmidT"]) for tc in range(8)]
        modvo = [np.asarray(r1[tc]["modvo"]) for tc in range(8)]
        del r1
        maps = []
        for c in range(8):
            m = {"h2a": h2_all, "ident": ident}
            wra = np.zeros((8 * TG, 33), dtype=np.float32)
            wra[:, 0:32] = wr_full[:, 32 * c:32 * c + 32]
            wra[:, 32] = 1.0 if c == 0 else 0.0
            m["wra"] = wra
            m["ewg"] = np.concatenate([inp["expert_w_gate"][layer][32 * c:32 * c + 32], inp["shared_w_gate"][layer][None]], 0)
            m["ewu"] = np.concatenate([inp["expert_w_up"][layer][32 * c:32 * c + 32], inp["shared_w_up"][layer][None]], 0)
            m["ewd"] = np.concatenate([inp["expert_w_down"][layer][32 * c:32 * c + 32], inp["shared_w_down"][layer][None]], 0)
            maps.append(m)
        r2 = _run(build_B2(last), maps)
        yparts = [np.asarray(r2[c]["ypart"]) for c in range(8)]
        del r2, maps
        maps = []
        gfv = np.ascontiguousarray(inp["final_norm_g"].reshape(8, 128).T)
        for tc in range(8):
            m = {"yp8": np.ascontiguousarray(np.stack([yparts[c][tc * TG:tc * TG + T] for c in range(8)], 0)),
                 "xmi": xmid[tc], "modvi": modvo[tc], "gf": gfv, "identf": identf, "ones": ones}
            maps.append(m)
        r3 = _run(build_B3(last), maps)
        x_new = np.zeros_like(x_cur)
        xc_new = np.zeros_like(xc_cur)
        for tc in range(8):
            b, q = tc // 4, tc % 4
            o = np.asarray(r3[tc]["outT"])
            x_new[b, q * 2048:(q + 1) * 2048] = o[:, 0:2048].T
            if not last:
                xc_new[b, q * 64:(q + 1) * 64] = o[:, 2048:2112].T
        x_cur, xc_cur = x_new, xc_new
        del r3, yparts, maps
    return np.ascontiguousarray(x_cur, dtype=np.float32)
```

```python
import os
import numpy as np
from contextlib import ExitStack
import concourse.bass as bass
import concourse.mybir as mybir
from concourse.bass_utils import run_bass_kernel_spmd

F32 = mybir.dt.float32
BF16 = mybir.dt.bfloat16
AF = mybir.ActivationFunctionType
ALU = mybir.AluOpType
AX = mybir.AxisListType

ENGS = ["pe", "act", "dve", "pool", "sp"]
NDMA = 40


class Prog:
    def __init__(self, nc):
        self.nc = nc
        self.es = ExitStack()
        self.streams = {e: [] for e in ENGS}
        self.cnt = {e: 0 for e in ENGS}
        self.sem = {e: self.es.enter_context(nc.semaphore(f"sem_{e}")) for e in ENGS}
        self.dsem = [self.es.enter_context(nc.semaphore(f"dsem{i}")) for i in range(NDMA)]
        self.dcnt = [0] * NDMA
        self.dnext = 0
        self.waited = {e: {} for e in ENGS}
        self.state = {}
        self.nwaits = 0

    def sb(self, name, shape, dt):
        return self.es.enter_context(self.nc.sbuf_tensor(name, list(shape), dt))

    def ps(self, name, shape, dt=F32):
        return self.es.enter_context(self.nc.psum_tensor(name, list(shape), dt))

    def _st(self, k):
        s = self.state.get(k)
        if s is None:
            s = {"w": None, "r": {}}
            self.state[k] = s
        return s

    def _need(self, eng, tok, waits):
        if tok is None:
            return
        kind, a, v = tok
        if kind == "eng":
            if a == "pe" and eng == "pe":
                return
            key = ("e", a)
        else:
            key = ("d", a)
        if self.waited[eng].get(key, 0) >= v:
            return
        self.waited[eng][key] = v
        waits.append((self.sem[a] if kind == "eng" else self.dsem[a], v))

    def _deps(self, eng, reads, writes):
        waits = []
        for k in reads:
            self._need(eng, self._st(k)["w"], waits)
        for k in writes:
            s = self._st(k)
            self._need(eng, s["w"], waits)
            for t in s["r"].values():
                self._need(eng, t, waits)
        self.nwaits += len(waits)
        return waits

    def _commit(self, tok, reads, writes):
        for k in reads:
            s = self._st(k)
            rk = (tok[0], tok[1])
            s["r"][rk] = tok
        for k in writes:
            s = self._st(k)
            s["w"] = tok
            s["r"] = {}

    def op(self, eng, fn, reads=(), writes=()):
        pr = [k for k in reads if isinstance(k, str) and k.startswith("ps")]
        if pr:
            reads = [k for k in reads if k not in pr]
            writes = list(writes) + pr
        waits = self._deps(eng, reads, writes)
        self.cnt[eng] += 1
        tok = ("eng", eng, self.cnt[eng])
        self._commit(tok, reads, writes)
        self.streams[eng].append((waits, fn, True))

    def dma(self, eng, out, in_, reads=(), writes=(), **kw):
        s = self.dnext
        self.dnext = (self.dnext + 1) % NDMA
        waits = self._deps(eng, reads, writes)
        if self.dcnt[s] > 0:
            self._need(eng, ("dma", s, 16 * self.dcnt[s]), waits)
        self.dcnt[s] += 1
        tok = ("dma", s, 16 * self.dcnt[s])
        self._commit(tok, reads, writes)
        sem = self.dsem[s]

        def fn(e, out=out, in_=in_, kw=kw, sem=sem):
            e.dma_start(out=out, in_=in_, **kw).then_inc(sem, 16)
            return None

        self.streams[eng].append((waits, fn, False))

    def finish(self):
        waits = []
        for s in range(NDMA):
            if self.dcnt[s] > 0:
                self._need("sp", ("dma", s, 16 * self.dcnt[s]), waits)
        for e in ENGS:
            if e != "sp" and self.cnt[e] > 0:
                self._need("sp", ("eng", e, self.cnt[e]), waits)
        self.streams["sp"].append((waits, None, False))

    def emit(self):
        self.finish()
        nc = self.nc
        with nc.Block() as block:
            def mk(name):
                def body(e):
                    sem = self.sem[name]
                    for waits, fn, track in self.streams[name]:
                        for (s, v) in waits:
                            e.wait_ge(s, v)
                        if fn is None:
                            continue
                        ins = fn(e)
                        if track:
                            ins.then_inc(sem, 1)
                return body
            block.tensor(mk("pe"))
            block.scalar(mk("act"))
            block.vector(mk("dve"))
            block.gpsimd(mk("pool"))
            block.sync(mk("sp"))
        self.es.close()


def _mm(P, out, lhsT, rhs, start=True, stop=True, reads=(), writes=()):
    P.op("pe", lambda e: e.matmul(out, lhsT=lhsT, rhs=rhs, start=start, stop=stop), reads, writes)

def _tr(P, out, in_, ident, reads=(), writes=()):
    P.op("pe", lambda e: e.transpose(out, in_, ident), reads, writes)

def _act(P, out, in_, func, reads=(), writes=(), **kw):
    P.op("act", lambda e: e.activation(out=out, in_=in_, func=func, **kw), reads, writes)

def _tt(P, eng, out, in0, in1, op, reads=(), writes=()):
    P.op(eng, lambda e: e.tensor_tensor(out=out, in0=in0, in1=in1, op=op), reads, writes)

def _ts(P, eng, out, in0, s1, s2, op0, op1=None, reads=(), writes=()):
    if op1 is None:
        P.op(eng, lambda e: e.tensor_scalar(out=out, in0=in0, scalar1=s1, scalar2=None, op0=op0), reads, writes)
    else:
        P.op(eng, lambda e: e.tensor_scalar(out=out, in0=in0, scalar1=s1, scalar2=s2, op0=op0, op1=op1), reads, writes)

def _stt(P, eng, out, in0, scalar, in1, op0, op1, reads=(), writes=()):
    P.op(eng, lambda e: e.scalar_tensor_tensor(out=out, in0=in0, scalar=scalar, in1=in1, op0=op0, op1=op1), reads, writes)

def _cp(P, eng, out, in_, reads=(), writes=()):
    if eng == "act":
        P.op("act", lambda e: e.copy(out=out, in_=in_), reads, writes)
    else:
        P.op(eng, lambda e: e.tensor_copy(out=out, in_=in_), reads, writes)

def _barrier(P):
    toks = [("eng", e, P.cnt[e]) for e in ENGS if P.cnt[e] > 0]
    toks += [("dma", s, 16 * P.dcnt[s]) for s in range(NDMA) if P.dcnt[s] > 0]
    for e in ENGS:
        waits = []
        for t in toks:
            P._need(e, t, waits)
        if waits:
            P.streams[e].append((waits, None, False))
    P.state = {}


import math
import numpy as np
import ml_dtypes

BFNP = ml_dtypes.bfloat16
S = 8192
L = 256
EPS = 1e-6
NCH = 16
W_G1, W_G2, W_G3, W_G4, W_CB, W_V, W_GC = 0, 128, 256, 384, 512, 576, 704
WCOLS = 832


def consts_A():
    c = {}
    c["ident"] = np.eye(128, dtype=np.float32).astype(BFNP)
    c["ones"] = np.ones((128, 128), np.float32).astype(BFNP)
    e = np.zeros((65, 64), np.float32); e[64, :] = 1.0
    c["e64"] = e
    c["ones64"] = np.ones((64, 64), np.float32)
    n = np.arange(128)
    a = 2 * np.pi * np.outer(n, n) / 128.0
    c["dft128"] = np.stack([np.cos(a), -np.sin(a), -np.cos(a)], 1).astype(BFNP)
    k1 = np.arange(128)[:, None]; n2 = np.arange(64)[None, :]
    t = 2 * np.pi * (k1 * n2) / 8192.0
    c["tw"] = np.stack([np.cos(t), np.sin(t)], 1).astype(np.float32)
    m = np.arange(64)
    a64 = 2 * np.pi * np.outer(m, m) / 64.0
    c["dft64"] = np.stack([np.cos(a64), np.sin(a64)], 1).astype(BFNP)
    c["cs64"] = np.concatenate([np.cos(a64), np.sin(a64)], 1).astype(np.float32)
    q = np.arange(256)
    a256 = 2 * np.pi * np.outer(q, q) / 256.0
    d = np.stack([np.cos(a256), -np.sin(a256)], 1)
    c["dft256"] = d.reshape(2, 128, 2, 256).transpose(1, 0, 2, 3).astype(BFNP)
    tt_ = np.arange(S)
    half = 16
    inv = 1.0 / (10000.0 ** (np.arange(0, half, 2, dtype=np.float32) / half))
    ang_r = (tt_ // 64).astype(np.float32)[:, None] * inv
    ang_c = (tt_ % 64).astype(np.float32)[:, None] * inv
    cos32 = np.concatenate([np.cos(ang_r), np.cos(ang_r), np.cos(ang_c), np.cos(ang_c)], 1)
    sin32 = np.concatenate([-np.sin(ang_r), np.sin(ang_r), -np.sin(ang_c), np.sin(ang_c)], 1)
    c["ropec"] = np.ascontiguousarray(np.concatenate([cos32, cos32], 1).T.astype(np.float32))
    c["ropes"] = np.ascontiguousarray(np.concatenate([sin32, sin32], 1).T.astype(np.float32))
    return c


ROPE_PERM = np.concatenate([np.arange(8, 16), np.arange(0, 8), np.arange(24, 32), np.arange(16, 24)])


def prep_A(inp, layer, x_cur, xc_cur, cst):
    maps = []
    w_in = inp["w_in"][layer]
    rb = inp["na_rel_bias"][layer]
    qc = np.arange(64)[:, None]; kc = np.arange(64)[None, :]
    col_lo = np.clip(qc - 8, 0, 48)
    ok = (kc >= col_lo) & (kc < col_lo + 16)
    dc = np.clip(kc - qc + 15, 0, 30)
    for core in range(8):
        b, g = core // 4, core % 4
        m = {}
        m["xT"] = np.ascontiguousarray(x_cur[b].T)
        m["cT"] = np.ascontiguousarray(xc_cur[b].T)
        cv = np.stack([inp["c"][b], inp["c_ctx"]], 0)
        m["cvec"] = np.ascontiguousarray(cv.reshape(2, 8, 128).transpose(2, 0, 1).reshape(128, 16))
        m["adaw"] = np.ascontiguousarray(inp["ada_w"][layer][:, 0:2048])
        m["adab"] = np.ascontiguousarray(inp["ada_b"][layer][0:2048].reshape(16, 128).T)
        m["g1"] = np.ascontiguousarray(inp["norm1_g"][layer].reshape(8, 128).T)
        def cols(off, perm=None):
            w = w_in[:, off + 64 * g: off + 64 * g + 64]
            if perm is not None:
                w = w[:, np.concatenate([perm, 32 + perm])]
            return w
        OFF = dict(A=0, BQ=256, BK=512, BV=768, CB=1024, CC=1280, CX=1536, DQ=1792, DK=2048, DV=2304)
        wh = np.concatenate([cols(OFF["DQ"]), cols(OFF["BQ"]), cols(OFF["DK"]), cols(OFF["BK"]),
                             cols(OFF["DQ"], ROPE_PERM), cols(OFF["CC"]), cols(OFF["DK"], ROPE_PERM), cols(OFF["CX"]),
                             cols(OFF["CB"]), cols(OFF["DV"]), cols(OFF["BV"])], 1)
        m["wh"] = np.ascontiguousarray(wh)
        m["waT"] = np.ascontiguousarray(cols(OFF["A"]).T)
        cw = np.zeros((128, 3), np.float32)
        cw[64:128, :] = inp["conv_w"][layer][:, 64 * g: 64 * g + 64].T
        m["cw"] = cw
        tb = rb[g][:, dc]
        tb = np.where(ok[None], tb, np.float32(-30000.0)).astype(np.float32)
        tb = np.ascontiguousarray(tb.transpose(2, 0, 1))
        tpad = np.concatenate([tb, np.zeros((64, 1, 64), np.float32)], 1)
        nab = np.zeros((2, 128, 16, 64), np.float32)
        nab[0, 0:64] = tpad; nab[0, 64:128, 0:15] = tpad[:, 1:16]
        nab[1, 64:128] = tpad; nab[1, 0:64, 0:15] = tpad[:, 1:16]
        m["nab"] = np.ascontiguousarray(nab.transpose(1, 0, 2, 3).reshape(128, 2 * 16 * 64))
        m["dl"] = np.ascontiguousarray(np.broadcast_to(inp["diff_lambda"][layer].reshape(1, 128), (128, 128)))
        m["subg"] = np.ascontiguousarray(inp["diff_subln_g"][layer].reshape(64, 1))
        for k in ("ident", "ones", "e64", "ones64", "dft128", "tw", "dft64", "cs64", "dft256", "ropec", "ropes"):
            m[k] = cst[k]
        maps.append(m)
    return maps


def build_A(layer, ctx_br, stop=99):
    lam_init = 0.8 - 0.6 * math.exp(-0.3 * layer)
    nc = bass.Bass("TRN2", target_bir_lowering=False)
    P = Prog(nc)

    def din(name, shape, dt=F32):
        return nc.dram_tensor(name, list(shape), dt, kind="ExternalInput").ap()

    def dout(name, shape, dt=BF16):
        return nc.dram_tensor(name, list(shape), dt, kind="ExternalOutput").ap()

    xT = din("xT", [1024, S]); cT = din("cT", [1024, L])
    cvec_d = din("cvec", [128, 16]); adaw_d = din("adaw", [1024, 2048]); adab_d = din("adab", [128, 16])
    g1_d = din("g1", [128, 8]); wh_d = din("wh", [1024, 704]); waT_d = din("waT", [64, 1024])
    cw_d = din("cw", [128, 3]); nab_d = din("nab", [128, 2048]); dl_d = din("dl", [128, 128]); subg_d = din("subg", [64, 1])
    ident_d = din("ident", [128, 128], BF16); ones_d = din("ones", [128, 128], BF16)
    e64_d = din("e64", [65, 64]); ones64_d = din("ones64", [64, 64])
    dft128_d = din("dft128", [128, 3, 128], BF16); tw_d = din("tw", [128, 2, 64])
    dft64_d = din("dft64", [64, 2, 64], BF16); cs64_d = din("cs64", [64, 128])
    dft256_d = din("dft256", [128, 2, 2, 256], BF16)
    ropec_d = din("ropec", [64, S]); ropes_d = din("ropes", [64, S])
    ya_d = dout("ya", [64, 8192]); yb_d = dout("ybT", [64, S]); yc_d = dout("ycT", [64, S]); yd_d = dout("ydT", [64, S])
    yctx_d = dout("yctx", [4, 64, L]) if ctx_br else None

    TQ = P.sb("TQ", [128, S], BF16)
    TK = P.sb("TK", [128, S + L], BF16)
    VD = P.sb("VD", [128, 66, 65], BF16)
    VB = P.sb("VB", [128, 66, 65], BF16)
    S1 = P.sb("S1", [128, S], BF16)
    S2 = P.sb("S2", [128, S + 2], BF16)
    S3 = P.sb("S3", [128, S], BF16)
    S4 = P.sb("S4", [128, 8, 512], F32)
    S5 = P.sb("S5", [128, 2, 8, 512], BF16)
    wbf = P.sb("wbf", [128, 8, WCOLS], BF16)
    tmpf = [P.sb(f"tmpf{i}", [128, 512], F32) for i in range(6)]
    tmpb = [P.sb(f"tmpb{i}", [128, 512], BF16) for i in range(4)]
    ident = P.sb("ident_s", [128, 128], BF16); ones = P.sb("ones_s", [128, 128], BF16)
    e64 = P.sb("e64_s", [65, 64], F32); ones64 = P.sb("ones64_s", [64, 64], F32)
    dft128 = P.sb("dft128_s", [128, 3, 128], BF16); tw = P.sb("tw_s", [128, 2, 64], F32)
    dft64 = P.sb("dft64_s", [64, 2, 64], BF16); cs64 = P.sb("cs64_s", [64, 128], F32)
    dft256 = P.sb("dft256_s", [128, 2, 2, 256], BF16)
    cvec = P.sb("cvec_s", [128, 16], F32); scv = P.sb("scv", [128, 16], F32)
    adab = P.sb("adab_s", [128, 16], F32); g1 = P.sb("g1_s", [128, 8], F32)
    modv = P.sb("modv", [128, 2, 16], F32)
    Amod = P.sb("Amod", [128, 2, 8], F32)
    cw = P.sb("cw_s", [128, 3], F32); nab = P.sb("nab_s", [128, 2, 16, 64], F32)
    dl = P.sb("dl_s", [128, 128], F32); subg = P.sb("subg_s", [64, 1], F32)
    lamt = P.sb("lamt", [128, 8], F32)
    epsb = P.sb("epsb", [128, 1], F32)
    ropec = P.sb("ropec_s", [64, 512], F32); ropes = P.sb("ropes_s", [64, 512], F32)
    Osb = [P.sb(f"Osb{i}", [65, 512], F32) for i in range(2)]
    obuf = [P.sb(f"obuf{i}", [64, 512], BF16) for i in range(2)]
    VBo = P.sb("VBo", [64, 66, 65], BF16)
    TQc = P.sb("TQc", [128, L], BF16)
    Uc = P.sb("Uc", [128, L + 2], BF16); CBc = P.sb("CBc", [128, L], BF16)
    Gtok = P.sb("Gtok", [128, 2, 128], BF16)
    ps = [P.ps(f"ps{i}", [128, 512], F32) for i in range(8)]

    P.op("pool", lambda e: e.memset(epsb[:], EPS), writes=["epsb"])
    waT = S3[:].bitcast(F32)[0:64, 0:1024]
    for (dst, src, key) in [(ident, ident_d, "ident"), (ones, ones_d, "ones"), (e64, e64_d, "e64"), (ones64, ones64_d, "ones64"),
                            (dft128, dft128_d, "dft128"), (tw, tw_d, "tw"), (dft64, dft64_d, "dft64"), (cs64, cs64_d, "cs64"),
                            (dft256, dft256_d, "dft256"), (cvec, cvec_d, "cvec"), (adab, adab_d, "adab"), (g1, g1_d, "g1"),
                            (cw, cw_d, "cw"), (dl, dl_d, "dl"), (subg, subg_d, "subg")]:
        P.dma("sp", dst[:], src, writes=[key])
    P.dma("sp", waT, waT_d, writes=["waT"])
    P.dma("sp", nab[:], nab_d.rearrange("p (v d q) -> p v d q", v=2, d=16), writes=["nab"])

    whv = wh_d.rearrange("(k p) c -> p k c", p=128)
    P.dma("sp", S4[:, :, 0:512], whv[:, :, 0:512], writes=["S4"])
    _cp(P, "dve", wbf[:, :, 0:512], S4[:, :, 0:512], reads=["S4"], writes=["wbf"])
    P.dma("sp", S4[:, :, 0:192], whv[:, :, 512:704], writes=["S4"])
    _cp(P, "dve", wbf[:, :, 512:704], S4[:, :, 0:192], reads=["S4"], writes=["wbf"])
    for k in range(8):
        _mm(P, ps[k % 2][:, 0:128], lhsT=waT[:, k * 128:(k + 1) * 128], rhs=cs64[:], reads=["waT", "cs64"], writes=[f"ps{k % 2}"])
        _cp(P, "act", wbf[:, k, W_GC:W_GC + 128], ps[k % 2][:, 0:128], reads=[f"ps{k % 2}"], writes=["wbf"])

    _act(P, scv[:], cvec[:], AF.Silu, reads=["cvec"], writes=["scv"])
    adv = adaw_d.rearrange("(k p) c -> p k c", p=128)
    for q4 in range(4):
        P.dma("sp", S4[:], adv[:, :, q4 * 512:(q4 + 1) * 512], writes=["S4"])
        for jj in range(4):
            j = q4 * 4 + jj
            for k in range(8):
                _mm(P, ps[2][:, 2 * j:2 * j + 2], lhsT=S4[:, k, jj * 128:(jj + 1) * 128], rhs=scv[:, k::8],
                    start=(k == 0), stop=(k == 7), reads=["S4", "scv"], writes=["ps2"])
    for v in range(2):
        _tt(P, "dve", modv[:, v, :], ps[2][:, v:32:2], adab[:], ALU.add, reads=["ps2", "adab"], writes=["modv"])
        _stt(P, "dve", Amod[:, v, :], modv[:, v, 8:16], 1.0, g1[:], ALU.add, ALU.mult, reads=["modv", "g1"], writes=["Amod"])

    _tt(P, "dve", tmpf[0][:, 0:32], dl[:, 0:32], dl[:, 32:64], ALU.mult, reads=["dl"], writes=["tmpf0"])
    _tt(P, "dve", tmpf[0][:, 32:64], dl[:, 64:96], dl[:, 96:128], ALU.mult, reads=["dl"], writes=["tmpf0"])
    P.op("dve", lambda e: e.reduce_sum(out=lamt[:, 0:1], in_=tmpf[0][:, 0:32], axis=AX.X), reads=["tmpf0"], writes=["lamt"])
    P.op("dve", lambda e: e.reduce_sum(out=lamt[:, 1:2], in_=tmpf[0][:, 32:64], axis=AX.X), reads=["tmpf0"], writes=["lamt"])
    _act(P, lamt[:, 2:4], lamt[:, 0:2], AF.Exp, reads=["lamt"], writes=["lamt"])
    _stt(P, "dve", lamt[:, 4:5], lamt[:, 3:4], -lam_init, lamt[:, 2:3], ALU.add, ALU.subtract, reads=["lamt"], writes=["lamt"])
    _ts(P, "dve", lamt[0:64, 5:6], subg[:], 1.0 - lam_init, None, ALU.mult, reads=["subg", "lamt"], writes=["lamt"])

    _barrier(P)

    if stop == 0:
        P.emit(); return nc
    GT = S1; U = S2; CB = S3
    sq = S5[:, 0]; hh = S5[:, 1]
    P.op("pool", lambda e: e.memset(U[:, 0:1], 0.0), writes=[("U", -1)])
    P.op("pool", lambda e: e.memset(U[:, S + 1:S + 2], 0.0), writes=[("U", 99)])
    P.op("pool", lambda e: e.memset(Uc[:], 0.0), writes=["Uc"])
    P.op("pool", lambda e: e.memset(VD[:, :, 64:65], 1.0), writes=["VDones"])
    P.op("pool", lambda e: e.memset(VB[:, :, 64:65], 1.0), writes=["VBones"])

    def chunk(src_ap, n, vec, j):
        lat = vec == 0
        P.dma("sp", S4[:, :, 0:n], src_ap, writes=["x"])
        if lat:
            P.dma("sp", ropec[:], ropec_d[:, j * 512:(j + 1) * 512], writes=["ropec"])
            P.dma("sp", ropes[:], ropes_d[:, j * 512:(j + 1) * 512], writes=["ropes"])
        _act(P, sq[:, :, 0:n], S4[:, :, 0:n], AF.Square, reads=["x"], writes=["sq"])
        for k in range(8):
            _mm(P, ps[0][:, 0:n], lhsT=ones[:], rhs=sq[:, k, 0:n], start=(k == 0), stop=(k == 7), reads=["sq", "ones"], writes=["ps0"])
        _act(P, tmpf[0][:, 0:n], ps[0][:, 0:n], AF.Sqrt, scale=1.0 / 1024.0, bias=epsb[:, 0:1], reads=["ps0"], writes=["tmpf0"])
        P.op("dve", lambda e: e.reciprocal(out=tmpf[1][:, 0:n], in_=tmpf[0][:, 0:n]), reads=["tmpf0"], writes=["rstd"])
        for k in range(8):
            t = tmpf[2 + (k % 2)]
            _stt(P, "dve", t[:, 0:n], S4[:, k, 0:n], Amod[:, vec, k:k + 1], tmpf[1][:, 0:n], ALU.mult, ALU.mult,
                 reads=["x", "rstd", "Amod"], writes=[f"tmpf{2 + k % 2}"])
            _act(P, hh[:, k, 0:n], t[:, 0:n], AF.Identity, bias=modv[:, vec, k:k + 1], reads=[f"tmpf{2 + k % 2}", "modv"], writes=[("h", k)])
        hkeys = [("h", k) for k in range(8)]

        def proj(pst, pkey, c0, m, p0=0):
            for k in range(8):
                _mm(P, pst[p0:p0 + m, 0:n], lhsT=wbf[:, k, c0:c0 + m], rhs=hh[:, k, 0:n], start=(k == 0), stop=(k == 7),
                    reads=hkeys + ["wbf"], writes=[pkey])

        tq_cols = slice(j * 512, (j + 1) * 512) if lat else None
        tk_cols = slice(j * 512, (j + 1) * 512) if lat else slice(S, S + L)
        proj(ps[1], "ps1", W_G1, 128)
        proj(ps[2], "ps2", W_G2, 128)
        proj(ps[3], "ps3", W_G3, 128)
        proj(ps[4], "ps4", W_G4, 128)
        if lat:
            for (pa, pak, pb_, pbk, dst, dk_) in [(ps[1], "ps1", ps[3], "ps3", TQ, ("TQ", j, 0)), (ps[2], "ps2", ps[4], "ps4", TK, ("TK", j, 0))]:
                _tt(P, "dve", tmpf[4][0:64, :], pa[0:64, :], ropec[:], ALU.mult, reads=[pak, "ropec"], writes=["tmpf4"])
                _tt(P, "dve", tmpf[5][0:64, :], pb_[0:64, :], ropes[:], ALU.mult, reads=[pbk, "ropes"], writes=["tmpf5"])
                _tt(P, "pool", dst[0:64, tq_cols], tmpf[4][0:64, :], tmpf[5][0:64, :], ALU.add, reads=["tmpf4", "tmpf5"], writes=[dk_])
            _cp(P, "act", TQ[64:128, tq_cols], ps[1][64:128, :], reads=["ps1"], writes=[("TQ", j, 1)])
            _cp(P, "act", TK[64:128, tk_cols], ps[2][64:128, :], reads=["ps2"], writes=[("TK", j, 1)])
        else:
            _cp(P, "act", TQc[:, :], ps[1][:, 0:n], reads=["ps1"], writes=["TQc"])
            _cp(P, "act", TK[:, tk_cols], ps[2][:, 0:n], reads=["ps2"], writes=[("TK", "c")])
        _cp(P, "act", tmpf[4][64:128, 0:n], ps[3][64:128, 0:n], reads=["ps3"], writes=["tmpf4"])
        if lat:
            _tt(P, "dve", U[64:128, 1 + j * 512:1 + (j + 1) * 512], tmpf[4][64:128, :], ps[4][64:128, :], ALU.mult,
                reads=["tmpf4", "ps4"], writes=[("U", j)])
        else:
            _tt(P, "dve", Uc[64:128, 1:1 + n], tmpf[4][64:128, 0:n], ps[4][64:128, 0:n], ALU.mult, reads=["tmpf4", "ps4", "Uc"], writes=["Uc"])
        proj(ps[5], "ps5", W_CB, 64, p0=64)
        if lat:
            _cp(P, "act", CB[64:128, j * 512:(j + 1) * 512], ps[5][64:128, :], reads=["ps5"], writes=[("CB", j)])
        else:
            _cp(P, "act", CBc[64:128, 0:n], ps[5][64:128, 0:n], reads=["ps5"], writes=["CBc"])
        if lat:
            proj(ps[6], "ps6", W_GC, 128)
            _cp(P, "act", GT[:, j * 512:(j + 1) * 512], ps[6][:, :], reads=["ps6"], writes=[("GT", j)])
        ntile = n // 128
        ncol = 128 if lat else 256
        for t in range(ntile):
            for k in range(8):
                _mm(P, ps[7][:, t * 128:(t + 1) * 128] if lat else ps[7][:, t * 256:(t + 1) * 256],
                    lhsT=hh[:, k, t * 128:(t + 1) * 128], rhs=wbf[:, k, W_V:W_V + ncol],
                    start=(k == 0), stop=(k == 7), reads=hkeys + ["wbf"], writes=["ps7"])
        t0 = j * 4 if lat else 64
        if lat:
            pv = ps[7][:, :].rearrange("p (t c) -> p t c", c=128)
            _cp(P, "dve", VD[:, t0:t0 + ntile, 0:64], pv[:, :, 0:64], reads=["ps7"], writes=[("VD", j)])
            _cp(P, "act", VB[:, t0:t0 + ntile, 0:64], pv[:, :, 64:128], reads=["ps7"], writes=[("VB", j)])
        else:
            pv = ps[7][:, :].rearrange("p (t c) -> p t c", c=256)
            _cp(P, "dve", VD[:, t0:t0 + ntile, 0:64], pv[:, :, 0:64], reads=["ps7"], writes=[("VD", "c")])
            _cp(P, "act", VB[:, t0:t0 + ntile, 0:64], pv[:, :, 64:128], reads=["ps7"], writes=[("VB", "c")])
            _cp(P, "dve", Gtok[:, :, :], pv[:, :, 128:256], reads=["ps7"], writes=["Gtok"])

    xv = xT.rearrange("(k p) t -> p k t", p=128)
    cv_ = cT.rearrange("(k p) t -> p k t", p=128)
    chunk(cv_, L, 1, None)
    import os
    for j in range(int(os.environ.get('CHUNKS', NCH))):
        chunk(xv[:, :, j * 512:(j + 1) * 512], 512, 0, j)

    if stop == 1:
        P.emit(); return nc
    P.dma("sp", VBo[:, :, :], VB[64:128, :, :], reads=[("VB", j) for j in range(NCH)] + [("VB", "c"), "VBones"], writes=["VBo"])
    def conv(Ut, CBt, n_tot, out_ap_fn, step):
        for c0 in range(0, n_tot, step):
            n = min(step, n_tot - c0)
            t = tmpf[0]; o = tmpb[0]
            _ts(P, "dve", t[64:128, 0:n], Ut[64:128, c0:c0 + n], cw[64:128, 0:1], None, ALU.mult, reads=["Uall", "cw"], writes=["tmpf0"])
            _stt(P, "dve", t[64:128, 0:n], Ut[64:128, c0 + 1:c0 + 1 + n], cw[64:128, 1:2], t[64:128, 0:n], ALU.mult, ALU.add,
                 reads=["Uall", "tmpf0"], writes=["tmpf0"])
            _stt(P, "dve", t[64:128, 0:n], Ut[64:128, c0 + 2:c0 + 2 + n], cw[64:128, 2:3], t[64:128, 0:n], ALU.mult, ALU.add,
                 reads=["Uall", "tmpf0"], writes=["tmpf0"])
            _tt(P, "dve", o[64:128, 0:n], t[64:128, 0:n], CBt[64:128, c0:c0 + n], ALU.mult, reads=["tmpf0", "CBall"], writes=["tmpb0"])
            P.dma("sp", out_ap_fn(c0, n), o[64:128, 0:n], reads=["tmpb0"])

    _barrier(P)
    conv(U, CB, S, lambda c0, n: yc_d[:, c0:c0 + n], 512)
    if ctx_br:
        conv(Uc, CBc, L, lambda c0, n: yctx_d[2, :, c0:c0 + n], 256)
    _barrier(P)

    if stop == 2:
        P.emit(); return nc
    Gsb = S4[:].rearrange("p a b -> p (a b)").bitcast(BF16).rearrange("p (n c) -> p n c", c=128)
    Apr = S3[:, 0:4096].rearrange("p (n c) -> p n c", c=64)
    Api = S3[:, 4096:8192].rearrange("p (n c) -> p n c", c=64)
    Atr = S2[0:64, 0:8192].rearrange("p (c k) -> p c k", k=128)
    Ati = S5[:].rearrange("p a b c -> p (a b c)")[0:64, 0:8192].rearrange("p (c k) -> p c k", k=128)
    psb = [p_[:].bitcast(BF16) for p_ in ps]
    for i in range(8):
        pb = psb[i % 2]
        for q in range(8):
            n2 = i * 8 + q
            _tr(P, pb[:, q * 128:(q + 1) * 128], GT[:, n2::64], ident[:], reads=["ident"], writes=[f"ps{i % 2}"])
        _cp(P, "act" if i % 2 else "dve", Gsb[:, i * 8:(i + 1) * 8, :], pb[:, :].rearrange("p (n c) -> p n c", c=128),
            reads=[f"ps{i % 2}"], writes=[("G", i)])
    for gI in range(8):
        gc = Gsb[:, gI * 8:(gI + 1) * 8, 0:64]; gs = Gsb[:, gI * 8:(gI + 1) * 8, 64:128]
        par, pai = ps[2 + (gI % 2) * 2], ps[3 + (gI % 2) * 2]
        kr, ki = f"ps{2 + (gI % 2) * 2}", f"ps{3 + (gI % 2) * 2}"
        _mm(P, par[:, :], lhsT=dft128[:, 0, :], rhs=gc, start=True, stop=False, reads=[("G", gI)], writes=[kr])
        _mm(P, par[:, :], lhsT=dft128[:, 1, :], rhs=gs, start=False, stop=True, reads=[("G", gI)], writes=[kr])
        _mm(P, pai[:, :], lhsT=dft128[:, 2, :], rhs=gs, start=True, stop=False, reads=[("G", gI)], writes=[ki])
        _mm(P, pai[:, :], lhsT=dft128[:, 1, :], rhs=gc, start=False, stop=True, reads=[("G", gI)], writes=[ki])
        tcb = tw[:, 0, gI * 8:(gI + 1) * 8].unsqueeze(2).broadcast_to([128, 8, 64])
        tsb = tw[:, 1, gI * 8:(gI + 1) * 8].unsqueeze(2).broadcast_to([128, 8, 64])
        v3 = lambda a: a[:, :].rearrange("p (n c) -> p n c", c=64)
        _tt(P, "dve", v3(tmpf[0]), v3(par), tcb, ALU.mult, reads=[kr], writes=["tmpf0"])
        _tt(P, "dve", v3(tmpf[1]), v3(pai), tsb, ALU.mult, reads=[ki], writes=["tmpf1"])
        _tt(P, "pool", Apr[:, gI * 8:(gI + 1) * 8, :], v3(tmpf[0]), v3(tmpf[1]), ALU.add, reads=["tmpf0", "tmpf1"], writes=[("Apr", gI)])
        _tt(P, "dve", v3(tmpf[2]), v3(pai), tcb, ALU.mult, reads=[ki], writes=["tmpf2"])
        _tt(P, "dve", v3(tmpf[3]), v3(par), tsb, ALU.mult, reads=[kr], writes=["tmpf3"])
        _tt(P, "pool", Api[:, gI * 8:(gI + 1) * 8, :], v3(tmpf[2]), v3(tmpf[3]), ALU.subtract, reads=["tmpf2", "tmpf3"], writes=[("Api", gI)])
    apr_all = [("Apr", i) for i in range(8)]; api_all = [("Api", i) for i in range(8)]
    for (src, dst, rk, nm) in [(Apr, Atr, apr_all, "Atr"), (Api, Ati, api_all, "Ati")]:
        for i in range(8):
            pb = psb[6 + (i % 2)]
            for q in range(8):
                c = i * 8 + q
                _tr(P, pb[0:64, q * 128:(q + 1) * 128], src[:, :, c], ident[:], reads=rk + ["ident"], writes=[f"ps{6 + i % 2}"])
            _cp(P, "act" if i % 2 else "dve", dst[:, i * 8:(i + 1) * 8, :], pb[0:64, :].rearrange("p (c k) -> p c k", k=128),
                reads=[f"ps{6 + i % 2}"], writes=[(nm, i)])
    nrm = 1.0 / math.sqrt(8192.0 * 64.0)
    for i in range(16):
        pp = ps[i % 2]; pk = f"ps{i % 2}"
        _mm(P, pp[0:64, :], lhsT=dft64[:, 0, :], rhs=Atr[:, i * 4:(i + 1) * 4, :], start=True, stop=False, reads=[("Atr", i // 2)], writes=[pk])
        _mm(P, pp[0:64, :], lhsT=dft64[:, 1, :], rhs=Ati[:, i * 4:(i + 1) * 4, :], start=False, stop=True, reads=[("Ati", i // 2)], writes=[pk])
        ob = tmpb[i % 2]
        _act(P, ob[0:64, :], pp[0:64, :], AF.Copy, scale=nrm, reads=[pk], writes=[f"tmpb{i % 2}"])
        P.dma("sp", ya_d[:, i * 512:(i + 1) * 512], ob[0:64, :], reads=[f"tmpb{i % 2}"])
    if ctx_br:
        for t in range(2):
            _mm(P, ps[2][0:64, 0:256], lhsT=Gtok[:, t, 0:64], rhs=dft256[:, t, 0, :], start=(t == 0), stop=False, reads=["Gtok", "dft256"], writes=["ps2"])
            _mm(P, ps[2][0:64, 0:256], lhsT=Gtok[:, t, 64:128], rhs=dft256[:, t, 1, :], start=False, stop=(t == 1), reads=["Gtok", "dft256"], writes=["ps2"])
        _act(P, tmpb[2][0:64, 0:256], ps[2][0:64, 0:256], AF.Copy, scale=1.0 / math.sqrt(256.0 * 64.0), reads=["ps2"], writes=["tmpb2"])
        P.dma("sp", yctx_d[0, :, :], tmpb[2][0:64, 0:256], reads=["tmpb2"])
    _barrier(P)

    if stop == 3:
        P.emit(); return nc
    def lbcast_recip(Ot, okey, n, dst, dkey, pp, pk):
        _mm(P, pp[0:64, 0:n], lhsT=e64[:], rhs=Ot[:, 0:n], reads=[okey, "e64"], writes=[pk])
        P.op("dve", lambda e: e.reciprocal(out=dst[0:64, 0:n], in_=pp[0:64, 0:n]), reads=[pk], writes=[dkey])

    def dense_attn(q_ap, k_tile_fn, v_tile_fn, nkt, n, scale, Ot, okey, pO, pOk, ps_s):
        def smm(kt):
            pp, pk = ps_s[kt % len(ps_s)]
            kap, kk = k_tile_fn(kt)
            _mm(P, pp[:, 0:n], lhsT=kap, rhs=q_ap[0], reads=list(kk) + list(q_ap[1]), writes=[pk])
        smm(0)
        for kt in range(nkt):
            if kt + 1 < nkt:
                smm(kt + 1)
            pp, pk = ps_s[kt % len(ps_s)]
            pt = tmpb[kt % 4]; ptk = f"tmpb{kt % 4}"
            _act(P, pt[:, 0:n], pp[:, 0:n], AF.Exp, scale=scale, reads=[pk], writes=[ptk])
            vap, vk = v_tile_fn(kt)
            _mm(P, pO[0:65, 0:n], lhsT=vap, rhs=pt[:, 0:n], start=(kt == 0), stop=(kt == nkt - 1), reads=list(vk) + [ptk], writes=[pOk])
        _cp(P, "dve", Ot[:, 0:n], pO[0:65, 0:n], reads=[pOk], writes=[okey])

    def diff_combine(n, out_dram):
        lbcast_recip(Osb[0], "Osb0", n, tmpf[0], "tmpf0", ps[6], "ps6")
        lbcast_recip(Osb[1], "Osb1", n, tmpf[1], "tmpf1", ps[7], "ps7")
        _tt(P, "dve", tmpf[2][0:64, 0:n], Osb[0][0:64, 0:n], tmpf[0][0:64, 0:n], ALU.mult, reads=["Osb0", "tmpf0"], writes=["tmpf2"])
        _tt(P, "pool", tmpf[3][0:64, 0:n], Osb[1][0:64, 0:n], tmpf[1][0:64, 0:n], ALU.mult, reads=["Osb1", "tmpf1"], writes=["tmpf3"])
        _stt(P, "dve", tmpf[2][0:64, 0:n], tmpf[3][0:64, 0:n], lamt[0:64, 4:5], tmpf[2][0:64, 0:n], ALU.mult, ALU.add,
             reads=["tmpf3", "tmpf2", "lamt"], writes=["tmpf2"])
        _act(P, tmpf[4][0:64, 0:n], tmpf[2][0:64, 0:n], AF.Square, reads=["tmpf2"], writes=["tmpf4"])
        _mm(P, ps[6][0:64, 0:n], lhsT=ones64[:], rhs=tmpf[4][0:64, 0:n], reads=["tmpf4", "ones64"], writes=["ps6"])
        _act(P, tmpf[4][0:64, 0:n], ps[6][0:64, 0:n], AF.Sqrt, scale=1.0 / 64.0, bias=epsb[0:64, 0:1], reads=["ps6"], writes=["tmpf4"])
        P.op("dve", lambda e: e.reciprocal(out=tmpf[5][0:64, 0:n], in_=tmpf[4][0:64, 0:n]), reads=["tmpf4"], writes=["tmpf5"])
        _stt(P, "dve", tmpb[0][0:64, 0:n], tmpf[2][0:64, 0:n], lamt[0:64, 5:6], tmpf[5][0:64, 0:n], ALU.mult, ALU.mult,
             reads=["tmpf2", "tmpf5", "lamt"], writes=["tmpb0"])
        P.dma("sp", out_dram, tmpb[0][0:64, 0:n], reads=["tmpb0"])

    tq_all = [("TQ", j, h) for j in range(NCH) for h in range(2)]
    tk_all = [("TK", j, h) for j in range(NCH) for h in range(2)] + [("TK", "c")]
    v_all = [("VD", j) for j in range(NCH)] + [("VB", j) for j in range(NCH)] + [("VD", "c"), ("VB", "c"), "VDones", "VBones"]

    sc_b = 64 ** -0.5
    for r8 in range(int(os.environ.get('NA_R8', 16))):
        pO = ps[4 + (r8 % 2)]; pOk = f"ps{4 + r8 % 2}"
        for rr in range(8):
            r = r8 * 8 + rr
            w0 = min(max(r - 4, 0), 120)
            dr0 = w0 - r + 7
            pw = ps[rr % 2]; pwk = f"ps{rr % 2}"
            pc = ps[2 + (rr % 2)]; pck = f"ps{2 + rr % 2}"
            qap = TQ[64:128, r * 64:(r + 1) * 64]
            for jw in range(8):
                w = w0 + jw
                _mm(P, pw[0:64, jw * 64:(jw + 1) * 64], lhsT=TK[64:128, w * 64:(w + 1) * 64], rhs=qap, writes=[pwk])
            for t in range(2):
                _mm(P, pc[:, t * 64:(t + 1) * 64], lhsT=TK[64:128, S + t * 128:S + (t + 1) * 128], rhs=qap, writes=[pck])
            bt = nab[0:64, 0, dr0:dr0 + 8, :]
            tf = tmpf[rr % 2]; tfk = f"tmpf{rr % 2}"
            _stt(P, "dve", tf[0:64, :].rearrange("p (a b) -> p a b", b=64), pw[0:64, :].rearrange("p (a b) -> p a b", b=64),
                 sc_b, bt, ALU.mult, ALU.add, reads=[pwk], writes=[tfk])
            pt = tmpb[rr % 2]; ptk = f"tmpb{rr % 2}"
            ptc = tmpb[2 + rr % 2]; ptck = f"tmpb{2 + rr % 2}"
            _act(P, pt[0:64, :], tf[0:64, :], AF.Exp, reads=[tfk], writes=[ptk])
            _act(P, ptc[:, 0:128], pc[:, 0:128], AF.Exp, scale=sc_b, reads=[pck], writes=[ptck])
            for jw in range(8):
                w = w0 + jw
                vsrc = VB if w % 2 == 0 else VBo
                _mm(P, pO[0:65, rr * 64:(rr + 1) * 64], lhsT=vsrc[0:64, w // 2, :],
                    rhs=pt[0:64, jw * 64:(jw + 1) * 64], start=(jw == 0), stop=False, reads=[ptk], writes=[pOk])
            for t in range(2):
                _mm(P, pO[0:65, rr * 64:(rr + 1) * 64], lhsT=VB[:, 64 + t, :], rhs=ptc[:, t * 64:(t + 1) * 64],
                    start=False, stop=(t == 1), reads=[ptck], writes=[pOk])
        Ot = Osb[r8 % 2]; okey = f"Osb{r8 % 2}"
        _cp(P, "dve", Ot[:, :], pO[0:65, :], reads=[pOk], writes=[okey])
        if os.environ.get('NA_EPI', '1') == '0':
            continue
        lbcast_recip(Ot, okey, 512, tmpf[2], "tmpf2", ps[6 + r8 % 2], f"ps{6 + r8 % 2}")
        ob = obuf[r8 % 2]; obk = f"obuf{r8 % 2}"
        _tt(P, "pool", ob[0:64, :], Ot[0:64, :], tmpf[2][0:64, :], ALU.mult, reads=[okey, "tmpf2"], writes=[obk])
        P.dma("sp", yb_d[:, r8 * 512:(r8 + 1) * 512], ob[0:64, :], reads=[obk])
    if ctx_br and os.environ.get('NA_CTX', '1') == '1':
        dense_attn((TQc[64:128, :], []), lambda kt: (TK[64:128, S + kt * 128:S + (kt + 1) * 128], []),
                   lambda kt: (VB[:, 64 + kt, :], []), 2, L, sc_b, Osb[0], "Osb0", ps[4], "ps4", [(ps[0], "ps0"), (ps[1], "ps1")])
        lbcast_recip(Osb[0], "Osb0", L, tmpf[2], "tmpf2", ps[6], "ps6")
        _tt(P, "pool", tmpb[2][0:64, 0:L], Osb[0][0:64, 0:L], tmpf[2][0:64, 0:L], ALU.mult, reads=["Osb0", "tmpf2"], writes=["tmpb2"])
        P.dma("sp", yctx_d[1, :, :], tmpb[2][0:64, 0:L], reads=["tmpb2"])
    _barrier(P)

    if stop == 4:
        P.emit(); return nc
    sc_d = 32 ** -0.5
    for qg in range(16):
        for mI in range(2):
            rows = slice(mI * 32, mI * 32 + 32)
            dense_attn((TQ[rows, qg * 512:(qg + 1) * 512], []),
                       lambda kt, rows=rows: (TK[rows, kt * 128:(kt + 1) * 128], []),
                       lambda kt: (VD[:, kt, :], []), 66, 512, sc_d, Osb[mI], f"Osb{mI}", ps[4 + mI], f"ps{4 + mI}",
                       [(ps[0], "ps0"), (ps[1], "ps1"), (ps[2], "ps2"), (ps[3], "ps3")])
        diff_combine(512, yd_d[:, qg * 512:(qg + 1) * 512])
    if ctx_br:
        for mI in range(2):
            rows = slice(mI * 32, mI * 32 + 32)
            dense_attn((TQc[rows, :], []), lambda kt, rows=rows: (TK[rows, S + kt * 128:S + (kt + 1) * 128], []),
                       lambda kt: (VD[:, 64 + kt, :], []), 2, L, sc_d, Osb[mI], f"Osb{mI}", ps[4 + mI], f"ps{4 + mI}",
                       [(ps[0], "ps0"), (ps[1], "ps1")])
        diff_combine(L, yctx_d[3, :, :])
    P.emit()
    return nc


import math
import numpy as np
import ml_dtypes

BFNP = ml_dtypes.bfloat16
EPS = 1e-6
NE = 256


def prep_B(inp, layer, x_cur, xc_cur, brT, brcT, last):
    maps = []
    for core in range(8):
        b, q = core // 4, core % 4
        m = {}
        xs = x_cur[b, q * 2048:(q + 1) * 2048].T
        bs = brT[b][:, q * 2048:(q + 1) * 2048]
        if not last:
            xs = np.concatenate([xs, xc_cur[b, q * 64:(q + 1) * 64].T], 1)
            bs = np.concatenate([bs, brcT[b][:, q * 64:(q + 1) * 64]], 1)
        m["xT"] = np.ascontiguousarray(xs, dtype=np.float32)
        m["brT"] = np.ascontiguousarray(bs)
        cv = np.stack([inp["c"][b], inp["c_ctx"]], 0)
        m["cvec"] = np.ascontiguousarray(cv.reshape(2, 8, 128).transpose(2, 0, 1).reshape(128, 16))
        m["adaw"] = inp["ada_w"][layer]
        m["adab"] = np.ascontiguousarray(inp["ada_b"][layer].reshape(48, 128).T)
        m["g1"] = np.ascontiguousarray(inp["norm1_g"][layer].reshape(8, 128).T)
        m["g2"] = np.ascontiguousarray(inp["norm2_g"][layer].reshape(8, 128).T)
        m["gf"] = np.ascontiguousarray(inp["final_norm_g"].reshape(8, 128).T)
        m["wgate"] = inp["w_branch_gate"][layer]
        m["wbr"] = inp["w_branch"][layer]
        m["wout"] = inp["w_out"][layer]
        m["wrt"] = inp["router_w"][layer]
        m["rbias"] = np.ascontiguousarray(np.broadcast_to(inp["router_bias"][layer][None, :], (128, NE)))
        m["ident"] = np.eye(128, dtype=np.float32).astype(BFNP)
        m["identf"] = np.eye(128, dtype=np.float32)
        m["ones"] = np.ones((128, 128), np.float32).astype(BFNP)
        maps.append(m)
    return maps


def build_B1(layer, last):
    import os
    T = 2048 if last else 2112
    NT = T // 128 if last else 17
    chunks = [(i * 512, 512, 0) for i in range(4)] + ([] if last else [(2048, 64, 1)])
    nc = bass.Bass("TRN2", target_bir_lowering=False)
    P = Prog(nc)

    def din(name, shape, dt=F32):
        return nc.dram_tensor(name, list(shape), dt, kind="ExternalInput").ap()

    xT = din("xT", [1024, T]); brT = din("brT", [1024, T], BF16)
    cvec_d = din("cvec", [128, 16]); adaw_d = din("adaw", [1024, 6144]); adab_d = din("adab", [128, 48])
    g1_d = din("g1", [128, 8]); g2_d = din("g2", [128, 8]); gf_d = din("gf", [128, 8])
    wgate_d = din("wgate", [4, 1024, 1024]); wbr_d = din("wbr", [4, 256, 1024]); wout_d = din("wout", [1024, 1024])
    wrt_d = din("wrt", [1024, NE]); rbias_d = din("rbias", [128, NE])
    ident_d = din("ident", [128, 128], BF16); identf_d = din("identf", [128, 128]); ones_d = din("ones", [128, 128], BF16)
    xmid_d = nc.dram_tensor("xmidT", [1024, T], F32, kind="ExternalOutput").ap()
    h2o_d = nc.dram_tensor("h2T", [1024, T], BF16, kind="ExternalOutput").ap()
    wro_d = nc.dram_tensor("wr", [NT * 128, NE], F32, kind="ExternalOutput").ap()
    modo_d = nc.dram_tensor("modvo", [128, 96], F32, kind="ExternalOutput").ap()

    H2 = P.sb("H2", [128, 8, T], BF16)
    Wr = P.sb("Wr", [128, NT, 260], F32)
    ident = P.sb("ident_s", [128, 128], BF16); identf = P.sb("identf_s", [128, 128], F32); ones = P.sb("ones_s", [128, 128], BF16)
    cvec = P.sb("cvec_s", [128, 16], F32); scv = P.sb("scv", [128, 16], F32)
    adab = P.sb("adab_s", [128, 48], F32)
    g1 = P.sb("g1_s", [128, 8], F32); g2 = P.sb("g2_s", [128, 8], F32); gf = P.sb("gf_s", [128, 8], F32)
    modv = P.sb("modv", [128, 2, 48], F32)
    Am1 = P.sb("Am1", [128, 2, 8], F32); Am2 = P.sb("Am2", [128, 2, 8], F32)
    epsb = P.sb("epsb", [128, 1], F32); zerob = P.sb("zerob", [128, 1], F32)
    rbias = P.sb("rbias_s", [128, NE], F32)
    wrt = P.sb("wrt_s", [128, 8, NE], F32)
    tmpf = [P.sb(f"tmpf{i}", [128, 512], F32) for i in range(5)]
    tmpb = [P.sb(f"tmpb{i}", [128, 512], BF16) for i in range(4)]
    rt = P.sb("rt", [128, 8, 8], F32); rs = P.sb("rs", [128, 64], F32)
    AR = P.sb("AR", [128, 56 * 1024], BF16)
    ps = [P.ps(f"ps{i}", [128, 512], F32) for i in range(8)]

    def carve(off_kb, nbytes, dt):
        a = AR[:, off_kb * 512: off_kb * 512 + nbytes // 2]
        return a if dt == BF16 else a.bitcast(F32)

    xc_ = carve(0, 16384, F32).rearrange("p (k t) -> p k t", k=8)
    sq = carve(16, 8192, BF16).rearrange("p (k t) -> p k t", k=8)
    hh = carve(24, 8192, BF16).rearrange("p (k t) -> p k t", k=8)
    brc = carve(32, 8192, BF16).rearrange("p (k t) -> p k t", k=8)
    macc = carve(40, 16384, F32).rearrange("p (k t) -> p k t", k=8)
    mrg = carve(56, 8192, BF16).rearrange("p (k t) -> p k t", k=8)
    wg = carve(64, 16384, BF16).rearrange("p (k c) -> p k c", k=8)
    wb = carve(80, 16384, BF16).rearrange("p (k c) -> p k c", k=8)
    stg = carve(96, 16384, F32).rearrange("p (k c) -> p k c", k=8)
    yacc = carve(0, NT * 4096, F32).rearrange("p (t f) -> p t f", t=NT)
    wG = carve(72, 8192, BF16).rearrange("p (k c) -> p k c", k=8)
    wU = carve(80, 8192, BF16).rearrange("p (k c) -> p k c", k=8)
    wD = carve(88, 8192, BF16).rearrange("p (c f) -> p c f", c=4)
    sgb = carve(96, 1024, BF16); hmb = carve(97, 1024, BF16)
    hmT = [carve(98 + i, 1024, BF16) for i in range(2)]
    xo = carve(72, 16384, F32).rearrange("p (k t) -> p k t", k=8)
    fo = carve(88, 16384, F32).rearrange("p (k t) -> p k t", k=8)
    sq2 = carve(104, 8192, BF16).rearrange("p (k t) -> p k t", k=8)

    for (dst, src, key) in [(ident, ident_d, "ident"), (identf, identf_d, "identf"), (ones, ones_d, "ones"), (cvec, cvec_d, "cvec"),
                            (adab, adab_d, "adab"), (g1, g1_d, "g1"), (g2, g2_d, "g2"), (gf, gf_d, "gf"), (rbias, rbias_d, "rbias")]:
        P.dma("sp", dst[:], src, writes=[key])
    P.dma("sp", wrt[:], wrt_d.rearrange("(k p) e -> p k e", p=128), writes=["wrt"])
    P.op("pool", lambda e: e.memset(epsb[:], EPS), writes=["epsb"])
    P.op("pool", lambda e: e.memset(zerob[:], 0.0), writes=["zerob"])
    P.op("pool", lambda e: e.memset(Wr[:], 0.0), writes=[("Wr", t_) for t_ in range(NT)])
    _act(P, scv[:], cvec[:], AF.Silu, reads=["cvec"], writes=["scv"])
    adv = adaw_d.rearrange("(k p) c -> p k c", p=128)
    for q12 in range(12):
        P.dma("sp", stg[:], adv[:, :, q12 * 512:(q12 + 1) * 512], writes=["stg"])
        for jj in range(4):
            j = q12 * 4 + jj
            for k in range(8):
                _mm(P, ps[2][:, 2 * j:2 * j + 2], lhsT=stg[:, k, jj * 128:(jj + 1) * 128], rhs=scv[:, k::8],
                    start=(k == 0), stop=(k == 7), reads=["stg", "scv"], writes=["ps2"])
    for v in range(2):
        _tt(P, "dve", modv[:, v, :], ps[2][:, v:96:2], adab[:], ALU.add, reads=["ps2", "adab"], writes=["modv"])
        _stt(P, "dve", Am1[:, v, :], modv[:, v, 8:16], 1.0, g1[:], ALU.add, ALU.mult, reads=["modv", "g1"], writes=["Am1"])
        _stt(P, "dve", Am2[:, v, :], modv[:, v, 32:40], 1.0, g2[:], ALU.add, ALU.mult, reads=["modv", "g2"], writes=["Am2"])
    for i in range(4):
        P.dma("pool", wb[:, 2 * i:2 * i + 2, :], wbr_d[i].rearrange("(h p) f -> p h f", p=128), writes=["wb"])
    _barrier(P)

    def norm_chunk(src, n, A_ap, B_ap, dst_bf=None, dst_f32=None, sqb=None, fkey="macc"):
        sqb = sq if sqb is None else sqb
        _act(P, sqb[:, :, 0:n], src[:, :, 0:n], AF.Square, reads=["x"], writes=["sq"])
        for k in range(8):
            _mm(P, ps[0][:, 0:n], lhsT=ones[:], rhs=sqb[:, k, 0:n], start=(k == 0), stop=(k == 7), reads=["sq", "ones"], writes=["ps0"])
        _act(P, tmpf[0][:, 0:n], ps[0][:, 0:n], AF.Sqrt, scale=1.0 / 1024.0, bias=epsb[:, 0:1], reads=["ps0"], writes=["tmpf0"])
        P.op("dve", lambda e: e.reciprocal(out=tmpf[1][:, 0:n], in_=tmpf[0][:, 0:n]), reads=["tmpf0"], writes=["rstd"])
        for k in range(8):
            t = tmpf[2 + (k % 2)]; tk = f"tmpf{2 + k % 2}"
            _stt(P, "dve", t[:, 0:n], src[:, k, 0:n], A_ap(k), tmpf[1][:, 0:n], ALU.mult, ALU.mult, reads=["x", "rstd"], writes=[tk])
            if dst_f32 is not None:
                _act(P, dst_f32[:, k, 0:n], t[:, 0:n], AF.Identity, bias=B_ap(k), reads=[tk], writes=[(fkey, k)])
                if dst_bf is not None:
                    _cp(P, "pool", dst_bf(k), dst_f32[:, k, 0:n], reads=[(fkey, k)], writes=[("h", k)])
            else:
                _act(P, dst_bf(k), t[:, 0:n], AF.Identity, bias=B_ap(k), reads=[tk], writes=[("h", k)])

    xv = xT.rearrange("(k p) t -> p k t", p=128)
    bv = brT.rearrange("(k p) t -> p k t", p=128)
    xmv = xmid_d.rearrange("(k p) t -> p k t", p=128)
    wov = wout_d.rearrange("(k p) f -> p k f", p=128)
    hkeys = [("h", k) for k in range(8)]
    for (c0, n, vec) in chunks:
        P.dma("sp", xc_[:, :, 0:n], xv[:, :, c0:c0 + n], writes=["x"])
        P.dma("sp", brc[:, :, 0:n], bv[:, :, c0:c0 + n], writes=["br"])
        norm_chunk(xc_, n, lambda k: Am1[:, vec, k:k + 1], lambda k: modv[:, vec, k:k + 1], lambda k: hh[:, k, 0:n])
        for i in range(4):
            P.dma("pool", wg[:], wgate_d[i].rearrange("(k p) f -> p k f", p=128), reads=[], writes=["wg"])
            for oc in range(8):
                pg = ps[1 + (oc % 2)]; pgk = f"ps{1 + oc % 2}"
                pp = ps[3 + (oc % 2)]; ppk = f"ps{3 + oc % 2}"
                for k in range(8):
                    _mm(P, pg[:, 0:n], lhsT=wg[:, k, oc * 128:(oc + 1) * 128], rhs=hh[:, k, 0:n], start=(k == 0), stop=(k == 7),
                        reads=hkeys + ["wg"], writes=[pgk])
                gt = tmpb[oc % 2]; gk = f"tmpb{oc % 2}"
                _act(P, gt[:, 0:n], pg[:, 0:n], AF.Sigmoid, reads=[pgk], writes=[gk])
                for h in range(2):
                    _mm(P, pp[:, 0:n], lhsT=wb[:, 2 * i + h, oc * 128:(oc + 1) * 128], rhs=brc[:, 2 * i + h, 0:n], start=(h == 0), stop=(h == 1),
                        reads=["br", "wb"], writes=[ppk])
                if i == 0:
                    _tt(P, "dve", macc[:, oc, 0:n], pp[:, 0:n], gt[:, 0:n], ALU.mult, reads=[ppk, gk], writes=[("macc", oc)])
                else:
                    t = tmpf[4]
                    _tt(P, "dve", t[:, 0:n], pp[:, 0:n], gt[:, 0:n], ALU.mult, reads=[ppk, gk], writes=["tmpf4"])
                    _tt(P, "pool", macc[:, oc, 0:n], macc[:, oc, 0:n], t[:, 0:n], ALU.add, reads=["tmpf4"], writes=[("macc", oc)])
        for oc in range(8):
            _cp(P, "act", mrg[:, oc, 0:n], macc[:, oc, 0:n], reads=[("macc", oc)], writes=[("mrg", oc)])
        P.dma("pool", wg[:], wov, writes=["wg"])
        mkeys = [("mrg", k) for k in range(8)]
        for oc in range(8):
            po = ps[5 + (oc % 2)]; pok = f"ps{5 + oc % 2}"
            for k in range(8):
                _mm(P, po[:, 0:n], lhsT=wg[:, k, oc * 128:(oc + 1) * 128], rhs=mrg[:, k, 0:n], start=(k == 0), stop=(k == 7),
                    reads=mkeys + ["wg"], writes=[pok])
            _stt(P, "dve", xc_[:, oc, 0:n], po[:, 0:n], modv[:, vec, 16 + oc:17 + oc], xc_[:, oc, 0:n], ALU.mult, ALU.add,
                 reads=[pok, "x"], writes=["x"])
        P.dma("sp", xmv[:, :, c0:c0 + n], xc_[:, :, 0:n], reads=["x"])
        norm_chunk(xc_, n, lambda k: Am2[:, vec, k:k + 1], lambda k: modv[:, vec, 24 + k:25 + k],
                   lambda k: H2[:, k, c0:c0 + n], dst_f32=macc)
        for tt_ in range((n + 127) // 128):
            tn = min(128, n - tt_ * 128)
            ti = c0 // 128 + tt_
            pr = ps[7]
            for k in range(8):
                _mm(P, pr[0:tn, 0:NE], lhsT=macc[:, k, tt_ * 128:tt_ * 128 + tn], rhs=wrt[:, k, :], start=(k == 0), stop=(k == 7),
                    reads=[("macc", kk) for kk in range(8)] + ["wrt"], writes=["ps7"])
            sc = tmpf[0][:, 0:NE]; sbv = tmpf[1][:, 0:NE]; ch = tmpf[2][:, 0:NE]
            _act(P, sc[0:tn], pr[0:tn, 0:NE], AF.Sigmoid, reads=["ps7"], writes=["tmpf0"])
            _tt(P, "dve", sbv[0:tn], sc[0:tn], rbias[0:tn, :], ALU.add, reads=["tmpf0", "rbias"], writes=["rstd"])
            for g in range(8):
                P.op("dve", lambda e, g=g, tn=tn, sbv=sbv: e.max(out=rt[0:tn, g, :], in_=sbv[0:tn, g * 32:(g + 1) * 32]), reads=["rstd"], writes=["rt"])
            _tt(P, "dve", rs[0:tn, 0:8], rt[0:tn, :, 0], rt[0:tn, :, 1], ALU.add, reads=["rt"], writes=["rs"])
            P.op("dve", lambda e, tn=tn: e.max(out=rs[0:tn, 8:16], in_=rs[0:tn, 0:8]), reads=["rs"], writes=["rs"])
            _ts(P, "dve", rs[0:tn, 16:24], rs[0:tn, 0:8], rs[0:tn, 11:12], None, ALU.is_ge, reads=["rs"], writes=["rs"])
            _ts(P, "dve", rs[0:tn, 24:32], rs[0:tn, 16:24], 1.0, 1.0e9, ALU.subtract, ALU.mult, reads=["rs"], writes=["rs"])
            gm = rs[0:tn, 16:24].unsqueeze(2).broadcast_to([tn, 8, 32])
            pen = rs[0:tn, 24:32].unsqueeze(2).broadcast_to([tn, 8, 32])
            v3 = lambda a: a.rearrange("p (g e) -> p g e", e=32)
            _tt(P, "dve", v3(ch[0:tn]), v3(sbv[0:tn]), gm, ALU.mult, reads=["rstd", "rs"], writes=["tmpf2"])
            _tt(P, "dve", v3(ch[0:tn]), v3(ch[0:tn]), pen, ALU.add, reads=["tmpf2", "rs"], writes=["tmpf2"])
            P.op("dve", lambda e, tn=tn, ch=ch: e.max(out=rs[0:tn, 32:40], in_=ch[0:tn]), reads=["tmpf2"], writes=["rs"])
            _ts(P, "dve", ch[0:tn], ch[0:tn], rs[0:tn, 39:40], None, ALU.is_ge, reads=["tmpf2", "rs"], writes=["tmpf2"])
            _tt(P, "dve", sc[0:tn], sc[0:tn], ch[0:tn], ALU.mult, reads=["tmpf0", "tmpf2"], writes=["tmpf0"])
            P.op("dve", lambda e, tn=tn, sc=sc: e.reduce_sum(out=rs[0:tn, 40:41], in_=sc[0:tn], axis=AX.X), reads=["tmpf0"], writes=["rs"])
            P.op("dve", lambda e, tn=tn: e.reciprocal(out=rs[0:tn, 41:42], in_=rs[0:tn, 40:41]), reads=["rs"], writes=["rs"])
            _ts(P, "dve", Wr[0:tn, ti, 0:NE], sc[0:tn], rs[0:tn, 41:42], 2.5, ALU.mult, ALU.mult, reads=["tmpf0", "rs"], writes=[("Wr", ti)])
    P.dma("sp", h2o_d.rearrange("(k p) t -> p k t", p=128), H2[:], reads=[("h", k) for k in range(8)])
    P.dma("sp", wro_d.rearrange("(t p) e -> p t e", p=128), Wr[:, :, 0:NE], reads=[("Wr", t_) for t_ in range(NT)])
    P.dma("sp", modo_d, modv[:].rearrange("p v j -> p (v j)"), reads=["modv"])
    P.emit()
    return nc


def build_B2(last, NG=8):
    NT = 16 if last else 17
    TG = NT * 128
    TA = NG * TG
    nc = bass.Bass("TRN2", target_bir_lowering=False)
    P = Prog(nc)

    def din(name, shape, dt=F32):
        return nc.dram_tensor(name, list(shape), dt, kind="ExternalInput").ap()

    h2_d = din("h2a", [1024, TA], BF16); wr_d = din("wra", [TA, 33])
    ewg_d = din("ewg", [33, 1024, 256]); ewu_d = din("ewu", [33, 1024, 256]); ewd_d = din("ewd", [33, 256, 1024])
    ident_d = din("ident", [128, 128], BF16)
    yp_d = nc.dram_tensor("ypart", [TA, 1024], F32, kind="ExternalOutput").ap()
    H2 = P.sb("H2", [128, 8, TG], BF16)
    Wr = P.sb("Wr", [128, NT, 33], F32)
    ident = P.sb("ident_s", [128, 128], BF16)
    yacc = P.sb("yacc", [128, NT, 1024], F32)
    wG = [P.sb(f"wG{i}", [128, 8, 512], BF16) for i in range(2)]
    wU = [P.sb(f"wU{i}", [128, 8, 512], BF16) for i in range(2)]
    wD = [P.sb(f"wD{i}", [128, 4, 1024], BF16) for i in range(2)]
    sgb = [P.sb(f"sgb{i}", [128, 512], BF16) for i in range(2)]
    hmb = [P.sb(f"hmb{i}", [128, 512], BF16) for i in range(2)]
    hmT = [P.sb(f"hmT{i}", [128, 512], BF16) for i in range(2)]
    ps = [P.ps(f"ps{i}", [128, 512], F32) for i in range(8)]
    P.dma("sp", ident[:], ident_d, writes=["ident"])
    units = [(2 * p_, 2) for p_ in range(16)] + [(32, 1)]
    ewg_v = ewg_d.rearrange("e (k p) h -> e p k h", p=128)
    ewu_v = ewu_d.rearrange("e (k p) h -> e p k h", p=128)
    ewd_v = ewd_d.rearrange("e (c p) f -> e p c f", p=128)
    h2v = h2_d.rearrange("(k p) t -> p k t", p=128)
    wrv = wr_d.rearrange("(t p) e -> p t e", p=128)
    ypv = yp_d.rearrange("(t p) f -> p t f", p=128)
    ucnt = 0
    icnt = 0
    for gi in range(NG):
        P.dma("sp", H2[:], h2v[:, :, gi * TG:(gi + 1) * TG], writes=["H2"])
        P.dma("sp", Wr[:], wrv[:, gi * NT:(gi + 1) * NT, :], writes=["Wr"])
        P.op("pool", lambda e: e.memset(yacc[:], 0.0), writes=["yacc"] + [("yacc", t, h) for t in range(NT) for h in range(2)])
        items = []
        for (e0, ne) in units:
            wb_ = ucnt % 2
            ucnt += 1
            for t in range(NT):
                items.append((e0, ne, wb_, t, t == 0, icnt % 2))
                icnt += 1

        def s1(it):
            e0, ne, wb_, t, first, par = it
            W = ne * 256
            if first:
                for j in range(ne):
                    P.dma("pool", wG[wb_][:, :, j * 256:(j + 1) * 256], ewg_v[e0 + j], writes=[f"wG{wb_}"])
                    P.dma("pool", wU[wb_][:, :, j * 256:(j + 1) * 256], ewu_v[e0 + j], writes=[f"wU{wb_}"])
                    P.dma("pool", wD[wb_][:, 2 * j:2 * j + 2, :], ewd_v[e0 + j], writes=[f"wD{wb_}"])
            pG, pGk = ps[par * 2], f"ps{par * 2}"
            pU, pUk = ps[par * 2 + 1], f"ps{par * 2 + 1}"
            for k in range(8):
                _mm(P, pG[:, 0:W], lhsT=H2[:, k, t * 128:(t + 1) * 128], rhs=wG[wb_][:, k, 0:W], start=(k == 0), stop=(k == 7),
                    reads=[f"wG{wb_}", "H2"], writes=[pGk])
            for k in range(8):
                _mm(P, pU[:, 0:W], lhsT=H2[:, k, t * 128:(t + 1) * 128], rhs=wU[wb_][:, k, 0:W], start=(k == 0), stop=(k == 7),
                    reads=[f"wU{wb_}", "H2"], writes=[pUk])
            _act(P, sgb[par][:, 0:W], pG[:, 0:W], AF.Silu, reads=[pGk], writes=[f"sgb{par}"])
            for j in range(ne):
                _stt(P, "dve", hmb[par][:, j * 256:(j + 1) * 256], pU[:, j * 256:(j + 1) * 256], Wr[:, t, e0 + j:e0 + j + 1],
                     sgb[par][:, j * 256:(j + 1) * 256], ALU.mult, ALU.mult, reads=[pUk, f"sgb{par}", "Wr"], writes=[f"hmb{par}"])

        def s2(it):
            e0, ne, wb_, t, first, par = it
            pT = ps[4 + par][:].bitcast(BF16); pTk = f"ps{4 + par}"
            for c in range(2 * ne):
                _tr(P, pT[:, c * 128:(c + 1) * 128], hmb[par][:, c * 128:(c + 1) * 128], ident[:], reads=[f"hmb{par}", "ident"], writes=[pTk])
            _cp(P, "act", hmT[par][:, 0:2 * ne * 128], pT[:, 0:2 * ne * 128], reads=[pTk], writes=[f"hmT{par}"])

        def s3(it):
            e0, ne, wb_, t, first, par = it
            for half in range(2):
                py = ps[6 + half]; pyk = f"ps{6 + half}"
                for c in range(2 * ne):
                    _mm(P, py[:, :], lhsT=hmT[par][:, c * 128:(c + 1) * 128], rhs=wD[wb_][:, c, half * 512:(half + 1) * 512],
                        start=(c == 0), stop=(c == 2 * ne - 1), reads=[f"hmT{par}", f"wD{wb_}"], writes=[pyk])
                _tt(P, "dve", yacc[:, t, half * 512:(half + 1) * 512], yacc[:, t, half * 512:(half + 1) * 512], py[:, :], ALU.add,
                    reads=[pyk], writes=[("yacc", t, half)])

        n_it = len(items)
        for idx in range(n_it + 2):
            if idx < n_it:
                s1(items[idx])
            if 0 <= idx - 1 < n_it:
                s2(items[idx - 1])
            if 0 <= idx - 2 < n_it:
                s3(items[idx - 2])
        P.dma("sp", ypv[:, gi * NT:(gi + 1) * NT, :], yacc[:], reads=[("yacc", t, h) for t in range(NT) for h in range(2)])
    P.emit()
    return nc


def build_B3(last):
    T = 2048 if last else 2112
    chunks = [(i * 512, 512, 0) for i in range(4)] + ([] if last else [(2048, 64, 1)])
    nc = bass.Bass("TRN2", target_bir_lowering=False)
    P = Prog(nc)

    def din(name, shape, dt=F32):
        return nc.dram_tensor(name, list(shape), dt, kind="ExternalInput").ap()

    yp_d = din("yp8", [8, T, 1024]); xm_d = din("xmi", [1024, T]); mod_d = din("modvi", [128, 96]); gf_d = din("gf", [128, 8])
    identf_d = din("identf", [128, 128]); ones_d = din("ones", [128, 128], BF16)
    out_d = nc.dram_tensor("outT", [1024, T], F32, kind="ExternalOutput").ap()
    modv = P.sb("modv", [128, 2, 48], F32); gf = P.sb("gf_s", [128, 8], F32)
    identf = P.sb("identf_s", [128, 128], F32); ones = P.sb("ones_s", [128, 128], BF16)
    epsb = P.sb("epsb", [128, 1], F32); zerob = P.sb("zerob", [128, 1], F32)
    ysum = [P.sb(f"ysum{i}", [128, 8, 1024], F32) for i in range(2)]
    xo = P.sb("xo", [128, 8, 512], F32); fo = P.sb("fo", [128, 8, 512], F32); sq = P.sb("sq", [128, 8, 512], BF16)
    ytile = P.sb("ytile", [128, 4, 1024], F32)
    tmpf = [P.sb(f"tmpf{i}", [128, 512], F32) for i in range(5)]
    ps = [P.ps(f"ps{i}", [128, 512], F32) for i in range(8)]
    P.dma("sp", modv[:].rearrange("p v j -> p (v j)"), mod_d, writes=["modv"])
    P.dma("sp", gf[:], gf_d, writes=["gf"]); P.dma("sp", identf[:], identf_d, writes=["identf"]); P.dma("sp", ones[:], ones_d, writes=["ones"])
    P.op("pool", lambda e: e.memset(epsb[:], EPS), writes=["epsb"])
    P.op("pool", lambda e: e.memset(zerob[:], 0.0), writes=["zerob"])
    ypv = yp_d.rearrange("c t f -> t c f")
    xmv = xm_d.rearrange("(k p) t -> p k t", p=128)
    outv = out_d.rearrange("(k p) t -> p k t", p=128)
    ti_g = 0
    for (c0, n, vec) in chunks:
        P.dma("sp", xo[:, :, 0:n], xmv[:, :, c0:c0 + n], writes=["x"])
        ntl = (n + 127) // 128
        for tt_ in range(ntl):
            tn = min(128, n - tt_ * 128)
            ys = ysum[ti_g % 2]; ysk = f"ysum{ti_g % 2}"
            ti_g += 1
            P.dma("sp", ys[0:tn], ypv[c0 + tt_ * 128:c0 + tt_ * 128 + tn, :, :], writes=[ysk])
            _tt(P, "dve", ys[0:tn, 0:4, :], ys[0:tn, 0:4, :], ys[0:tn, 4:8, :], ALU.add, reads=[ysk], writes=[ysk])
            _tt(P, "pool", ys[0:tn, 0:2, :], ys[0:tn, 0:2, :], ys[0:tn, 2:4, :], ALU.add, reads=[ysk], writes=[ysk])
            _tt(P, "dve", ytile[0:tn, tt_, :], ys[0:tn, 0, :], ys[0:tn, 1, :], ALU.add, reads=[ysk], writes=[("yt", tt_)])
        for oc in range(8):
            py = ps[oc % 2]; pyk = f"ps{oc % 2}"
            for tt_ in range(ntl):
                tn = min(128, n - tt_ * 128)
                _tr(P, py[:, tt_ * 128:tt_ * 128 + tn], ytile[0:tn, tt_, oc * 128:(oc + 1) * 128], identf[0:tn, 0:tn],
                    reads=[("yt", tt_), "identf"], writes=[pyk])
            _stt(P, "dve", xo[:, oc, 0:n], py[:, 0:n], modv[:, vec, 40 + oc:41 + oc], xo[:, oc, 0:n], ALU.mult, ALU.add,
                 reads=[pyk, "x", "modv"], writes=["x"])
        if last:
            _act(P, sq[:, :, 0:n], xo[:, :, 0:n], AF.Square, reads=["x"], writes=["sq"])
            for k in range(8):
                _mm(P, ps[2][:, 0:n], lhsT=ones[:], rhs=sq[:, k, 0:n], start=(k == 0), stop=(k == 7), reads=["sq", "ones"], writes=["ps2"])
            _act(P, tmpf[0][:, 0:n], ps[2][:, 0:n], AF.Sqrt, scale=1.0 / 1024.0, bias=epsb[:, 0:1], reads=["ps2", "epsb"], writes=["tmpf0"])
            P.op("dve", lambda e, n=n: e.reciprocal(out=tmpf[1][:, 0:n], in_=tmpf[0][:, 0:n]), reads=["tmpf0"], writes=["rstd"])
            for k in range(8):
                _stt(P, "dve", fo[:, k, 0:n], xo[:, k, 0:n], gf[:, k:k + 1], tmpf[1][:, 0:n], ALU.mult, ALU.mult, reads=["x", "rstd", "gf"], writes=[("fo", k)])
            P.dma("sp", outv[:, :, c0:c0 + n], fo[:, :, 0:n], reads=[("fo", k) for k in range(8)])
        else:
            P.dma("sp", outv[:, :, c0:c0 + n], xo[:, :, 0:n], reads=["x"])
    P.emit()
    return nc


def _run(nc, maps):
    res = run_bass_kernel_spmd(nc, maps, core_ids=list(range(8)))
    return res.results


def kernel(**inp):
    inp = {k: np.asarray(v) for k, v in inp.items()}
    cst = consts_A()
    x_cur = np.ascontiguousarray(inp["x"], dtype=np.float32)
    xc_cur = np.ascontiguousarray(inp["ctx"], dtype=np.float32)
    ident = np.eye(128, dtype=np.float32).astype(BFNP)
    identf = np.eye(128, dtype=np.float32)
    ones = np.ones((128, 128), np.float32).astype(BFNP)
    for layer in range(2):
        last = layer == 1
        ra = _run(build_A(layer, not last), prep_A(inp, layer, x_cur, xc_cur, cst))
        brT = np.zeros((2, 1024, S), dtype=BFNP)
        brcT = None if last else np.zeros((2, 1024, L), dtype=BFNP)
        for core in range(8):
            b, g = core // 4, core % 4
            r = ra[core]
            ya = np.asarray(r["ya"]).reshape(64, 64, 128).transpose(1, 0, 2).reshape(64, S)
            for i, arr in enumerate([ya, np.asarray(r["ybT"]), np.asarray(r["ycT"]), np.asarray(r["ydT"])]):
                brT[b, i * 256 + 64 * g:i * 256 + 64 * g + 64, :] = arr
            if not last:
                yc = np.asarray(r["yctx"])
                for i in range(4):
                    brcT[b, i * 256 + 64 * g:i * 256 + 64 * g + 64, :] = yc[i]
        del ra
        r1 = _run(build_B1(layer, last), prep_B(inp, layer, x_cur, xc_cur, brT, brcT, last))
        T = 2048 if last else 2112
        NT = 16 if last else 17
        TG = NT * 128
        h2_all = np.zeros((1024, 8 * TG), dtype=BFNP)
        wr_full = np.zeros((8 * TG, NE), dtype=np.float32)
        for tc in range(8):
            h2_all[:, tc * TG:tc * TG + T] = np.asarray(r1[tc]["h2T"])
            wr_full[tc * TG:(tc + 1) * TG] = np.asarray(r1[tc]["wr"])
        xmid = [np.asarray(r1[tc]["xmidT"]) for tc in range(8)]
        modvo = [np.asarray(r1[tc]["modvo"]) for tc in range(8)]
        del r1
        maps = []
        for c in range(8):
            m = {"h2a": h2_all, "ident": ident}
            wra = np.zeros((8 * TG, 33), dtype=np.float32)
            wra[:, 0:32] = wr_full[:, 32 * c:32 * c + 32]
            wra[:, 32] = 1.0 if c == 0 else 0.0
            m["wra"] = wra
            m["ewg"] = np.concatenate([inp["expert_w_gate"][layer][32 * c:32 * c + 32], inp["shared_w_gate"][layer][None]], 0)
            m["ewu"] = np.concatenate([inp["expert_w_up"][layer][32 * c:32 * c + 32], inp["shared_w_up"][layer][None]], 0)
            m["ewd"] = np.concatenate([inp["expert_w_down"][layer][32 * c:32 * c + 32], inp["shared_w_down"][layer][None]], 0)
            maps.append(m)
        r2 = _run(build_B2(last), maps)
        yparts = [np.asarray(r2[c]["ypart"]) for c in range(8)]
        del r2, maps
        maps = []
        gfv = np.ascontiguousarray(inp["final_norm_g"].reshape(8, 128).T)
        for tc in range(8):
            m = {"yp8": np.ascontiguousarray(np.stack([yparts[c][tc * TG:tc * TG + T] for c in range(8)], 0)),
                 "xmi": xmid[tc], "modvi": modvo[tc], "gf": gfv, "identf": identf, "ones": ones}
            maps.append(m)
        r3 = _run(build_B3(last), maps)
        x_new = np.zeros_like(x_cur)
        xc_new = np.zeros_like(xc_cur)
        for tc in range(8):
            b, q = tc // 4, tc % 4
            o = np.asarray(r3[tc]["outT"])
            x_new[b, q * 2048:(q + 1) * 2048] = o[:, 0:2048].T
            if not last:
                xc_new[b, q * 64:(q + 1) * 64] = o[:, 2048:2112].T
        x_cur, xc_cur = x_new, xc_new
        del r3, yparts, maps
    return np.ascontiguousarray(x_cur, dtype=np.float32)
```

```python
import os
import numpy as np
from contextlib import ExitStack
import concourse.bass as bass
import concourse.mybir as mybir
from concourse.bass_utils import run_bass_kernel_spmd

F32 = mybir.dt.float32
BF16 = mybir.dt.bfloat16
AF = mybir.ActivationFunctionType
ALU = mybir.AluOpType
AX = mybir.AxisListType

ENGS = ["pe", "act", "dve", "pool", "sp"]
NDMA = 40


class Prog:
    def __init__(self, nc):
        self.nc = nc
        self.es = ExitStack()
        self.streams = {e: [] for e in ENGS}
        self.cnt = {e: 0 for e in ENGS}
        self.sem = {e: self.es.enter_context(nc.semaphore(f"sem_{e}")) for e in ENGS}
        self.dsem = [self.es.enter_context(nc.semaphore(f"dsem{i}")) for i in range(NDMA)]
        self.dcnt = [0] * NDMA
        self.dnext = 0
        self.waited = {e: {} for e in ENGS}
        self.state = {}
        self.nwaits = 0

    def sb(self, name, shape, dt):
        return self.es.enter_context(self.nc.sbuf_tensor(name, list(shape), dt))

    def ps(self, name, shape, dt=F32):
        return self.es.enter_context(self.nc.psum_tensor(name, list(shape), dt))

    def _st(self, k):
        s = self.state.get(k)
        if s is None:
            s = {"w": None, "r": {}}
            self.state[k] = s
        return s

    def _need(self, eng, tok, waits):
        if tok is None:
            return
        kind, a, v = tok
        if kind == "eng":
            if a == "pe" and eng == "pe":
                return
            key = ("e", a)
        else:
            key = ("d", a)
        if self.waited[eng].get(key, 0) >= v:
            return
        self.waited[eng][key] = v
        waits.append((self.sem[a] if kind == "eng" else self.dsem[a], v))

    def _deps(self, eng, reads, writes):
        waits = []
        for k in reads:
            self._need(eng, self._st(k)["w"], waits)
        for k in writes:
            s = self._st(k)
            self._need(eng, s["w"], waits)
            for t in s["r"].values():
                self._need(eng, t, waits)
        self.nwaits += len(waits)
        return waits

    def _commit(self, tok, reads, writes):
        for k in reads:
            s = self._st(k)
            rk = (tok[0], tok[1])
            s["r"][rk] = tok
        for k in writes:
            s = self._st(k)
            s["w"] = tok
            s["r"] = {}

    def op(self, eng, fn, reads=(), writes=()):
        pr = [k for k in reads if isinstance(k, str) and k.startswith("ps")]
        if pr:
            reads = [k for k in reads if k not in pr]
            writes = list(writes) + pr
        waits = self._deps(eng, reads, writes)
        self.cnt[eng] += 1
        tok = ("eng", eng, self.cnt[eng])
        self._commit(tok, reads, writes)
        self.streams[eng].append((waits, fn, True))

    def dma(self, eng, out, in_, reads=(), writes=(), **kw):
        s = self.dnext
        self.dnext = (self.dnext + 1) % NDMA
        waits = self._deps(eng, reads, writes)
        if self.dcnt[s] > 0:
            self._need(eng, ("dma", s, 16 * self.dcnt[s]), waits)
        self.dcnt[s] += 1
        tok = ("dma", s, 16 * self.dcnt[s])
        self._commit(tok, reads, writes)
        sem = self.dsem[s]

        def fn(e, out=out, in_=in_, kw=kw, sem=sem):
            e.dma_start(out=out, in_=in_, **kw).then_inc(sem, 16)
            return None

        self.streams[eng].append((waits, fn, False))

    def finish(self):
        waits = []
        for s in range(NDMA):
            if self.dcnt[s] > 0:
                self._need("sp", ("dma", s, 16 * self.dcnt[s]), waits)
        for e in ENGS:
            if e != "sp" and self.cnt[e] > 0:
                self._need("sp", ("eng", e, self.cnt[e]), waits)
        self.streams["sp"].append((waits, None, False))

    def emit(self):
        self.finish()
        nc = self.nc
        with nc.Block() as block:
            def mk(name):
                def body(e):
                    sem = self.sem[name]
                    for waits, fn, track in self.streams[name]:
                        for (s, v) in waits:
                            e.wait_ge(s, v)
                        if fn is None:
                            continue
                        ins = fn(e)
                        if track:
                            ins.then_inc(sem, 1)
                return body
            block.tensor(mk("pe"))
            block.scalar(mk("act"))
            block.vector(mk("dve"))
            block.gpsimd(mk("pool"))
            block.sync(mk("sp"))
        self.es.close()


def _mm(P, out, lhsT, rhs, start=True, stop=True, reads=(), writes=()):
    P.op("pe", lambda e: e.matmul(out, lhsT=lhsT, rhs=rhs, start=start, stop=stop), reads, writes)

def _tr(P, out, in_, ident, reads=(), writes=()):
    P.op("pe", lambda e: e.transpose(out, in_, ident), reads, writes)

def _act(P, out, in_, func, reads=(), writes=(), **kw):
    P.op("act", lambda e: e.activation(out=out, in_=in_, func=func, **kw), reads, writes)

def _tt(P, eng, out, in0, in1, op, reads=(), writes=()):
    P.op(eng, lambda e: e.tensor_tensor(out=out, in0=in0, in1=in1, op=op), reads, writes)

def _ts(P, eng, out, in0, s1, s2, op0, op1=None, reads=(), writes=()):
    if op1 is None:
        P.op(eng, lambda e: e.tensor_scalar(out=out, in0=in0, scalar1=s1, scalar2=None, op0=op0), reads, writes)
    else:
        P.op(eng, lambda e: e.tensor_scalar(out=out, in0=in0, scalar1=s1, scalar2=s2, op0=op0, op1=op1), reads, writes)

def _stt(P, eng, out, in0, scalar, in1, op0, op1, reads=(), writes=()):
    P.op(eng, lambda e: e.scalar_tensor_tensor(out=out, in0=in0, scalar=scalar, in1=in1, op0=op0, op1=op1), reads, writes)

def _cp(P, eng, out, in_, reads=(), writes=()):
    if eng == "act":
        P.op("act", lambda e: e.copy(out=out, in_=in_), reads, writes)
    else:
        P.op(eng, lambda e: e.tensor_copy(out=out, in_=in_), reads, writes)

def _barrier(P):
    toks = [("eng", e, P.cnt[e]) for e in ENGS if P.cnt[e] > 0]
    toks += [("dma", s, 16 * P.dcnt[s]) for s in range(NDMA) if P.dcnt[s] > 0]
    for e in ENGS:
        waits = []
        for t in toks:
            P._need(e, t, waits)
        if waits:
            P.streams[e].append((waits, None, False))
    P.state = {}


import math
import numpy as np
import ml_dtypes

BFNP = ml_dtypes.bfloat16
S = 8192
L = 256
EPS = 1e-6
NCH = 16
W_G1, W_G2, W_G3, W_G4, W_CB, W_V, W_GC = 0, 128, 256, 384, 512, 576, 704
WCOLS = 832


def consts_A():
    c = {}
    c["ident"] = np.eye(128, dtype=np.float32).astype(BFNP)
    c["ones"] = np.ones((128, 128), np.float32).astype(BFNP)
    e = np.zeros((65, 64), np.float32); e[64, :] = 1.0
    c["e64"] = e
    c["ones64"] = np.ones((64, 64), np.float32)
    n = np.arange(128)
    a = 2 * np.pi * np.outer(n, n) / 128.0
    c["dft128"] = np.stack([np.cos(a), -np.sin(a), -np.cos(a)], 1).astype(BFNP)
    k1 = np.arange(128)[:, None]; n2 = np.arange(64)[None, :]
    t = 2 * np.pi * (k1 * n2) / 8192.0
    c["tw"] = np.stack([np.cos(t), np.sin(t)], 1).astype(np.float32)
    m = np.arange(64)
    a64 = 2 * np.pi * np.outer(m, m) / 64.0
    c["dft64"] = np.stack([np.cos(a64), np.sin(a64)], 1).astype(BFNP)
    c["cs64"] = np.concatenate([np.cos(a64), np.sin(a64)], 1).astype(np.float32)
    q = np.arange(256)
    a256 = 2 * np.pi * np.outer(q, q) / 256.0
    d = np.stack([np.cos(a256), -np.sin(a256)], 1)
    c["dft256"] = d.reshape(2, 128, 2, 256).transpose(1, 0, 2, 3).astype(BFNP)
    tt_ = np.arange(S)
    half = 16
    inv = 1.0 / (10000.0 ** (np.arange(0, half, 2, dtype=np.float32) / half))
    ang_r = (tt_ // 64).astype(np.float32)[:, None] * inv
    ang_c = (tt_ % 64).astype(np.float32)[:, None] * inv
    cos32 = np.concatenate([np.cos(ang_r), np.cos(ang_r), np.cos(ang_c), np.cos(ang_c)], 1)
    sin32 = np.concatenate([-np.sin(ang_r), np.sin(ang_r), -np.sin(ang_c), np.sin(ang_c)], 1)
    c["ropec"] = np.ascontiguousarray(np.concatenate([cos32, cos32], 1).T.astype(np.float32))
    c["ropes"] = np.ascontiguousarray(np.concatenate([sin32, sin32], 1).T.astype(np.float32))
    return c


ROPE_PERM = np.concatenate([np.arange(8, 16), np.arange(0, 8), np.arange(24, 32), np.arange(16, 24)])


def prep_A(inp, layer, x_cur, xc_cur, cst):
    maps = []
    w_in = inp["w_in"][layer]
    rb = inp["na_rel_bias"][layer]
    qc = np.arange(64)[:, None]; kc = np.arange(64)[None, :]
    col_lo = np.clip(qc - 8, 0, 48)
    ok = (kc >= col_lo) & (kc < col_lo + 16)
    dc = np.clip(kc - qc + 15, 0, 30)
    for core in range(8):
        b, g = core // 4, core % 4
        m = {}
        m["xT"] = np.ascontiguousarray(x_cur[b].T)
        m["cT"] = np.ascontiguousarray(xc_cur[b].T)
        cv = np.stack([inp["c"][b], inp["c_ctx"]], 0)
        m["cvec"] = np.ascontiguousarray(cv.reshape(2, 8, 128).transpose(2, 0, 1).reshape(128, 16))
        m["adaw"] = np.ascontiguousarray(inp["ada_w"][layer][:, 0:2048])
        m["adab"] = np.ascontiguousarray(inp["ada_b"][layer][0:2048].reshape(16, 128).T)
        m["g1"] = np.ascontiguousarray(inp["norm1_g"][layer].reshape(8, 128).T)
        def cols(off, perm=None):
            w = w_in[:, off + 64 * g: off + 64 * g + 64]
            if perm is not None:
                w = w[:, np.concatenate([perm, 32 + perm])]
            return w
        OFF = dict(A=0, BQ=256, BK=512, BV=768, CB=1024, CC=1280, CX=1536, DQ=1792, DK=2048, DV=2304)
        wh = np.concatenate([cols(OFF["DQ"]), cols(OFF["BQ"]), cols(OFF["DK"]), cols(OFF["BK"]),
                             cols(OFF["DQ"], ROPE_PERM), cols(OFF["CC"]), cols(OFF["DK"], ROPE_PERM), cols(OFF["CX"]),
                             cols(OFF["CB"]), cols(OFF["DV"]), cols(OFF["BV"])], 1)
        m["wh"] = np.ascontiguousarray(wh)
        m["waT"] = np.ascontiguousarray(cols(OFF["A"]).T)
        cw = np.zeros((128, 3), np.float32)
        cw[64:128, :] = inp["conv_w"][layer][:, 64 * g: 64 * g + 64].T
        m["cw"] = cw
        tb = rb[g][:, dc]
        tb = np.where(ok[None], tb, np.float32(-30000.0)).astype(np.float32)
        tb = np.ascontiguousarray(tb.transpose(2, 0, 1))
        tpad = np.concatenate([tb, np.zeros((64, 1, 64), np.float32)], 1)
        nab = np.zeros((2, 128, 16, 64), np.float32)
        nab[0, 0:64] = tpad; nab[0, 64:128, 0:15] = tpad[:, 1:16]
        nab[1, 64:128] = tpad; nab[1, 0:64, 0:15] = tpad[:, 1:16]
        m["nab"] = np.ascontiguousarray(nab.transpose(1, 0, 2, 3).reshape(128, 2 * 16 * 64))
        m["dl"] = np.ascontiguousarray(np.broadcast_to(inp["diff_lambda"][layer].reshape(1, 128), (128, 128)))
        m["subg"] = np.ascontiguousarray(inp["diff_subln_g"][layer].reshape(64, 1))
        for k in ("ident", "ones", "e64", "ones64", "dft128", "tw", "dft64", "cs64", "dft256", "ropec", "ropes"):
            m[k] = cst[k]
        maps.append(m)
    return maps


def build_A(layer, ctx_br, stop=99):
    lam_init = 0.8 - 0.6 * math.exp(-0.3 * layer)
    nc = bass.Bass("TRN2", target_bir_lowering=False)
    P = Prog(nc)

    def din(name, shape, dt=F32):
        return nc.dram_tensor(name, list(shape), dt, kind="ExternalInput").ap()

    def dout(name, shape, dt=BF16):
        return nc.dram_tensor(name, list(shape), dt, kind="ExternalOutput").ap()

    xT = din("xT", [1024, S]); cT = din("cT", [1024, L])
    cvec_d = din("cvec", [128, 16]); adaw_d = din("adaw", [1024, 2048]); adab_d = din("adab", [128, 16])
    g1_d = din("g1", [128, 8]); wh_d = din("wh", [1024, 704]); waT_d = din("waT", [64, 1024])
    cw_d = din("cw", [128, 3]); nab_d = din("nab", [128, 2048]); dl_d = din("dl", [128, 128]); subg_d = din("subg", [64, 1])
    ident_d = din("ident", [128, 128], BF16); ones_d = din("ones", [128, 128], BF16)
    e64_d = din("e64", [65, 64]); ones64_d = din("ones64", [64, 64])
    dft128_d = din("dft128", [128, 3, 128], BF16); tw_d = din("tw", [128, 2, 64])
    dft64_d = din("dft64", [64, 2, 64], BF16); cs64_d = din("cs64", [64, 128])
    dft256_d = din("dft256", [128, 2, 2, 256], BF16)
    ropec_d = din("ropec", [64, S]); ropes_d = din("ropes", [64, S])
    ya_d = dout("ya", [64, 8192]); yb_d = dout("ybT", [64, S]); yc_d = dout("ycT", [64, S]); yd_d = dout("ydT", [64, S])
    yctx_d = dout("yctx", [4, 64, L]) if ctx_br else None

    TQ = P.sb("TQ", [128, S], BF16)
    TK = P.sb("TK", [128, S + L], BF16)
    VD = P.sb("VD", [128, 66, 65], BF16)
    VB = P.sb("VB", [128, 66, 65], BF16)
    S1 = P.sb("S1", [128, S], BF16)
    S2 = P.sb("S2", [128, S + 2], BF16)
    S3 = P.sb("S3", [128, S], BF16)
    S4 = P.sb("S4", [128, 8, 512], F32)
    S5 = P.sb("S5", [128, 2, 8, 512], BF16)
    wbf = P.sb("wbf", [128, 8, WCOLS], BF16)
    tmpf = [P.sb(f"tmpf{i}", [128, 512], F32) for i in range(6)]
    tmpb = [P.sb(f"tmpb{i}", [128, 512], BF16) for i in range(4)]
    ident = P.sb("ident_s", [128, 128], BF16); ones = P.sb("ones_s", [128, 128], BF16)
    e64 = P.sb("e64_s", [65, 64], F32); ones64 = P.sb("ones64_s", [64, 64], F32)
    dft128 = P.sb("dft128_s", [128, 3, 128], BF16); tw = P.sb("tw_s", [128, 2, 64], F32)
    dft64 = P.sb("dft64_s", [64, 2, 64], BF16); cs64 = P.sb("cs64_s", [64, 128], F32)
    dft256 = P.sb("dft256_s", [128, 2, 2, 256], BF16)
    cvec = P.sb("cvec_s", [128, 16], F32); scv = P.sb("scv", [128, 16], F32)
    adab = P.sb("adab_s", [128, 16], F32); g1 = P.sb("g1_s", [128, 8], F32)
    modv = P.sb("modv", [128, 2, 16], F32)
    Amod = P.sb("Amod", [128, 2, 8], F32)
    cw = P.sb("cw_s", [128, 3], F32); nab = P.sb("nab_s", [128, 2, 16, 64], F32)
    dl = P.sb("dl_s", [128, 128], F32); subg = P.sb("subg_s", [64, 1], F32)
    lamt = P.sb("lamt", [128, 8], F32)
    epsb = P.sb("epsb", [128, 1], F32)
    ropec = P.sb("ropec_s", [64, 512], F32); ropes = P.sb("ropes_s", [64, 512], F32)
    Osb = [P.sb(f"Osb{i}", [65, 512], F32) for i in range(2)]
    obuf = [P.sb(f"obuf{i}", [64, 512], BF16) for i in range(2)]
    VBo = P.sb("VBo", [64, 66, 65], BF16)
    TQc = P.sb("TQc", [128, L], BF16)
    Uc = P.sb("Uc", [128, L + 2], BF16); CBc = P.sb("CBc", [128, L], BF16)
    Gtok = P.sb("Gtok", [128, 2, 128], BF16)
    ps = [P.ps(f"ps{i}", [128, 512], F32) for i in range(8)]

    P.op("pool", lambda e: e.memset(epsb[:], EPS), writes=["epsb"])
    waT = S3[:].bitcast(F32)[0:64, 0:1024]
    for (dst, src, key) in [(ident, ident_d, "ident"), (ones, ones_d, "ones"), (e64, e64_d, "e64"), (ones64, ones64_d, "ones64"),
                            (dft128, dft128_d, "dft128"), (tw, tw_d, "tw"), (dft64, dft64_d, "dft64"), (cs64, cs64_d, "cs64"),
                            (dft256, dft256_d, "dft256"), (cvec, cvec_d, "cvec"), (adab, adab_d, "adab"), (g1, g1_d, "g1"),
                            (cw, cw_d, "cw"), (dl, dl_d, "dl"), (subg, subg_d, "subg")]:
        P.dma("sp", dst[:], src, writes=[key])
    P.dma("sp", waT, waT_d, writes=["waT"])
    P.dma("sp", nab[:], nab_d.rearrange("p (v d q) -> p v d q", v=2, d=16), writes=["nab"])

    whv = wh_d.rearrange("(k p) c -> p k c", p=128)
    P.dma("sp", S4[:, :, 0:512], whv[:, :, 0:512], writes=["S4"])
    _cp(P, "dve", wbf[:, :, 0:512], S4[:, :, 0:512], reads=["S4"], writes=["wbf"])
    P.dma("sp", S4[:, :, 0:192], whv[:, :, 512:704], writes=["S4"])
    _cp(P, "dve", wbf[:, :, 512:704], S4[:, :, 0:192], reads=["S4"], writes=["wbf"])
    for k in range(8):
        _mm(P, ps[k % 2][:, 0:128], lhsT=waT[:, k * 128:(k + 1) * 128], rhs=cs64[:], reads=["waT", "cs64"], writes=[f"ps{k % 2}"])
        _cp(P, "act", wbf[:, k, W_GC:W_GC + 128], ps[k % 2][:, 0:128], reads=[f"ps{k % 2}"], writes=["wbf"])

    _act(P, scv[:], cvec[:], AF.Silu, reads=["cvec"], writes=["scv"])
    adv = adaw_d.rearrange("(k p) c -> p k c", p=128)
    for q4 in range(4):
        P.dma("sp", S4[:], adv[:, :, q4 * 512:(q4 + 1) * 512], writes=["S4"])
        for jj in range(4):
            j = q4 * 4 + jj
            for k in range(8):
                _mm(P, ps[2][:, 2 * j:2 * j + 2], lhsT=S4[:, k, jj * 128:(jj + 1) * 128], rhs=scv[:, k::8],
                    start=(k == 0), stop=(k == 7), reads=["S4", "scv"], writes=["ps2"])
    for v in range(2):
        _tt(P, "dve", modv[:, v, :], ps[2][:, v:32:2], adab[:], ALU.add, reads=["ps2", "adab"], writes=["modv"])
        _stt(P, "dve", Amod[:, v, :], modv[:, v, 8:16], 1.0, g1[:], ALU.add, ALU.mult, reads=["modv", "g1"], writes=["Amod"])

    _tt(P, "dve", tmpf[0][:, 0:32], dl[:, 0:32], dl[:, 32:64], ALU.mult, reads=["dl"], writes=["tmpf0"])
    _tt(P, "dve", tmpf[0][:, 32:64], dl[:, 64:96], dl[:, 96:128], ALU.mult, reads=["dl"], writes=["tmpf0"])
    P.op("dve", lambda e: e.reduce_sum(out=lamt[:, 0:1], in_=tmpf[0][:, 0:32], axis=AX.X), reads=["tmpf0"], writes=["lamt"])
    P.op("dve", lambda e: e.reduce_sum(out=lamt[:, 1:2], in_=tmpf[0][:, 32:64], axis=AX.X), reads=["tmpf0"], writes=["lamt"])
    _act(P, lamt[:, 2:4], lamt[:, 0:2], AF.Exp, reads=["lamt"], writes=["lamt"])
    _stt(P, "dve", lamt[:, 4:5], lamt[:, 3:4], -lam_init, lamt[:, 2:3], ALU.add, ALU.subtract, reads=["lamt"], writes=["lamt"])
    _ts(P, "dve", lamt[0:64, 5:6], subg[:], 1.0 - lam_init, None, ALU.mult, reads=["subg", "lamt"], writes=["lamt"])

    _barrier(P)

    if stop == 0:
        P.emit(); return nc
    GT = S1; U = S2; CB = S3
    sq = S5[:, 0]; hh = S5[:, 1]
    P.op("pool", lambda e: e.memset(U[:, 0:1], 0.0), writes=[("U", -1)])
    P.op("pool", lambda e: e.memset(U[:, S + 1:S + 2], 0.0), writes=[("U", 99)])
    P.op("pool", lambda e: e.memset(Uc[:], 0.0), writes=["Uc"])
    P.op("pool", lambda e: e.memset(VD[:, :, 64:65], 1.0), writes=["VDones"])
    P.op("pool", lambda e: e.memset(VB[:, :, 64:65], 1.0), writes=["VBones"])

    def chunk(src_ap, n, vec, j):
        lat = vec == 0
        P.dma("sp", S4[:, :, 0:n], src_ap, writes=["x"])
        if lat:
            P.dma("sp", ropec[:], ropec_d[:, j * 512:(j + 1) * 512], writes=["ropec"])
            P.dma("sp", ropes[:], ropes_d[:, j * 512:(j + 1) * 512], writes=["ropes"])
        _act(P, sq[:, :, 0:n], S4[:, :, 0:n], AF.Square, reads=["x"], writes=["sq"])
        for k in range(8):
            _mm(P, ps[0][:, 0:n], lhsT=ones[:], rhs=sq[:, k, 0:n], start=(k == 0), stop=(k == 7), reads=["sq", "ones"], writes=["ps0"])
        _act(P, tmpf[0][:, 0:n], ps[0][:, 0:n], AF.Sqrt, scale=1.0 / 1024.0, bias=epsb[:, 0:1], reads=["ps0"], writes=["tmpf0"])
        P.op("dve", lambda e: e.reciprocal(out=tmpf[1][:, 0:n], in_=tmpf[0][:, 0:n]), reads=["tmpf0"], writes=["rstd"])
        for k in range(8):
            t = tmpf[2 + (k % 2)]
            _stt(P, "dve", t[:, 0:n], S4[:, k, 0:n], Amod[:, vec, k:k + 1], tmpf[1][:, 0:n], ALU.mult, ALU.mult,
                 reads=["x", "rstd", "Amod"], writes=[f"tmpf{2 + k % 2}"])
            _act(P, hh[:, k, 0:n], t[:, 0:n], AF.Identity, bias=modv[:, vec, k:k + 1], reads=[f"tmpf{2 + k % 2}", "modv"], writes=[("h", k)])
        hkeys = [("h", k) for k in range(8)]

        def proj(pst, pkey, c0, m, p0=0):
            for k in range(8):
                _mm(P, pst[p0:p0 + m, 0:n], lhsT=wbf[:, k, c0:c0 + m], rhs=hh[:, k, 0:n], start=(k == 0), stop=(k == 7),
                    reads=hkeys + ["wbf"], writes=[pkey])

        tq_cols = slice(j * 512, (j + 1) * 512) if lat else None
        tk_cols = slice(j * 512, (j + 1) * 512) if lat else slice(S, S + L)
        proj(ps[1], "ps1", W_G1, 128)
        proj(ps[2], "ps2", W_G2, 128)
        proj(ps[3], "ps3", W_G3, 128)
        proj(ps[4], "ps4", W_G4, 128)
        if lat:
            for (pa, pak, pb_, pbk, dst, dk_) in [(ps[1], "ps1", ps[3], "ps3", TQ, ("TQ", j, 0)), (ps[2], "ps2", ps[4], "ps4", TK, ("TK", j, 0))]:
                _tt(P, "dve", tmpf[4][0:64, :], pa[0:64, :], ropec[:], ALU.mult, reads=[pak, "ropec"], writes=["tmpf4"])
                _tt(P, "dve", tmpf[5][0:64, :], pb_[0:64, :], ropes[:], ALU.mult, reads=[pbk, "ropes"], writes=["tmpf5"])
                _tt(P, "pool", dst[0:64, tq_cols], tmpf[4][0:64, :], tmpf[5][0:64, :], ALU.add, reads=["tmpf4", "tmpf5"], writes=[dk_])
            _cp(P, "act", TQ[64:128, tq_cols], ps[1][64:128, :], reads=["ps1"], writes=[("TQ", j, 1)])
            _cp(P, "act", TK[64:128, tk_cols], ps[2][64:128, :], reads=["ps2"], writes=[("TK", j, 1)])
        else:
            _cp(P, "act", TQc[:, :], ps[1][:, 0:n], reads=["ps1"], writes=["TQc"])
            _cp(P, "act", TK[:, tk_cols], ps[2][:, 0:n], reads=["ps2"], writes=[("TK", "c")])
        _cp(P, "act", tmpf[4][64:128, 0:n], ps[3][64:128, 0:n], reads=["ps3"], writes=["tmpf4"])
        if lat:
            _tt(P, "dve", U[64:128, 1 + j * 512:1 + (j + 1) * 512], tmpf[4][64:128, :], ps[4][64:128, :], ALU.mult,
                reads=["tmpf4", "ps4"], writes=[("U", j)])
        else:
            _tt(P, "dve", Uc[64:128, 1:1 + n], tmpf[4][64:128, 0:n], ps[4][64:128, 0:n], ALU.mult, reads=["tmpf4", "ps4", "Uc"], writes=["Uc"])
        proj(ps[5], "ps5", W_CB, 64, p0=64)
        if lat:
            _cp(P, "act", CB[64:128, j * 512:(j + 1) * 512], ps[5][64:128, :], reads=["ps5"], writes=[("CB", j)])
        else:
            _cp(P, "act", CBc[64:128, 0:n], ps[5][64:128, 0:n], reads=["ps5"], writes=["CBc"])
        if lat:
            proj(ps[6], "ps6", W_GC, 128)
            _cp(P, "act", GT[:, j * 512:(j + 1) * 512], ps[6][:, :], reads=["ps6"], writes=[("GT", j)])
        ntile = n // 128
        ncol = 128 if lat else 256
        for t in range(ntile):
            for k in range(8):
                _mm(P, ps[7][:, t * 128:(t + 1) * 128] if lat else ps[7][:, t * 256:(t + 1) * 256],
                    lhsT=hh[:, k, t * 128:(t + 1) * 128], rhs=wbf[:, k, W_V:W_V + ncol],
                    start=(k == 0), stop=(k == 7), reads=hkeys + ["wbf"], writes=["ps7"])
        t0 = j * 4 if lat else 64
        if lat:
            pv = ps[7][:, :].rearrange("p (t c) -> p t c", c=128)
            _cp(P, "dve", VD[:, t0:t0 + ntile, 0:64], pv[:, :, 0:64], reads=["ps7"], writes=[("VD", j)])
            _cp(P, "act", VB[:, t0:t0 + ntile, 0:64], pv[:, :, 64:128], reads=["ps7"], writes=[("VB", j)])
        else:
            pv = ps[7][:, :].rearrange("p (t c) -> p t c", c=256)
            _cp(P, "dve", VD[:, t0:t0 + ntile, 0:64], pv[:, :, 0:64], reads=["ps7"], writes=[("VD", "c")])
            _cp(P, "act", VB[:, t0:t0 + ntile, 0:64], pv[:, :, 64:128], reads=["ps7"], writes=[("VB", "c")])
            _cp(P, "dve", Gtok[:, :, :], pv[:, :, 128:256], reads=["ps7"], writes=["Gtok"])

    xv = xT.rearrange("(k p) t -> p k t", p=128)
    cv_ = cT.rearrange("(k p) t -> p k t", p=128)
    chunk(cv_, L, 1, None)
    import os
    for j in range(int(os.environ.get('CHUNKS', NCH))):
        chunk(xv[:, :, j * 512:(j + 1) * 512], 512, 0, j)

    if stop == 1:
        P.emit(); return nc
    P.dma("sp", VBo[:, :, :], VB[64:128, :, :], reads=[("VB", j) for j in range(NCH)] + [("VB", "c"), "VBones"], writes=["VBo"])
    def conv(Ut, CBt, n_tot, out_ap_fn, step):
        for c0 in range(0, n_tot, step):
            n = min(step, n_tot - c0)
            t = tmpf[0]; o = tmpb[0]
            _ts(P, "dve", t[64:128, 0:n], Ut[64:128, c0:c0 + n], cw[64:128, 0:1], None, ALU.mult, reads=["Uall", "cw"], writes=["tmpf0"])
            _stt(P, "dve", t[64:128, 0:n], Ut[64:128, c0 + 1:c0 + 1 + n], cw[64:128, 1:2], t[64:128, 0:n], ALU.mult, ALU.add,
                 reads=["Uall", "tmpf0"], writes=["tmpf0"])
            _stt(P, "dve", t[64:128, 0:n], Ut[64:128, c0 + 2:c0 + 2 + n], cw[64:128, 2:3], t[64:128, 0:n], ALU.mult, ALU.add,
                 reads=["Uall", "tmpf0"], writes=["tmpf0"])
            _tt(P, "dve", o[64:128, 0:n], t[64:128, 0:n], CBt[64:128, c0:c0 + n], ALU.mult, reads=["tmpf0", "CBall"], writes=["tmpb0"])
            P.dma("sp", out_ap_fn(c0, n), o[64:128, 0:n], reads=["tmpb0"])

    _barrier(P)
    conv(U, CB, S, lambda c0, n: yc_d[:, c0:c0 + n], 512)
    if ctx_br:
        conv(Uc, CBc, L, lambda c0, n: yctx_d[2, :, c0:c0 + n], 256)
    _barrier(P)

    if stop == 2:
        P.emit(); return nc
    Gsb = S4[:].rearrange("p a b -> p (a b)").bitcast(BF16).rearrange("p (n c) -> p n c", c=128)
    Apr = S3[:, 0:4096].rearrange("p (n c) -> p n c", c=64)
    Api = S3[:, 4096:8192].rearrange("p (n c) -> p n c", c=64)
    Atr = S2[0:64, 0:8192].rearrange("p (c k) -> p c k", k=128)
    Ati = S5[:].rearrange("p a b c -> p (a b c)")[0:64, 0:8192].rearrange("p (c k) -> p c k", k=128)
    psb = [p_[:].bitcast(BF16) for p_ in ps]
    for i in range(8):
        pb = psb[i % 2]
        for q in range(8):
            n2 = i * 8 + q
            _tr(P, pb[:, q * 128:(q + 1) * 128], GT[:, n2::64], ident[:], reads=["ident"], writes=[f"ps{i % 2}"])
        _cp(P, "act" if i % 2 else "dve", Gsb[:, i * 8:(i + 1) * 8, :], pb[:, :].rearrange("p (n c) -> p n c", c=128),
            reads=[f"ps{i % 2}"], writes=[("G", i)])
    for gI in range(8):
        gc = Gsb[:, gI * 8:(gI + 1) * 8, 0:64]; gs = Gsb[:, gI * 8:(gI + 1) * 8, 64:128]
        par, pai = ps[2 + (gI % 2) * 2], ps[3 + (gI % 2) * 2]
        kr, ki = f"ps{2 + (gI % 2) * 2}", f"ps{3 + (gI % 2) * 2}"
        _mm(P, par[:, :], lhsT=dft128[:, 0, :], rhs=gc, start=True, stop=False, reads=[("G", gI)], writes=[kr])
        _mm(P, par[:, :], lhsT=dft128[:, 1, :], rhs=gs, start=False, stop=True, reads=[("G", gI)], writes=[kr])
        _mm(P, pai[:, :], lhsT=dft128[:, 2, :], rhs=gs, start=True, stop=False, reads=[("G", gI)], writes=[ki])
        _mm(P, pai[:, :], lhsT=dft128[:, 1, :], rhs=gc, start=False, stop=True, reads=[("G", gI)], writes=[ki])
        tcb = tw[:, 0, gI * 8:(gI + 1) * 8].unsqueeze(2).broadcast_to([128, 8, 64])
        tsb = tw[:, 1, gI * 8:(gI + 1) * 8].unsqueeze(2).broadcast_to([128, 8, 64])
        v3 = lambda a: a[:, :].rearrange("p (n c) -> p n c", c=64)
        _tt(P, "dve", v3(tmpf[0]), v3(par), tcb, ALU.mult, reads=[kr], writes=["tmpf0"])
        _tt(P, "dve", v3(tmpf[1]), v3(pai), tsb, ALU.mult, reads=[ki], writes=["tmpf1"])
        _tt(P, "pool", Apr[:, gI * 8:(gI + 1) * 8, :], v3(tmpf[0]), v3(tmpf[1]), ALU.add, reads=["tmpf0", "tmpf1"], writes=[("Apr", gI)])
        _tt(P, "dve", v3(tmpf[2]), v3(pai), tcb, ALU.mult, reads=[ki], writes=["tmpf2"])
        _tt(P, "dve", v3(tmpf[3]), v3(par), tsb, ALU.mult, reads=[kr], writes=["tmpf3"])
        _tt(P, "pool", Api[:, gI * 8:(gI + 1) * 8, :], v3(tmpf[2]), v3(tmpf[3]), ALU.subtract, reads=["tmpf2", "tmpf3"], writes=[("Api", gI)])
    apr_all = [("Apr", i) for i in range(8)]; api_all = [("Api", i) for i in range(8)]
    for (src, dst, rk, nm) in [(Apr, Atr, apr_all, "Atr"), (Api, Ati, api_all, "Ati")]:
        for i in range(8):
            pb = psb[6 + (i % 2)]
            for q in range(8):
                c = i * 8 + q
                _tr(P, pb[0:64, q * 128:(q + 1) * 128], src[:, :, c], ident[:], reads=rk + ["ident"], writes=[f"ps{6 + i % 2}"])
            _cp(P, "act" if i % 2 else "dve", dst[:, i * 8:(i + 1) * 8, :], pb[0:64, :].rearrange("p (c k) -> p c k", k=128),
                reads=[f"ps{6 + i % 2}"], writes=[(nm, i)])
    nrm = 1.0 / math.sqrt(8192.0 * 64.0)
    for i in range(16):
        pp = ps[i % 2]; pk = f"ps{i % 2}"
        _mm(P, pp[0:64, :], lhsT=dft64[:, 0, :], rhs=Atr[:, i * 4:(i + 1) * 4, :], start=True, stop=False, reads=[("Atr", i // 2)], writes=[pk])
        _mm(P, pp[0:64, :], lhsT=dft64[:, 1, :], rhs=Ati[:, i * 4:(i + 1) * 4, :], start=False, stop=True, reads=[("Ati", i // 2)], writes=[pk])
        ob = tmpb[i % 2]
        _act(P, ob[0:64, :], pp[0:64, :], AF.Copy, scale=nrm, reads=[pk], writes=[f"tmpb{i % 2}"])
        P.dma("sp", ya_d[:, i * 512:(i + 1) * 512], ob[0:64, :], reads=[f"tmpb{i % 2}"])
    if ctx_br:
        for t in range(2):
            _mm(P, ps[2][0:64, 0:256], lhsT=Gtok[:, t, 0:64], rhs=dft256[:, t, 0, :], start=(t == 0), stop=False, reads=["Gtok", "dft256"], writes=["ps2"])
            _mm(P, ps[2][0:64, 0:256], lhsT=Gtok[:, t, 64:128], rhs=dft256[:, t, 1, :], start=False, stop=(t == 1), reads=["Gtok", "dft256"], writes=["ps2"])
        _act(P, tmpb[2][0:64, 0:256], ps[2][0:64, 0:256], AF.Copy, scale=1.0 / math.sqrt(256.0 * 64.0), reads=["ps2"], writes=["tmpb2"])
        P.dma("sp", yctx_d[0, :, :], tmpb[2][0:64, 0:256], reads=["tmpb2"])
    _barrier(P)

    if stop == 3:
        P.emit(); return nc
    def lbcast_recip(Ot, okey, n, dst, dkey, pp, pk):
        _mm(P, pp[0:64, 0:n], lhsT=e64[:], rhs=Ot[:, 0:n], reads=[okey, "e64"], writes=[pk])
        P.op("dve", lambda e: e.reciprocal(out=dst[0:64, 0:n], in_=pp[0:64, 0:n]), reads=[pk], writes=[dkey])

    def dense_attn(q_ap, k_tile_fn, v_tile_fn, nkt, n, scale, Ot, okey, pO, pOk, ps_s):
        def smm(kt):
            pp, pk = ps_s[kt % len(ps_s)]
            kap, kk = k_tile_fn(kt)
            _mm(P, pp[:, 0:n], lhsT=kap, rhs=q_ap[0], reads=list(kk) + list(q_ap[1]), writes=[pk])
        smm(0)
        for kt in range(nkt):
            if kt + 1 < nkt:
                smm(kt + 1)
            pp, pk = ps_s[kt % len(ps_s)]
            pt = tmpb[kt % 4]; ptk = f"tmpb{kt % 4}"
            _act(P, pt[:, 0:n], pp[:, 0:n], AF.Exp, scale=scale, reads=[pk], writes=[ptk])
            vap, vk = v_tile_fn(kt)
            _mm(P, pO[0:65, 0:n], lhsT=vap, rhs=pt[:, 0:n], start=(kt == 0), stop=(kt == nkt - 1), reads=list(vk) + [ptk], writes=[pOk])
        _cp(P, "dve", Ot[:, 0:n], pO[0:65, 0:n], reads=[pOk], writes=[okey])

    def diff_combine(n, out_dram):
        lbcast_recip(Osb[0], "Osb0", n, tmpf[0], "tmpf0", ps[6], "ps6")
        lbcast_recip(Osb[1], "Osb1", n, tmpf[1], "tmpf1", ps[7], "ps7")
        _tt(P, "dve", tmpf[2][0:64, 0:n], Osb[0][0:64, 0:n], tmpf[0][0:64, 0:n], ALU.mult, reads=["Osb0", "tmpf0"], writes=["tmpf2"])
        _tt(P, "pool", tmpf[3][0:64, 0:n], Osb[1][0:64, 0:n], tmpf[1][0:64, 0:n], ALU.mult, reads=["Osb1", "tmpf1"], writes=["tmpf3"])
        _stt(P, "dve", tmpf[2][0:64, 0:n], tmpf[3][0:64, 0:n], lamt[0:64, 4:5], tmpf[2][0:64, 0:n], ALU.mult, ALU.add,
             reads=["tmpf3", "tmpf2", "lamt"], writes=["tmpf2"])
        _act(P, tmpf[4][0:64, 0:n], tmpf[2][0:64, 0:n], AF.Square, reads=["tmpf2"], writes=["tmpf4"])
        _mm(P, ps[6][0:64, 0:n], lhsT=ones64[:], rhs=tmpf[4][0:64, 0:n], reads=["tmpf4", "ones64"], writes=["ps6"])
        _act(P, tmpf[4][0:64, 0:n], ps[6][0:64, 0:n], AF.Sqrt, scale=1.0 / 64.0, bias=epsb[0:64, 0:1], reads=["ps6"], writes=["tmpf4"])
        P.op("dve", lambda e: e.reciprocal(out=tmpf[5][0:64, 0:n], in_=tmpf[4][0:64, 0:n]), reads=["tmpf4"], writes=["tmpf5"])
        _stt(P, "dve", tmpb[0][0:64, 0:n], tmpf[2][0:64, 0:n], lamt[0:64, 5:6], tmpf[5][0:64, 0:n], ALU.mult, ALU.mult,
             reads=["tmpf2", "tmpf5", "lamt"], writes=["tmpb0"])
        P.dma("sp", out_dram, tmpb[0][0:64, 0:n], reads=["tmpb0"])

    tq_all = [("TQ", j, h) for j in range(NCH) for h in range(2)]
    tk_all = [("TK", j, h) for j in range(NCH) for h in range(2)] + [("TK", "c")]
    v_all = [("VD", j) for j in range(NCH)] + [("VB", j) for j in range(NCH)] + [("VD", "c"), ("VB", "c"), "VDones", "VBones"]

    sc_b = 64 ** -0.5
    for r8 in range(int(os.environ.get('NA_R8', 16))):
        pO = ps[4 + (r8 % 2)]; pOk = f"ps{4 + r8 % 2}"
        for rr in range(8):
            r = r8 * 8 + rr
            w0 = min(max(r - 4, 0), 120)
            dr0 = w0 - r + 7
            pw = ps[rr % 2]; pwk = f"ps{rr % 2}"
            pc = ps[2 + (rr % 2)]; pck = f"ps{2 + rr % 2}"
            qap = TQ[64:128, r * 64:(r + 1) * 64]
            for jw in range(8):
                w = w0 + jw
                _mm(P, pw[0:64, jw * 64:(jw + 1) * 64], lhsT=TK[64:128, w * 64:(w + 1) * 64], rhs=qap, writes=[pwk])
            for t in range(2):
                _mm(P, pc[:, t * 64:(t + 1) * 64], lhsT=TK[64:128, S + t * 128:S + (t + 1) * 128], rhs=qap, writes=[pck])
            bt = nab[0:64, 0, dr0:dr0 + 8, :]
            tf = tmpf[rr % 2]; tfk = f"tmpf{rr % 2}"
            _stt(P, "dve", tf[0:64, :].rearrange("p (a b) -> p a b", b=64), pw[0:64, :].rearrange("p (a b) -> p a b", b=64),
                 sc_b, bt, ALU.mult, ALU.add, reads=[pwk], writes=[tfk])
            pt = tmpb[rr % 2]; ptk = f"tmpb{rr % 2}"
            ptc = tmpb[2 + rr % 2]; ptck = f"tmpb{2 + rr % 2}"
            _act(P, pt[0:64, :], tf[0:64, :], AF.Exp, reads=[tfk], writes=[ptk])
            _act(P, ptc[:, 0:128], pc[:, 0:128], AF.Exp, scale=sc_b, reads=[pck], writes=[ptck])
            for jw in range(8):
                w = w0 + jw
                vsrc = VB if w % 2 == 0 else VBo
                _mm(P, pO[0:65, rr * 64:(rr + 1) * 64], lhsT=vsrc[0:64, w // 2, :],
                    rhs=pt[0:64, jw * 64:(jw + 1) * 64], start=(jw == 0), stop=False, reads=[ptk], writes=[pOk])
            for t in range(2):
                _mm(P, pO[0:65, rr * 64:(rr + 1) * 64], lhsT=VB[:, 64 + t, :], rhs=ptc[:, t * 64:(t + 1) * 64],
                    start=False, stop=(t == 1), reads=[ptck], writes=[pOk])
        Ot = Osb[r8 % 2]; okey = f"Osb{r8 % 2}"
        _cp(P, "dve", Ot[:, :], pO[0:65, :], reads=[pOk], writes=[okey])
        if os.environ.get('NA_EPI', '1') == '0':
            continue
        lbcast_recip(Ot, okey, 512, tmpf[2], "tmpf2", ps[6 + r8 % 2], f"ps{6 + r8 % 2}")
        ob = obuf[r8 % 2]; obk = f"obuf{r8 % 2}"
        _tt(P, "pool", ob[0:64, :], Ot[0:64, :], tmpf[2][0:64, :], ALU.mult, reads=[okey, "tmpf2"], writes=[obk])
        P.dma("sp", yb_d[:, r8 * 512:(r8 + 1) * 512], ob[0:64, :], reads=[obk])
    if ctx_br and os.environ.get('NA_CTX', '1') == '1':
        dense_attn((TQc[64:128, :], []), lambda kt: (TK[64:128, S + kt * 128:S + (kt + 1) * 128], []),
                   lambda kt: (VB[:, 64 + kt, :], []), 2, L, sc_b, Osb[0], "Osb0", ps[4], "ps4", [(ps[0], "ps0"), (ps[1], "ps1")])
        lbcast_recip(Osb[0], "Osb0", L, tmpf[2], "tmpf2", ps[6], "ps6")
        _tt(P, "pool", tmpb[2][0:64, 0:L], Osb[0][0:64, 0:L], tmpf[2][0:64, 0:L], ALU.mult, reads=["Osb0", "tmpf2"], writes=["tmpb2"])
        P.dma("sp", yctx_d[1, :, :], tmpb[2][0:64, 0:L], reads=["tmpb2"])
    _barrier(P)

    if stop == 4:
        P.emit(); return nc
    sc_d = 32 ** -0.5
    for qg in range(16):
        for mI in range(2):
            rows = slice(mI * 32, mI * 32 + 32)
            dense_attn((TQ[rows, qg * 512:(qg + 1) * 512], []),
                       lambda kt, rows=rows: (TK[rows, kt * 128:(kt + 1) * 128], []),
                       lambda kt: (VD[:, kt, :], []), 66, 512, sc_d, Osb[mI], f"Osb{mI}", ps[4 + mI], f"ps{4 + mI}",
                       [(ps[0], "ps0"), (ps[1], "ps1"), (ps[2], "ps2"), (ps[3], "ps3")])
        diff_combine(512, yd_d[:, qg * 512:(qg + 1) * 512])
    if ctx_br:
        for mI in range(2):
            rows = slice(mI * 32, mI * 32 + 32)
            dense_attn((TQc[rows, :], []), lambda kt, rows=rows: (TK[rows, S + kt * 128:S + (kt + 1) * 128], []),
                       lambda kt: (VD[:, 64 + kt, :], []), 2, L, sc_d, Osb[mI], f"Osb{mI}", ps[4 + mI], f"ps{4 + mI}",
                       [(ps[0], "ps0"), (ps[1], "ps1")])
        diff_combine(L, yctx_d[3, :, :])
    P.emit()
    return nc


import math
import numpy as np
import ml_dtypes

BFNP = ml_dtypes.bfloat16
EPS = 1e-6
NE = 256


def prep_B(inp, layer, x_cur, xc_cur, brT, brcT, last):
    maps = []
    for core in range(8):
        b, q = core // 4, core % 4
        m = {}
        xs = x_cur[b, q * 2048:(q + 1) * 2048].T
        bs = brT[b][:, q * 2048:(q + 1) * 2048]
        if not last:
            xs = np.concatenate([xs, xc_cur[b, q * 64:(q + 1) * 64].T], 1)
            bs = np.concatenate([bs, brcT[b][:, q * 64:(q + 1) * 64]], 1)
        m["xT"] = np.ascontiguousarray(xs, dtype=np.float32)
        m["brT"] = np.ascontiguousarray(bs)
        cv = np.stack([inp["c"][b], inp["c_ctx"]], 0)
        m["cvec"] = np.ascontiguousarray(cv.reshape(2, 8, 128).transpose(2, 0, 1).reshape(128, 16))
        m["adaw"] = inp["ada_w"][layer]
        m["adab"] = np.ascontiguousarray(inp["ada_b"][layer].reshape(48, 128).T)
        m["g1"] = np.ascontiguousarray(inp["norm1_g"][layer].reshape(8, 128).T)
        m["g2"] = np.ascontiguousarray(inp["norm2_g"][layer].reshape(8, 128).T)
        m["gf"] = np.ascontiguousarray(inp["final_norm_g"].reshape(8, 128).T)
        m["wgate"] = inp["w_branch_gate"][layer]
        m["wbr"] = inp["w_branch"][layer]
        m["wout"] = inp["w_out"][layer]
        m["wrt"] = inp["router_w"][layer]
        m["rbias"] = np.ascontiguousarray(np.broadcast_to(inp["router_bias"][layer][None, :], (128, NE)))
        m["ident"] = np.eye(128, dtype=np.float32).astype(BFNP)
        m["identf"] = np.eye(128, dtype=np.float32)
        m["ones"] = np.ones((128, 128), np.float32).astype(BFNP)
        maps.append(m)
    return maps


def build_B1(layer, last):
    import os
    T = 2048 if last else 2112
    NT = T // 128 if last else 17
    chunks = [(i * 512, 512, 0) for i in range(4)] + ([] if last else [(2048, 64, 1)])
    nc = bass.Bass("TRN2", target_bir_lowering=False)
    P = Prog(nc)

    def din(name, shape, dt=F32):
        return nc.dram_tensor(name, list(shape), dt, kind="ExternalInput").ap()

    xT = din("xT", [1024, T]); brT = din("brT", [1024, T], BF16)
    cvec_d = din("cvec", [128, 16]); adaw_d = din("adaw", [1024, 6144]); adab_d = din("adab", [128, 48])
    g1_d = din("g1", [128, 8]); g2_d = din("g2", [128, 8]); gf_d = din("gf", [128, 8])
    wgate_d = din("wgate", [4, 1024, 1024]); wbr_d = din("wbr", [4, 256, 1024]); wout_d = din("wout", [1024, 1024])
    wrt_d = din("wrt", [1024, NE]); rbias_d = din("rbias", [128, NE])
    ident_d = din("ident", [128, 128], BF16); identf_d = din("identf", [128, 128]); ones_d = din("ones", [128, 128], BF16)
    xmid_d = nc.dram_tensor("xmidT", [1024, T], F32, kind="ExternalOutput").ap()
    h2o_d = nc.dram_tensor("h2T", [1024, T], BF16, kind="ExternalOutput").ap()
    wro_d = nc.dram_tensor("wr", [NT * 128, NE], F32, kind="ExternalOutput").ap()
    modo_d = nc.dram_tensor("modvo", [128, 96], F32, kind="ExternalOutput").ap()

    H2 = P.sb("H2", [128, 8, T], BF16)
    Wr = P.sb("Wr", [128, NT, 260], F32)
    ident = P.sb("ident_s", [128, 128], BF16); identf = P.sb("identf_s", [128, 128], F32); ones = P.sb("ones_s", [128, 128], BF16)
    cvec = P.sb("cvec_s", [128, 16], F32); scv = P.sb("scv", [128, 16], F32)
    adab = P.sb("adab_s", [128, 48], F32)
    g1 = P.sb("g1_s", [128, 8], F32); g2 = P.sb("g2_s", [128, 8], F32); gf = P.sb("gf_s", [128, 8], F32)
    modv = P.sb("modv", [128, 2, 48], F32)
    Am1 = P.sb("Am1", [128, 2, 8], F32); Am2 = P.sb("Am2", [128, 2, 8], F32)
    epsb = P.sb("epsb", [128, 1], F32); zerob = P.sb("zerob", [128, 1], F32)
    rbias = P.sb("rbias_s", [128, NE], F32)
    wrt = P.sb("wrt_s", [128, 8, NE], F32)
    tmpf = [P.sb(f"tmpf{i}", [128, 512], F32) for i in range(5)]
    tmpb = [P.sb(f"tmpb{i}", [128, 512], BF16) for i in range(4)]
    rt = P.sb("rt", [128, 8, 8], F32); rs = P.sb("rs", [128, 64], F32)
    AR = P.sb("AR", [128, 56 * 1024], BF16)
    ps = [P.ps(f"ps{i}", [128, 512], F32) for i in range(8)]

    def carve(off_kb, nbytes, dt):
        a = AR[:, off_kb * 512: off_kb * 512 + nbytes // 2]
        return a if dt == BF16 else a.bitcast(F32)

    xc_ = carve(0, 16384, F32).rearrange("p (k t) -> p k t", k=8)
    sq = carve(16, 8192, BF16).rearrange("p (k t) -> p k t", k=8)
    hh = carve(24, 8192, BF16).rearrange("p (k t) -> p k t", k=8)
    brc = carve(32, 8192, BF16).rearrange("p (k t) -> p k t", k=8)
    macc = carve(40, 16384, F32).rearrange("p (k t) -> p k t", k=8)
    mrg = carve(56, 8192, BF16).rearrange("p (k t) -> p k t", k=8)
    wg = carve(64, 16384, BF16).rearrange("p (k c) -> p k c", k=8)
    wb = carve(80, 16384, BF16).rearrange("p (k c) -> p k c", k=8)
    stg = carve(96, 16384, F32).rearrange("p (k c) -> p k c", k=8)
    yacc = carve(0, NT * 4096, F32).rearrange("p (t f) -> p t f", t=NT)
    wG = carve(72, 8192, BF16).rearrange("p (k c) -> p k c", k=8)
    wU = carve(80, 8192, BF16).rearrange("p (k c) -> p k c", k=8)
    wD = carve(88, 8192, BF16).rearrange("p (c f) -> p c f", c=4)
    sgb = carve(96, 1024, BF16); hmb = carve(97, 1024, BF16)
    hmT = [carve(98 + i, 1024, BF16) for i in range(2)]
    xo = carve(72, 16384, F32).rearrange("p (k t) -> p k t", k=8)
    fo = carve(88, 16384, F32).rearrange("p (k t) -> p k t", k=8)
    sq2 = carve(104, 8192, BF16).rearrange("p (k t) -> p k t", k=8)

    for (dst, src, key) in [(ident, ident_d, "ident"), (identf, identf_d, "identf"), (ones, ones_d, "ones"), (cvec, cvec_d, "cvec"),
                            (adab, adab_d, "adab"), (g1, g1_d, "g1"), (g2, g2_d, "g2"), (gf, gf_d, "gf"), (rbias, rbias_d, "rbias")]:
        P.dma("sp", dst[:], src, writes=[key])
    P.dma("sp", wrt[:], wrt_d.rearrange("(k p) e -> p k e", p=128), writes=["wrt"])
    P.op("pool", lambda e: e.memset(epsb[:], EPS), writes=["epsb"])
    P.op("pool", lambda e: e.memset(zerob[:], 0.0), writes=["zerob"])
    P.op("pool", lambda e: e.memset(Wr[:], 0.0), writes=[("Wr", t_) for t_ in range(NT)])
    _act(P, scv[:], cvec[:], AF.Silu, reads=["cvec"], writes=["scv"])
    adv = adaw_d.rearrange("(k p) c -> p k c", p=128)
    for q12 in range(12):
        P.dma("sp", stg[:], adv[:, :, q12 * 512:(q12 + 1) * 512], writes=["stg"])
        for jj in range(4):
            j = q12 * 4 + jj
            for k in range(8):
                _mm(P, ps[2][:, 2 * j:2 * j + 2], lhsT=stg[:, k, jj * 128:(jj + 1) * 128], rhs=scv[:, k::8],
                    start=(k == 0), stop=(k == 7), reads=["stg", "scv"], writes=["ps2"])
    for v in range(2):
        _tt(P, "dve", modv[:, v, :], ps[2][:, v:96:2], adab[:], ALU.add, reads=["ps2", "adab"], writes=["modv"])
        _stt(P, "dve", Am1[:, v, :], modv[:, v, 8:16], 1.0, g1[:], ALU.add, ALU.mult, reads=["modv", "g1"], writes=["Am1"])
        _stt(P, "dve", Am2[:, v, :], modv[:, v, 32:40], 1.0, g2[:], ALU.add, ALU.mult, reads=["modv", "g2"], writes=["Am2"])
    for i in range(4):
        P.dma("pool", wb[:, 2 * i:2 * i + 2, :], wbr_d[i].rearrange("(h p) f -> p h f", p=128), writes=["wb"])
    _barrier(P)

    def norm_chunk(src, n, A_ap, B_ap, dst_bf=None, dst_f32=None, sqb=None, fkey="macc"):
        sqb = sq if sqb is None else sqb
        _act(P, sqb[:, :, 0:n], src[:, :, 0:n], AF.Square, reads=["x"], writes=["sq"])
        for k in range(8):
            _mm(P, ps[0][:, 0:n], lhsT=ones[:], rhs=sqb[:, k, 0:n], start=(k == 0), stop=(k == 7), reads=["sq", "ones"], writes=["ps0"])
        _act(P, tmpf[0][:, 0:n], ps[0][:, 0:n], AF.Sqrt, scale=1.0 / 1024.0, bias=epsb[:, 0:1], reads=["ps0"], writes=["tmpf0"])
        P.op("dve", lambda e: e.reciprocal(out=tmpf[1][:, 0:n], in_=tmpf[0][:, 0:n]), reads=["tmpf0"], writes=["rstd"])
        for k in range(8):
            t = tmpf[2 + (k % 2)]; tk = f"tmpf{2 + k % 2}"
            _stt(P, "dve", t[:, 0:n], src[:, k, 0:n], A_ap(k), tmpf[1][:, 0:n], ALU.mult, ALU.mult, reads=["x", "rstd"], writes=[tk])
            if dst_f32 is not None:
                _act(P, dst_f32[:, k, 0:n], t[:, 0:n], AF.Identity, bias=B_ap(k), reads=[tk], writes=[(fkey, k)])
                if dst_bf is not None:
                    _cp(P, "pool", dst_bf(k), dst_f32[:, k, 0:n], reads=[(fkey, k)], writes=[("h", k)])
            else:
                _act(P, dst_bf(k), t[:, 0:n], AF.Identity, bias=B_ap(k), reads=[tk], writes=[("h", k)])

    xv = xT.rearrange("(k p) t -> p k t", p=128)
    bv = brT.rearrange("(k p) t -> p k t", p=128)
    xmv = xmid_d.rearrange("(k p) t -> p k t", p=128)
    wov = wout_d.rearrange("(k p) f -> p k f", p=128)
    hkeys = [("h", k) for k in range(8)]
    for (c0, n, vec) in chunks:
        P.dma("sp", xc_[:, :, 0:n], xv[:, :, c0:c0 + n], writes=["x"])
        P.dma("sp", brc[:, :, 0:n], bv[:, :, c0:c0 + n], writes=["br"])
        norm_chunk(xc_, n, lambda k: Am1[:, vec, k:k + 1], lambda k: modv[:, vec, k:k + 1], lambda k: hh[:, k, 0:n])
        for i in range(4):
            wgv_ = wgate_d[i].rearrange("(k p) f -> p k f", p=128)
            P.dma("pool", wg[:, :, 0:512], wgv_[:, :, 0:512], reads=[], writes=["wgA"])
            P.dma("pool", wg[:, :, 512:1024], wgv_[:, :, 512:1024], reads=[], writes=["wgB"])
            for oc in range(8):
                pg = ps[1 + (oc % 2)]; pgk = f"ps{1 + oc % 2}"
                pp = ps[3 + (oc % 2)]; ppk = f"ps{3 + oc % 2}"
                for k in range(8):
                    _mm(P, pg[:, 0:n], lhsT=wg[:, k, oc * 128:(oc + 1) * 128], rhs=hh[:, k, 0:n], start=(k == 0), stop=(k == 7),
                        reads=hkeys + ["wgA" if oc < 4 else "wgB"], writes=[pgk])
                gt = tmpb[oc % 2]; gk = f"tmpb{oc % 2}"
                _act(P, gt[:, 0:n], pg[:, 0:n], AF.Sigmoid, reads=[pgk], writes=[gk])
                for h in range(2):
                    _mm(P, pp[:, 0:n], lhsT=wb[:, 2 * i + h, oc * 128:(oc + 1) * 128], rhs=brc[:, 2 * i + h, 0:n], start=(h == 0), stop=(h == 1),
                        reads=["br", "wb"], writes=[ppk])
                if i == 0:
                    _tt(P, "dve", macc[:, oc, 0:n], pp[:, 0:n], gt[:, 0:n], ALU.mult, reads=[ppk, gk], writes=[("macc", oc)])
                else:
                    t = tmpf[4]
                    _tt(P, "dve", t[:, 0:n], pp[:, 0:n], gt[:, 0:n], ALU.mult, reads=[ppk, gk], writes=["tmpf4"])
                    _tt(P, "pool", macc[:, oc, 0:n], macc[:, oc, 0:n], t[:, 0:n], ALU.add, reads=["tmpf4"], writes=[("macc", oc)])
        for oc in range(8):
            _cp(P, "act", mrg[:, oc, 0:n], macc[:, oc, 0:n], reads=[("macc", oc)], writes=[("mrg", oc)])
        P.dma("pool", wg[:, :, 0:512], wov[:, :, 0:512], writes=["wgA"])
        P.dma("pool", wg[:, :, 512:1024], wov[:, :, 512:1024], writes=["wgB"])
        mkeys = [("mrg", k) for k in range(8)]
        for oc in range(8):
            po = ps[5 + (oc % 2)]; pok = f"ps{5 + oc % 2}"
            for k in range(8):
                _mm(P, po[:, 0:n], lhsT=wg[:, k, oc * 128:(oc + 1) * 128], rhs=mrg[:, k, 0:n], start=(k == 0), stop=(k == 7),
                    reads=mkeys + ["wgA" if oc < 4 else "wgB"], writes=[pok])
            _stt(P, "dve", xc_[:, oc, 0:n], po[:, 0:n], modv[:, vec, 16 + oc:17 + oc], xc_[:, oc, 0:n], ALU.mult, ALU.add,
                 reads=[pok, "x"], writes=["x"])
        P.dma("sp", xmv[:, :, c0:c0 + n], xc_[:, :, 0:n], reads=["x"])
        norm_chunk(xc_, n, lambda k: Am2[:, vec, k:k + 1], lambda k: modv[:, vec, 24 + k:25 + k],
                   lambda k: H2[:, k, c0:c0 + n], dst_f32=macc)
        for tt_ in range((n + 127) // 128):
            tn = min(128, n - tt_ * 128)
            ti = c0 // 128 + tt_
            pr = ps[7]
            for k in range(8):
                _mm(P, pr[0:tn, 0:NE], lhsT=macc[:, k, tt_ * 128:tt_ * 128 + tn], rhs=wrt[:, k, :], start=(k == 0), stop=(k == 7),
                    reads=[("macc", kk) for kk in range(8)] + ["wrt"], writes=["ps7"])
            sc = tmpf[0][:, 0:NE]; sbv = tmpf[1][:, 0:NE]; ch = tmpf[2][:, 0:NE]
            _act(P, sc[0:tn], pr[0:tn, 0:NE], AF.Sigmoid, reads=["ps7"], writes=["tmpf0"])
            _tt(P, "dve", sbv[0:tn], sc[0:tn], rbias[0:tn, :], ALU.add, reads=["tmpf0", "rbias"], writes=["rstd"])
            for g in range(8):
                P.op("dve", lambda e, g=g, tn=tn, sbv=sbv: e.max(out=rt[0:tn, g, :], in_=sbv[0:tn, g * 32:(g + 1) * 32]), reads=["rstd"], writes=["rt"])
            _tt(P, "dve", rs[0:tn, 0:8], rt[0:tn, :, 0], rt[0:tn, :, 1], ALU.add, reads=["rt"], writes=["rs"])
            P.op("dve", lambda e, tn=tn: e.max(out=rs[0:tn, 8:16], in_=rs[0:tn, 0:8]), reads=["rs"], writes=["rs"])
            _ts(P, "dve", rs[0:tn, 16:24], rs[0:tn, 0:8], rs[0:tn, 11:12], None, ALU.is_ge, reads=["rs"], writes=["rs"])
            _ts(P, "dve", rs[0:tn, 24:32], rs[0:tn, 16:24], 1.0, 1.0e9, ALU.subtract, ALU.mult, reads=["rs"], writes=["rs"])
            gm = rs[0:tn, 16:24].unsqueeze(2).broadcast_to([tn, 8, 32])
            pen = rs[0:tn, 24:32].unsqueeze(2).broadcast_to([tn, 8, 32])
            v3 = lambda a: a.rearrange("p (g e) -> p g e", e=32)
            _tt(P, "dve", v3(ch[0:tn]), v3(sbv[0:tn]), gm, ALU.mult, reads=["rstd", "rs"], writes=["tmpf2"])
            _tt(P, "dve", v3(ch[0:tn]), v3(ch[0:tn]), pen, ALU.add, reads=["tmpf2", "rs"], writes=["tmpf2"])
            P.op("dve", lambda e, tn=tn, ch=ch: e.max(out=rs[0:tn, 32:40], in_=ch[0:tn]), reads=["tmpf2"], writes=["rs"])
            _ts(P, "dve", ch[0:tn], ch[0:tn], rs[0:tn, 39:40], None, ALU.is_ge, reads=["tmpf2", "rs"], writes=["tmpf2"])
            _tt(P, "dve", sc[0:tn], sc[0:tn], ch[0:tn], ALU.mult, reads=["tmpf0", "tmpf2"], writes=["tmpf0"])
            P.op("dve", lambda e, tn=tn, sc=sc: e.reduce_sum(out=rs[0:tn, 40:41], in_=sc[0:tn], axis=AX.X), reads=["tmpf0"], writes=["rs"])
            P.op("dve", lambda e, tn=tn: e.reciprocal(out=rs[0:tn, 41:42], in_=rs[0:tn, 40:41]), reads=["rs"], writes=["rs"])
            _ts(P, "dve", Wr[0:tn, ti, 0:NE], sc[0:tn], rs[0:tn, 41:42], 2.5, ALU.mult, ALU.mult, reads=["tmpf0", "rs"], writes=[("Wr", ti)])
    P.dma("sp", h2o_d.rearrange("(k p) t -> p k t", p=128), H2[:], reads=[("h", k) for k in range(8)])
    P.dma("sp", wro_d.rearrange("(t p) e -> p t e", p=128), Wr[:, :, 0:NE], reads=[("Wr", t_) for t_ in range(NT)])
    P.dma("sp", modo_d, modv[:].rearrange("p v j -> p (v j)"), reads=["modv"])
    P.emit()
    return nc


def build_B2(last, NG=8):
    NT = 16 if last else 17
    TG = NT * 128
    TA = NG * TG
    nc = bass.Bass("TRN2", target_bir_lowering=False)
    P = Prog(nc)

    def din(name, shape, dt=F32):
        return nc.dram_tensor(name, list(shape), dt, kind="ExternalInput").ap()

    h2_d = din("h2a", [1024, TA], BF16); wr_d = din("wra", [TA, 33])
    ewg_d = din("ewg", [33, 1024, 256]); ewu_d = din("ewu", [33, 1024, 256]); ewd_d = din("ewd", [33, 256, 1024])
    ident_d = din("ident", [128, 128], BF16)
    yp_d = nc.dram_tensor("ypart", [TA, 1024], F32, kind="ExternalOutput").ap()
    H2s = [P.sb(f"H2_{i}", [128, 8, TG], BF16) for i in range(2)]
    Wrs = [P.sb(f"Wr_{i}", [128, NT, 33], F32) for i in range(2)]
    ident = P.sb("ident_s", [128, 128], BF16)
    yacc = P.sb("yacc", [128, NT, 1024], F32)
    wG = [P.sb(f"wG{i}", [128, 8, 512], BF16) for i in range(2)]
    wU = [P.sb(f"wU{i}", [128, 8, 512], BF16) for i in range(2)]
    wD = [P.sb(f"wD{i}", [128, 4, 1024], BF16) for i in range(2)]
    sgb = [P.sb(f"sgb{i}", [128, 512], BF16) for i in range(2)]
    hmb = [P.sb(f"hmb{i}", [128, 512], BF16) for i in range(2)]
    hmT = [P.sb(f"hmT{i}", [128, 512], BF16) for i in range(2)]
    ps = [P.ps(f"ps{i}", [128, 512], F32) for i in range(8)]
    P.dma("sp", ident[:], ident_d, writes=["ident"])
    units = [(2 * p_, 2) for p_ in range(16)] + [(32, 1)]
    ewg_v = ewg_d.rearrange("e (k p) h -> e p k h", p=128)
    ewu_v = ewu_d.rearrange("e (k p) h -> e p k h", p=128)
    ewd_v = ewd_d.rearrange("e (c p) f -> e p c f", p=128)
    h2v = h2_d.rearrange("(k p) t -> p k t", p=128)
    wrv = wr_d.rearrange("(t p) e -> p t e", p=128)
    ypv = yp_d.rearrange("(t p) f -> p t f", p=128)
    ucnt = 0
    icnt = 0
    def load_group(gi):
        P.dma("sp", H2s[gi % 2][:], h2v[:, :, gi * TG:(gi + 1) * TG], writes=[f"H2_{gi % 2}"])
        P.dma("sp", Wrs[gi % 2][:], wrv[:, gi * NT:(gi + 1) * NT, :], writes=[f"Wr_{gi % 2}"])
    load_group(0)
    for gi in range(NG):
        H2 = H2s[gi % 2]; Wr = Wrs[gi % 2]
        hk = f"H2_{gi % 2}"; wk = f"Wr_{gi % 2}"
        if gi + 1 < NG:
            load_group(gi + 1)
        P.op("pool", lambda e: e.memset(yacc[:], 0.0), writes=["yacc"] + [("yacc", t, h) for t in range(NT) for h in range(2)])
        items = []
        for (e0, ne) in units:
            wb_ = ucnt % 2
            ucnt += 1
            for t in range(NT):
                items.append((e0, ne, wb_, t, t == 0, icnt % 2))
                icnt += 1

        def s1(it):
            e0, ne, wb_, t, first, par = it
            W = ne * 256
            if first:
                for j in range(ne):
                    P.dma("pool", wG[wb_][:, :, j * 256:(j + 1) * 256], ewg_v[e0 + j], writes=[f"wG{wb_}"])
                    P.dma("pool", wU[wb_][:, :, j * 256:(j + 1) * 256], ewu_v[e0 + j], writes=[f"wU{wb_}"])
                    P.dma("pool", wD[wb_][:, 2 * j:2 * j + 2, :], ewd_v[e0 + j], writes=[f"wD{wb_}"])
            pG, pGk = ps[par * 2], f"ps{par * 2}"
            pU, pUk = ps[par * 2 + 1], f"ps{par * 2 + 1}"
            for k in range(8):
                _mm(P, pG[:, 0:W], lhsT=H2[:, k, t * 128:(t + 1) * 128], rhs=wG[wb_][:, k, 0:W], start=(k == 0), stop=(k == 7),
                    reads=[f"wG{wb_}", hk], writes=[pGk])
            for k in range(8):
                _mm(P, pU[:, 0:W], lhsT=H2[:, k, t * 128:(t + 1) * 128], rhs=wU[wb_][:, k, 0:W], start=(k == 0), stop=(k == 7),
                    reads=[f"wU{wb_}", hk], writes=[pUk])
            _act(P, sgb[par][:, 0:W], pG[:, 0:W], AF.Silu, reads=[pGk], writes=[f"sgb{par}"])
            for j in range(ne):
                _stt(P, "dve", hmb[par][:, j * 256:(j + 1) * 256], pU[:, j * 256:(j + 1) * 256], Wr[:, t, e0 + j:e0 + j + 1],
                     sgb[par][:, j * 256:(j + 1) * 256], ALU.mult, ALU.mult, reads=[pUk, f"sgb{par}", wk], writes=[f"hmb{par}"])

        def s2(it):
            e0, ne, wb_, t, first, par = it
            pT = ps[4 + par][:].bitcast(BF16); pTk = f"ps{4 + par}"
            for c in range(2 * ne):
                _tr(P, pT[:, c * 128:(c + 1) * 128], hmb[par][:, c * 128:(c + 1) * 128], ident[:], reads=[f"hmb{par}", "ident"], writes=[pTk])
            _cp(P, "act", hmT[par][:, 0:2 * ne * 128], pT[:, 0:2 * ne * 128], reads=[pTk], writes=[f"hmT{par}"])

        def s3(it):
            e0, ne, wb_, t, first, par = it
            for half in range(2):
                py = ps[6 + half]; pyk = f"ps{6 + half}"
                for c in range(2 * ne):
                    _mm(P, py[:, :], lhsT=hmT[par][:, c * 128:(c + 1) * 128], rhs=wD[wb_][:, c, half * 512:(half + 1) * 512],
                        start=(c == 0), stop=(c == 2 * ne - 1), reads=[f"hmT{par}", f"wD{wb_}"], writes=[pyk])
                _tt(P, "dve", yacc[:, t, half * 512:(half + 1) * 512], yacc[:, t, half * 512:(half + 1) * 512], py[:, :], ALU.add,
                    reads=[pyk], writes=[("yacc", t, half)])

        n_it = len(items)
        for idx in range(n_it + 2):
            if idx < n_it:
                s1(items[idx])
            if 0 <= idx - 1 < n_it:
                s2(items[idx - 1])
            if 0 <= idx - 2 < n_it:
                s3(items[idx - 2])
        P.dma("sp", ypv[:, gi * NT:(gi + 1) * NT, :], yacc[:], reads=[("yacc", t, h) for t in range(NT) for h in range(2)])
    P.emit()
    return nc


def build_B3(last):
    T = 2048 if last else 2112
    chunks = [(i * 512, 512, 0) for i in range(4)] + ([] if last else [(2048, 64, 1)])
    nc = bass.Bass("TRN2", target_bir_lowering=False)
    P = Prog(nc)

    def din(name, shape, dt=F32):
        return nc.dram_tensor(name, list(shape), dt, kind="ExternalInput").ap()

    yp_d = din("yp8", [8, T, 1024]); xm_d = din("xmi", [1024, T]); mod_d = din("modvi", [128, 96]); gf_d = din("gf", [128, 8])
    identf_d = din("identf", [128, 128]); ones_d = din("ones", [128, 128], BF16)
    out_d = nc.dram_tensor("outT", [1024, T], F32, kind="ExternalOutput").ap()
    modv = P.sb("modv", [128, 2, 48], F32); gf = P.sb("gf_s", [128, 8], F32)
    identf = P.sb("identf_s", [128, 128], F32); ones = P.sb("ones_s", [128, 128], BF16)
    epsb = P.sb("epsb", [128, 1], F32); zerob = P.sb("zerob", [128, 1], F32)
    ysum = [P.sb(f"ysum{i}", [128, 8, 1024], F32) for i in range(2)]
    xo = P.sb("xo", [128, 8, 512], F32); fo = P.sb("fo", [128, 8, 512], F32); sq = P.sb("sq", [128, 8, 512], BF16)
    ytile = P.sb("ytile", [128, 4, 1024], F32)
    tmpf = [P.sb(f"tmpf{i}", [128, 512], F32) for i in range(5)]
    ps = [P.ps(f"ps{i}", [128, 512], F32) for i in range(8)]
    P.dma("sp", modv[:].rearrange("p v j -> p (v j)"), mod_d, writes=["modv"])
    P.dma("sp", gf[:], gf_d, writes=["gf"]); P.dma("sp", identf[:], identf_d, writes=["identf"]); P.dma("sp", ones[:], ones_d, writes=["ones"])
    P.op("pool", lambda e: e.memset(epsb[:], EPS), writes=["epsb"])
    P.op("pool", lambda e: e.memset(zerob[:], 0.0), writes=["zerob"])
    ypv = yp_d.rearrange("c t f -> t c f")
    xmv = xm_d.rearrange("(k p) t -> p k t", p=128)
    outv = out_d.rearrange("(k p) t -> p k t", p=128)
    ti_g = 0
    for (c0, n, vec) in chunks:
        P.dma("sp", xo[:, :, 0:n], xmv[:, :, c0:c0 + n], writes=["x"])
        ntl = (n + 127) // 128
        for tt_ in range(ntl):
            tn = min(128, n - tt_ * 128)
            ys = ysum[ti_g % 2]; ysk = f"ysum{ti_g % 2}"
            ti_g += 1
            P.dma("sp", ys[0:tn], ypv[c0 + tt_ * 128:c0 + tt_ * 128 + tn, :, :], writes=[ysk])
            _tt(P, "dve", ys[0:tn, 0:4, :], ys[0:tn, 0:4, :], ys[0:tn, 4:8, :], ALU.add, reads=[ysk], writes=[ysk])
            _tt(P, "pool", ys[0:tn, 0:2, :], ys[0:tn, 0:2, :], ys[0:tn, 2:4, :], ALU.add, reads=[ysk], writes=[ysk])
            _tt(P, "dve", ytile[0:tn, tt_, :], ys[0:tn, 0, :], ys[0:tn, 1, :], ALU.add, reads=[ysk], writes=[("yt", tt_)])
        for oc in range(8):
            py = ps[oc % 2]; pyk = f"ps{oc % 2}"
            for tt_ in range(ntl):
                tn = min(128, n - tt_ * 128)
                _tr(P, py[:, tt_ * 128:tt_ * 128 + tn], ytile[0:tn, tt_, oc * 128:(oc + 1) * 128], identf[0:tn, 0:tn],
                    reads=[("yt", tt_), "identf"], writes=[pyk])
            _stt(P, "dve", xo[:, oc, 0:n], py[:, 0:n], modv[:, vec, 40 + oc:41 + oc], xo[:, oc, 0:n], ALU.mult, ALU.add,
                 reads=[pyk, "x", "modv"], writes=["x"])
        if last:
            _act(P, sq[:, :, 0:n], xo[:, :, 0:n], AF.Square, reads=["x"], writes=["sq"])
            for k in range(8):
                _mm(P, ps[2][:, 0:n], lhsT=ones[:], rhs=sq[:, k, 0:n], start=(k == 0), stop=(k == 7), reads=["sq", "ones"], writes=["ps2"])
            _act(P, tmpf[0][:, 0:n], ps[2][:, 0:n], AF.Sqrt, scale=1.0 / 1024.0, bias=epsb[:, 0:1], reads=["ps2", "epsb"], writes=["tmpf0"])
            P.op("dve", lambda e, n=n: e.reciprocal(out=tmpf[1][:, 0:n], in_=tmpf[0][:, 0:n]), reads=["tmpf0"], writes=["rstd"])
            for k in range(8):
                _stt(P, "dve", fo[:, k, 0:n], xo[:, k, 0:n], gf[:, k:k + 1], tmpf[1][:, 0:n], ALU.mult, ALU.mult, reads=["x", "rstd", "gf"], writes=[("fo", k)])
            P.dma("sp", outv[:, :, c0:c0 + n], fo[:, :, 0:n], reads=[("fo", k) for k in range(8)])
        else:
            P.dma("sp", outv[:, :, c0:c0 + n], xo[:, :, 0:n], reads=["x"])
    P.emit()
    return nc


def _run(nc, maps):
    res = run_bass_kernel_spmd(nc, maps, core_ids=list(range(8)))
    return res.results


def kernel(**inp):
    inp = {k: np.asarray(v) for k, v in inp.items()}
    cst = consts_A()
    x_cur = np.ascontiguousarray(inp["x"], dtype=np.float32)
    xc_cur = np.ascontiguousarray(inp["ctx"], dtype=np.float32)
    ident = np.eye(128, dtype=np.float32).astype(BFNP)
    identf = np.eye(128, dtype=np.float32)
    ones = np.ones((128, 128), np.float32).astype(BFNP)
    for layer in range(2):
        last = layer == 1
        ra = _run(build_A(layer, not last), prep_A(inp, layer, x_cur, xc_cur, cst))
        brT = np.zeros((2, 1024, S), dtype=BFNP)
        brcT = None if last else np.zeros((2, 1024, L), dtype=BFNP)
        for core in range(8):
            b, g = core // 4, core % 4
            r = ra[core]
            ya = np.asarray(r["ya"]).reshape(64, 64, 128).transpose(1, 0, 2).reshape(64, S)
            for i, arr in enumerate([ya, np.asarray(r["ybT"]), np.asarray(r["ycT"]), np.asarray(r["ydT"])]):
                brT[b, i * 256 + 64 * g:i * 256 + 64 * g + 64, :] = arr
            if not last:
                yc = np.asarray(r["yctx"])
                for i in range(4):
                    brcT[b, i * 256 + 64 * g:i * 256 + 64 * g + 64, :] = yc[i]
        del ra
        r1 = _run(build_B1(layer, last), prep_B(inp, layer, x_cur, xc_cur, brT, brcT, last))
        T = 2048 if last else 2112
        NT = 16 if last else 17
        TG = NT * 128
        h2_all = np.zeros((1024, 8 * TG), dtype=BFNP)
        wr_full = np.zeros((8 * TG, NE), dtype=np.float32)
        for tc in range(8):
            h2_all[:, tc * TG:tc * TG + T] = np.asarray(r1[tc]["h2T"])
            wr_full[tc * TG:(tc + 1) * TG] = np.asarray(r1[tc]["wr"])
        xmid = [np.asarray(r1[tc]["xmidT"]) for tc in range(8)]
        modvo = [np.asarray(r1[tc]["modvo"]) for tc in range(8)]
        del r1
        maps = []
        for c in range(8):
            m = {"h2a": h2_all, "ident": ident}
            wra = np.zeros((8 * TG, 33), dtype=np.float32)
            wra[:, 0:32] = wr_full[:, 32 * c:32 * c + 32]
            wra[:, 32] = 1.0 if c == 0 else 0.0
            m["wra"] = wra
            m["ewg"] = np.concatenate([inp["expert_w_gate"][layer][32 * c:32 * c + 32], inp["shared_w_gate"][layer][None]], 0)
            m["ewu"] = np.concatenate([inp["expert_w_up"][layer][32 * c:32 * c + 32], inp["shared_w_up"][layer][None]], 0)
            m["ewd"] = np.concatenate([inp["expert_w_down"][layer][32 * c:32 * c + 32], inp["shared_w_down"][layer][None]], 0)
            maps.append(m)
        r2 = _run(build_B2(last), maps)
        yparts = [np.asarray(r2[c]["ypart"]) for c in range(8)]
        del r2, maps
        maps = []
        gfv = np.ascontiguousarray(inp["final_norm_g"].reshape(8, 128).T)
        for tc in range(8):
            m = {"yp8": np.ascontiguousarray(np.stack([yparts[c][tc * TG:tc * TG + T] for c in range(8)], 0)),
                 "xmi": xmid[tc], "modvi": modvo[tc], "gf": gfv, "identf": identf, "ones": ones}
            maps.append(m)
        r3 = _run(build_B3(last), maps)
        x_new = np.zeros_like(x_cur)
        xc_new = np.zeros_like(xc_cur)
        for tc in range(8):
            b, q = tc // 4, tc % 4
            o = np.asarray(r3[tc]["outT"])
            x_new[b, q * 2048:(q + 1) * 2048] = o[:, 0:2048].T
            if not last:
                xc_new[b, q * 64:(q + 1) * 64] = o[:, 2048:2112].T
        x_cur, xc_cur = x_new, xc_new
        del r3, yparts, maps
    return np.ascontiguousarray(x_cur, dtype=np.float32)
```
